# Optimizing a Trainium2 kernel written in Bass

```python
import jax, jax.numpy as jnp
from jax import lax
import numpy as np

D_MODEL = 1024
BATCH = 4
SEQ = 4096
DEPTH = 4

GRID_W = 64
CTX_LEN = 256
EPS = 1e-6
F_FLOOR = 1e-20
N_MOD = 6
HG_WIDTH = 512
HG_HEADS = 4
HG_DIM = HG_WIDTH // HG_HEADS
CHUNK = 16
SC_WIDTH = 512
SC_HALF = SC_WIDTH // 2
CONV_W = 3
IN_COLS = 5 * HG_WIDTH + 3 * SC_WIDTH
SPLITS = tuple(HG_WIDTH * i for i in range(1, 6)) + (5 * HG_WIDTH + SC_WIDTH, 5 * HG_WIDTH + 2 * SC_WIDTH)
PEER_HEADS = 8
PEER_QDIM = 256
PEER_HALF = PEER_QDIM // 2
N_KEYS = 128
N_EXPERTS = N_KEYS * N_KEYS
PEER_TOPK = 16
PEER_BLOCK = 128

kernel_name = 'hybrid_hgrn2_shortconv_peer_dit'


def rms_norm(x, g):
    xf = x.astype(jnp.float32)
    y = xf * lax.rsqrt(jnp.mean(xf * xf, axis=-1, keepdims=True) + EPS)
    return (y * g.astype(jnp.float32)).astype(x.dtype)


def modulate(h, shift, scale):
    return h * (1 + scale) + shift


def to_heads(a):
    return a.reshape(a.shape[0], a.shape[1], HG_HEADS, HG_DIM).astype(jnp.float32)


def flip(a):
    return a[:, ::-1]


def forget_terms(z, lb):
    f = lb + (1.0 - lb) * jax.nn.sigmoid(z)
    logf = jnp.log(jnp.maximum(f, F_FLOOR))
    key = (1.0 - lb) * jax.nn.sigmoid(-z)
    return logf, key


def gla_chunk(q, k, v, logf, s0):
    bsz, t, h, _ = q.shape
    dv = v.shape[-1]
    n = t // CHUNK
    q, k, v, logf = (a.reshape(bsz, n, CHUNK, h, a.shape[-1]) for a in (q, k, v, logf))
    b = jnp.cumsum(logf, axis=2)
    mask = jnp.tril(jnp.ones((CHUNK, CHUNK), bool))[None, None, :, :, None, None]
    diff = b[:, :, :, None] - b[:, :, None, :]
    decay = jnp.where(mask, jnp.exp(jnp.where(mask, diff, 0.0)), 0.0)
    att = jnp.einsum('bnthk,bntshk,bnshk->bnhts', q, decay, k)
    o_intra = jnp.einsum('bnhts,bnshv->bnthv', att, v)
    b_last = b[:, :, -1]
    q_in = q * jnp.exp(b)
    k_out = k * jnp.exp(b_last[:, :, None] - b)

    def step(s, xs):
        qc, kc, vc, dc = xs
        o = jnp.einsum('bchk,bhkv->bchv', qc, s)
        s = dc[..., None] * s + jnp.einsum('bchk,bchv->bhkv', kc, vc)
        return s, o

    xs = tuple(jnp.moveaxis(a, 1, 0) for a in (q_in, k_out, v, jnp.exp(b_last)))
    s_final, o_inter = lax.scan(step, s0, xs)
    o = o_intra + jnp.moveaxis(o_inter, 0, 1)
    return o.reshape(bsz, t, h, dv), s_final


def gla_final_state(k, v, logf):
    b = jnp.cumsum(logf, axis=1)
    kd = k * jnp.exp(b[:, -1:] - b)
    return jnp.einsum('bthk,bthv->bhkv', kd, v)


def hgrn2_mix(q, iv, zf, zb, g, lbf, lbb, s0f, s0b):
    q, iv, zf, zb = (to_heads(a) for a in (q, iv, zf, zb))
    logf_f, k_f = forget_terms(zf, lbf)
    o_f, s_f = gla_chunk(q, k_f, iv, logf_f, s0f)
    logf_b, k_b = forget_terms(zb, lbb)
    o_b, s_b = gla_chunk(flip(q), flip(k_b), flip(iv), flip(logf_b), s0b)
    o = o_f + flip(o_b)
    o = o * lax.rsqrt(jnp.mean(o * o, axis=-1, keepdims=True) + EPS)
    o = o.reshape(g.shape).astype(g.dtype) * jax.nn.silu(g)
    return o, s_f, s_b


def hgrn2_final_states(iv, zf, zb, lbf, lbb):
    iv, zf, zb = (to_heads(a) for a in (iv, zf, zb))
    logf_f, k_f = forget_terms(zf, lbf)
    s_f = gla_final_state(k_f, iv, logf_f)
    logf_b, k_b = forget_terms(zb, lbb)
    s_b = gla_final_state(flip(k_b), flip(iv), flip(logf_b))
    return s_f, s_b


def conv3(u, w, axis):
    n = u.shape[axis]
    pad = [(0, 0)] * u.ndim
    pad[axis] = (1, 1)
    up = jnp.pad(u, pad)
    return (w[0] * lax.slice_in_dim(up, 0, n, axis=axis)
            + w[1] * lax.slice_in_dim(up, 1, n + 1, axis=axis)
            + w[2] * lax.slice_in_dim(up, 2, n + 2, axis=axis))


def grid_conv(u, w, rows):
    bsz, t, ch = u.shape
    grid = u.reshape(bsz, rows, GRID_W, ch)
    horiz = conv3(grid[..., :SC_HALF], w[:, :SC_HALF], axis=2)
    vert = conv3(grid[..., SC_HALF:], w[:, SC_HALF:], axis=1)
    return jnp.concatenate([horiz, vert], axis=-1).reshape(bsz, t, ch)


def seq_conv(u, w):
    return conv3(u, w, axis=1)


def token_mixer(h, w_in_l, w_out_l, conv_w_l, lbf, lbb, s0f, s0b, conv_fn):
    iv, zf, zb, q, g, cg, bg, hv = jnp.split(h @ w_in_l, SPLITS, axis=-1)
    o_rec, s_f, s_b = hgrn2_mix(q, iv, zf, zb, g, lbf, lbb, s0f, s0b)
    o_conv = bg * conv_fn(cg * hv, conv_w_l)
    y = jnp.concatenate([o_rec, o_conv], axis=-1) @ w_out_l
    return y, s_f, s_b


def peer_ffn(h, wq, subkeys, u, v):
    bsz, t, d = h.shape
    qr = (h @ wq).astype(jnp.float32).reshape(bsz, t, PEER_HEADS, 2, PEER_HALF)
    scores = jnp.einsum('bthpd,hpkd->bthpk', qr, subkeys.astype(jnp.float32))
    s1, i1 = lax.top_k(scores[..., 0, :], PEER_TOPK)
    s2, i2 = lax.top_k(scores[..., 1, :], PEER_TOPK)
    n_cand = PEER_TOPK * PEER_TOPK
    cand = (s1[..., :, None] + s2[..., None, :]).reshape(bsz, t, PEER_HEADS, n_cand)
    cidx = (i1[..., :, None] * N_KEYS + i2[..., None, :]).reshape(bsz, t, PEER_HEADS, n_cand)
    top, pos = lax.top_k(cand, PEER_TOPK)
    eidx = jnp.take_along_axis(cidx, pos, axis=-1)
    gate = jax.nn.softmax(top, axis=-1).astype(h.dtype)
    n_blk = bsz * t // PEER_BLOCK
    n_sel = PEER_HEADS * PEER_TOPK
    hb = h.reshape(n_blk, PEER_BLOCK, d)
    eb = eidx.reshape(n_blk, PEER_BLOCK, n_sel)
    gb = gate.reshape(n_blk, PEER_BLOCK, n_sel)

    def block(args):
        hk, ek, gk = args
        act = jax.nn.gelu(jnp.einsum('ted,td->te', u[ek], hk), approximate=False)
        return jnp.einsum('te,ted->td', gk * act, v[ek])

    return lax.map(block, (hb, eb, gb)).reshape(bsz, t, d)


def setup_inputs(seed: int = 0) -> dict:
    key = jax.random.key(seed)
    ks = jax.random.split(key, 17)
    nrm = lambda k, shape, scale: jax.random.normal(k, shape, jnp.float32) * scale
    mix_w = HG_WIDTH + SC_WIDTH
    return {
        'x': nrm(ks[0], (BATCH, SEQ, D_MODEL), 1.0),
        'c': nrm(ks[1], (BATCH, D_MODEL), 1.0),
        'ctx': nrm(ks[2], (BATCH, CTX_LEN, D_MODEL), 1.0),
        'c_ctx': nrm(ks[3], (D_MODEL,), 1.0),
        'w_mod': nrm(ks[4], (DEPTH, D_MODEL, N_MOD * D_MODEL), 0.5 * D_MODEL ** -0.5),
        'b_mod': nrm(ks[5], (DEPTH, N_MOD * D_MODEL), 0.01),
        'norm1_g': 1.0 + nrm(ks[6], (DEPTH, D_MODEL), 0.1),
        'norm2_g': 1.0 + nrm(ks[7], (DEPTH, D_MODEL), 0.1),
        'w_in': nrm(ks[8], (DEPTH, D_MODEL, IN_COLS), D_MODEL ** -0.5),
        'conv_w': nrm(ks[9], (DEPTH, CONV_W, SC_WIDTH), CONV_W ** -0.5),
        'w_out': nrm(ks[10], (DEPTH, mix_w, D_MODEL), mix_w ** -0.5),
        'lb_logits': nrm(ks[11], (DEPTH, 2, HG_WIDTH), 0.5),
        'peer_wq': nrm(ks[12], (DEPTH, D_MODEL, PEER_HEADS * PEER_QDIM), D_MODEL ** -0.5),
        'peer_subkeys': nrm(ks[13], (DEPTH, PEER_HEADS, 2, N_KEYS, PEER_HALF), PEER_HALF ** -0.5),
        'peer_u': nrm(ks[14], (DEPTH, N_EXPERTS, D_MODEL), D_MODEL ** -0.5),
        'peer_v': nrm(ks[15], (DEPTH, N_EXPERTS, D_MODEL), PEER_HEADS ** -0.5),
        'final_g': 1.0 + nrm(ks[16], (D_MODEL,), 0.1),
    }


def reference(x, c, ctx, c_ctx, w_mod, b_mod, norm1_g, norm2_g, w_in, conv_w, w_out,
              lb_logits, peer_wq, peer_subkeys, peer_u, peer_v, final_g):
    rows = x.shape[1] // GRID_W
    p_lb = jax.nn.softmax(lb_logits.astype(jnp.float32), axis=0)
    lower = jnp.cumsum(p_lb, axis=0) - p_lb[0]
    cond_x = jax.nn.silu(c)
    cond_c = jax.nn.silu(c_ctx)
    latent_conv = lambda u, w: grid_conv(u, w, rows)
    zero_state = jnp.zeros((ctx.shape[0], HG_HEADS, HG_DIM, HG_DIM), jnp.float32)
    xc = ctx
    for l in range(DEPTH):
        lbf = lower[l, 0].reshape(HG_HEADS, HG_DIM)
        lbb = lower[l, 1].reshape(HG_HEADS, HG_DIM)
        mod_x = (cond_x @ w_mod[l] + b_mod[l])[:, None, :]
        mod_c = (cond_c @ w_mod[l] + b_mod[l])[None, None, :]
        sh1x, sc1x, g1x, sh2x, sc2x, g2x = jnp.split(mod_x, N_MOD, axis=-1)
        sh1c, sc1c, g1c, sh2c, sc2c, g2c = jnp.split(mod_c, N_MOD, axis=-1)
        hc = modulate(rms_norm(xc, norm1_g[l]), sh1c, sc1c)
        if l < DEPTH - 1:
            yc, s_f, s_b = token_mixer(hc, w_in[l], w_out[l], conv_w[l], lbf, lbb,
                                       zero_state, zero_state, seq_conv)
            xc = xc + g1c * yc
            hc2 = modulate(rms_norm(xc, norm2_g[l]), sh2c, sc2c)
            xc = xc + g2c * peer_ffn(hc2, peer_wq[l], peer_subkeys[l], peer_u[l], peer_v[l])
        else:
            iv_c, zf_c, zb_c = jnp.split(hc @ w_in[l][:, :3 * HG_WIDTH], 3, axis=-1)
            s_f, s_b = hgrn2_final_states(iv_c, zf_c, zb_c, lbf, lbb)
        hx = modulate(rms_norm(x, norm1_g[l]), sh1x, sc1x)
        yx, _, _ = token_mixer(hx, w_in[l], w_out[l], conv_w[l], lbf, lbb, s_f, s_b, latent_conv)
        x = x + g1x * yx
        hx2 = modulate(rms_norm(x, norm2_g[l]), sh2x, sc2x)
        x = x + g2x * peer_ffn(hx2, peer_wq[l], peer_subkeys[l], peer_u[l], peer_v[l])
    return rms_norm(x, final_g)
```

```python
import numpy as np
import ml_dtypes
from contextlib import ExitStack
import concourse.bass as bass
import concourse.mybir as mybir
from concourse.bass_utils import run_bass_kernel_spmd

F32 = mybir.dt.float32
BF16 = mybir.dt.bfloat16
AF = mybir.ActivationFunctionType
ALU = mybir.AluOpType
AX = mybir.AxisListType

D = 1024
KC = 8
NT = 256
NP = 256
CH = 32
EPS = 1e-6
EPOCH = 30000
NDMA = 12
NEXP = 16384


class Tok:
    __slots__ = ("sem", "val", "eng")

    def __init__(self, sem, val, eng):
        self.sem = sem; self.val = val; self.eng = eng


class Buf:
    def __init__(self, name=""):
        self.name = name; self.w = None; self.r = {}


class Eng:
    def __init__(self, name, obj):
        self.name = name; self.obj = obj
        self.sems = []; self.count = 0; self.seen = {}
        self.dma_sems = []; self.dma_n = 0


class FW:
    def __init__(self, nc, stack):
        self.nc = nc; self.stack = stack
        self.E = {n: Eng(n, getattr(nc, o)) for n, o in
                  [("pe", "tensor"), ("dve", "vector"), ("act", "scalar"), ("pool", "gpsimd"), ("sp", "sync")]}
        self.ninst = 0

    def newsem(self, name):
        return self.stack.enter_context(self.nc.semaphore(name))

    def _wait(self, e, tok):
        if tok is None:
            return
        k = id(tok.sem)
        if e.seen.get(k, 0) >= tok.val:
            return
        e.obj.wait_ge(tok.sem, tok.val)
        e.seen[k] = tok.val

    def _deps(self, e, reads, writes):
        for b in reads:
            if b.w is not None:
                self._wait(e, b.w)
        for b in writes:
            if b.w is not None and b.w.eng != e.name:
                self._wait(e, b.w)
            for en, t in b.r.items():
                if en != e.name:
                    self._wait(e, t)

    def _commit(self, tok, reads, writes, rkey):
        for b in reads:
            b.r[rkey] = tok
        for b in writes:
            b.w = tok; b.r = {}

    def op(self, eng, fn, reads=(), writes=()):
        e = self.E[eng]
        self._deps(e, reads, writes)
        ep = e.count // EPOCH
        while len(e.sems) <= ep:
            e.sems.append(self.newsem(f"s_{eng}_{len(e.sems)}"))
        ins = fn(e.obj)
        val = e.count - ep * EPOCH + 1
        ins.then_inc(e.sems[ep], 1)
        e.count += 1
        self.ninst += 1
        tok = Tok(e.sems[ep], val, eng)
        self._commit(tok, reads, writes, eng)
        return tok

    def dma(self, eng, out, in_, reads=(), writes=(), **kw):
        e = self.E[eng]
        self._deps(e, reads, writes)
        j = e.dma_n % NDMA
        if len(e.dma_sems) <= j:
            e.dma_sems.append([self.newsem(f"d_{eng}_{j}"), 0])
        slot = e.dma_sems[j]
        if slot[1] > 0:
            self._wait(e, Tok(slot[0], slot[1], "dma"))
        slot[1] += 16
        e.obj.dma_start(out=out, in_=in_, **kw).then_inc(slot[0], 16)
        e.dma_n += 1
        self.ninst += 1
        tok = Tok(slot[0], slot[1], "dma")
        self._commit(tok, reads, writes, f"dma_{eng}_{j}")
        return tok

    def barrier(self):
        toks = []
        for en in self.E.values():
            if en.count > 0:
                ep = (en.count - 1) // EPOCH
                toks.append(Tok(en.sems[ep], en.count - ep * EPOCH, en.name))
            for slot in en.dma_sems:
                if slot[1] > 0:
                    toks.append(Tok(slot[0], slot[1], "dma"))
        for e in self.E.values():
            for t in toks:
                if t.eng != e.name:
                    self._wait(e, t)

    def finish(self):
        e = self.E["sp"]
        for en in self.E.values():
            for slot in en.dma_sems:
                if slot[1] > 0:
                    self._wait(e, Tok(slot[0], slot[1], "dma"))


class Tile:
    def __init__(self, t, name):
        self.t = t; self.b = Buf(name)

    def __getitem__(self, k):
        return self.t[k]


def build(depth, seq, ctxlen, stage=99, dbg=False):
    nc = bass.Bass("TRN2", target_bir_lowering=False)
    assert seq % NT == 0 and ctxlen <= NT and ctxlen % 128 == 0 and seq % NP == 0
    rows_per_tile = NT // 64

    def din(name, shape, dt=F32):
        return nc.dram_tensor(name, list(shape), dt, kind="ExternalInput").ap()

    def dscr(name, shape, dt=F32):
        return nc.dram_tensor(name, list(shape), dt).ap()

    xT_in = din("xT", [D, seq]); cxT_in = din("cxT", [D, ctxlen])
    cv_in = din("cv", [128, KC, 2])
    wmod_in = din("w_mod", [depth, D, 6 * D]); bmod_in = din("b_mod", [depth, 128, 48])
    n1_in = din("n1", [depth, 128, KC]); n2_in = din("n2", [depth, 128, KC]); fg_in = din("fg", [128, KC, 2])
    win_in = din("w_in", [depth, D, 4096]); wout_in = din("w_out", [depth, D, D])
    cw_in = din("cw", [depth, 128, 4, 3]); lbl_in = din("lbl", [128, depth, 2, 4])
    wq_in = din("wq", [depth, D, 2048]); skT_in = din("skT", [depth, 128, 16, 128])
    uT_in = din("uT", [depth, D, NEXP]); v_in = din("v", [depth, NEXP, D])
    ident_in = din("ident", [128, 128], BF16); ones_in = din("ones", [128, 128], BF16)
    maskF_in = din("maskF", [128, 128], BF16); maskB_in = din("maskB", [128, 128], BF16)
    identF_in = din("identF", [128, 128]); reset_in = din("resetm", [128, NT]); rowm_in = din("rowm", [128, 4])
    yT_out = nc.dram_tensor("yT", [D, seq], F32, kind="ExternalOutput").ap()
    dbg_out = nc.dram_tensor("dbg", [D, seq], F32, kind="ExternalOutput").ap() if dbg else None
    dbgc_out = nc.dram_tensor("dbgc", [D, ctxlen], F32, kind="ExternalOutput").ap() if dbg else None

    TM = max(seq, ctxlen)
    XR = dscr("XR", [D, seq]); CR = dscr("CR", [D, ctxlen])
    ZB = dscr("ZB", [512, TM]); QS = dscr("QS", [512, TM]); SG = dscr("SG", [512, TM])
    OF = dscr("OF", [512, TM]); US = dscr("US", [512, TM]); BG = dscr("BG", [512, TM])
    VS = dscr("VS", [TM, 512], BF16)
    EB = 256
    NEB = NEXP // EB
    U16 = dscr("U16", [NEB, 128, KC, EB], BF16); V16 = dscr("V16", [NEXP, D], BF16)
    U16b = Buf("U16"); V16b = Buf("V16")

    with ExitStack() as st:
        fw = FW(nc, st)

        def sb(name, shape, dt=F32):
            return Tile(st.enter_context(nc.sbuf_tensor("sb_" + name, list(shape), dt)), name)

        def dbuf(name):
            return Buf(name)

        psF = [Tile(st.enter_context(nc.psum_tensor(f"psF{i}", [128, 512], F32)), f"psF{i}") for i in range(6)]
        psB = [Tile(st.enter_context(nc.psum_tensor(f"psB{i}", [128, 1024], BF16)), f"psB{i}") for i in range(2)]
        cnt = {"f": 0, "b": 0, "n": 6}

        def nps():
            cnt["f"] += 1
            return psF[cnt["f"] % cnt["n"]]

        def npsb():
            cnt["b"] += 1
            return psB[cnt["b"] % 2]

        def OP(eng, fn, reads=(), writes=()):
            return fw.op(eng, fn, reads=[x.b if isinstance(x, Tile) else x for x in reads],
                         writes=[x.b if isinstance(x, Tile) else x for x in writes])

        def DMA(eng, out, in_, reads=(), writes=(), **kw):
            return fw.dma(eng, out, in_, reads=[x.b if isinstance(x, Tile) else x for x in reads],
                          writes=[x.b if isinstance(x, Tile) else x for x in writes], **kw)

        ident = sb("ident", [128, 128], BF16); ones = sb("ones", [128, 128], BF16)
        maskF = sb("maskF", [128, 128], BF16); maskB = sb("maskB", [128, 128], BF16)
        resetm = sb("resetm", [128, NT]); rowm = sb("rowm", [128, 4])
        DMA("sp", rowm[:], rowm_in[:, :], writes=[rowm])
        for tl, src in [(ident, ident_in), (ones, ones_in), (maskF, maskF_in), (maskB, maskB_in), (resetm, reset_in)]:
            DMA("sp", tl[:], src[:, :], writes=[tl])
        cond = sb("cond", [128, KC, 2])
        DMA("sp", cond[:], cv_in[:, :, :], writes=[cond])
        OP("act", lambda e: e.activation(out=cond[:], in_=cond[:], func=AF.Silu), reads=[cond], writes=[cond])
        epsc = sb("epsc", [128, 1])
        OP("dve", lambda e: e.memset(epsc[:], EPS), writes=[epsc])
        lbe = sb("lbe", [128, depth, 8]); lbs = sb("lbs", [128, 8]); lb = sb("lb", [128, depth, 8]); oml = sb("oml", [128, depth, 8])
        DMA("sp", lbe[:], lbl_in.rearrange("p l a b -> p l (a b)"), writes=[lbe])
        OP("act", lambda e: e.activation(out=lbe[:], in_=lbe[:], func=AF.Exp), reads=[lbe], writes=[lbe])
        OP("dve", lambda e: e.tensor_copy(out=lbs[:], in_=lbe[:, 0, :]), reads=[lbe], writes=[lbs])
        for l in range(1, depth):
            OP("dve", lambda e, l=l: e.tensor_tensor(out=lbs[:], in0=lbs[:], in1=lbe[:, l, :], op=ALU.add), reads=[lbs, lbe], writes=[lbs])
        OP("dve", lambda e: e.reciprocal(out=lbs[:], in_=lbs[:]), reads=[lbs], writes=[lbs])
        OP("dve", lambda e: e.memset(lb[:, 0, :], 0.0), writes=[lb])
        for l in range(1, depth):
            OP("dve", lambda e, l=l: e.tensor_tensor(out=lbe[:, l, :], in0=lbe[:, l, :], in1=lbs[:], op=ALU.mult), reads=[lbe, lbs], writes=[lbe])
            OP("dve", lambda e, l=l: e.tensor_tensor(out=lb[:, l, :], in0=lb[:, l - 1, :], in1=lbe[:, l, :], op=ALU.add), reads=[lb, lbe], writes=[lb])
        OP("dve", lambda e: e.tensor_scalar(out=oml[:], in0=lb[:], scalar1=-1.0, scalar2=1.0, op0=ALU.mult, op1=ALU.add), reads=[lb], writes=[oml])

        XRb = [dbuf(f"XR{i}") for i in range(seq // NT)]
        CRb = [dbuf("CR")]
        for i in range(seq // NT):
            DMA("sp", XR[:, i * NT:(i + 1) * NT], xT_in[:, i * NT:(i + 1) * NT], writes=[XRb[i]])
        DMA("sp", CR[:, :], cxT_in[:, :], writes=[CRb[0]])

        modv = sb("modv", [128, 48, 2])
        A1 = sb("A1", [128, KC, 2]); A2 = sb("A2", [128, KC, 2])
        n1t = sb("n1t", [128, KC]); n2t = sb("n2t", [128, KC]); bmt = sb("bmt", [128, 48]); cwt = sb("cwt", [128, 4, 3])
        fgt = sb("fgt", [128, KC, 2]); zerob = sb("zerob", [128, KC, 2])
        DMA("sp", fgt[:], fg_in[:, :, :], writes=[fgt])
        OP("dve", lambda e: e.memset(zerob[:], 0.0), writes=[zerob])
        identF = sb("identF", [128, 128])
        DMA("sp", identF[:], identF_in[:, :], writes=[identF])
        Sst = sb("Sst", [128, 8, 128])
        xtp = [sb(f"xt{i}", [128, KC, NT]) for i in range(2)]; xt = xtp[0]
        tmp8 = sb("tmp8", [128, KC, NT]); sq8 = sb("sq8", [128, KC, NT], BF16)
        hTp = [sb(f"hT{i}", [128, KC, NT], BF16) for i in range(2)]; hT = hTp[0]
        rstd = sb("rstd", [128, NT])

        streams = {
            "c": dict(T=ctxlen, R=CR, Rb=CRb, s=1, nt=1, n=ctxlen),
            "x": dict(T=seq, R=XR, Rb=XRb, s=0, nt=seq // NT, n=NT),
        }
        scr_b = {nm: [dbuf(f"{nm}{i}") for i in range(TM // min(NT, ctxlen) + 1)] for nm in ["ZB", "QS", "SG", "OF", "US", "BG", "VS"]}

        def rsview(R, t0, n):
            return R.rearrange("(k p) t -> p k t", p=128)[:, :, t0:t0 + n]

        def s4view(S, t0, n):
            return S.rearrange("(k p) t -> p k t", p=128)[:, :, t0:t0 + n]

        def norm_mod(n, A, Bt, Bm_col0, s, out_bf, xin=None):
            xin = xin if xin is not None else xt
            OP("act", lambda e: e.activation(out=sq8[:, :, :n], in_=xin[:, :, :n], func=AF.Square), reads=[xin], writes=[sq8])
            ps = nps()
            for k in range(KC):
                OP("pe", lambda e, k=k: e.matmul(ps[:, :n], lhsT=ones[:], rhs=sq8[:, k, :n], start=(k == 0), stop=(k == KC - 1)),
                   reads=[ones, sq8], writes=[ps])
            OP("act", lambda e: e.activation(out=rstd[:, :n], in_=ps[:, :n], func=AF.Sqrt, scale=1.0 / D, bias=epsc[:]), reads=[ps, epsc], writes=[rstd])
            OP("dve", lambda e: e.reciprocal(out=rstd[:, :n], in_=rstd[:, :n]), reads=[rstd], writes=[rstd])
            OP("dve", lambda e: e.tensor_tensor(out=tmp8[:, :, :n], in0=xin[:, :, :n],
                                                 in1=rstd[:, :n].unsqueeze(1).to_broadcast([128, KC, n]), op=ALU.mult),
               reads=[xin, rstd], writes=[tmp8])
            for k in range(KC):
                OP("act", lambda e, k=k: e.activation(out=out_bf[:, k, :n], in_=tmp8[:, k, :n], func=AF.Identity,
                                                       scale=A[:, k, s:s + 1], bias=Bt[:, Bm_col0 + k, s:s + 1]),
                   reads=[tmp8, A, Bt], writes=[out_bf])

        def gate_math(zps, qsrc_ap, qsrc_t, l, d, h, n, bwd):
            nchk = n // CH
            sg, f, key, lf, b, bc, e1, e2, e3, e4 = (W[x] for x in ["sg", "f", "key", "lf", "b", "bc", "e1", "e2", "e3", "e4"])
            col = d * 4 + h
            OP("act", lambda e: e.activation(out=sg[:, :n], in_=zps, func=AF.Sigmoid), reads=[zps_t[0]], writes=[sg])
            OP("dve", lambda e: e.tensor_scalar(out=f[:, :n], in0=sg[:, :n], scalar1=oml[:, l, col:col + 1], scalar2=lb[:, l, col:col + 1],
                                                 op0=ALU.mult, op1=ALU.add), reads=[sg, oml, lb], writes=[f])
            OP("pool", lambda e: e.tensor_scalar(out=f[:, :n], in0=f[:, :n], scalar1=1e-20, scalar2=None, op0=ALU.max), reads=[f], writes=[f])
            OP("pool", lambda e: e.tensor_scalar(out=key[:, :n], in0=f[:, :n], scalar1=-1.0, scalar2=1.0, op0=ALU.mult, op1=ALU.add),
               reads=[f], writes=[key])
            OP("act", lambda e: e.activation(out=lf[:, :n], in_=f[:, :n], func=AF.Ln), reads=[f], writes=[lf])
            OP("dve", lambda e: e.tensor_tensor_scan(out=b[:, :n], data0=resetm[:, :n], data1=lf[:, :n], initial=0.0, op0=ALU.mult, op1=ALU.add),
               reads=[resetm, lf], writes=[b])
            b3 = b[:, :n].rearrange("p (c t) -> p c t", t=CH)
            if bwd:
                lf3 = lf[:, :n].rearrange("p (c t) -> p c t", t=CH)
                bc3 = bc[:, :n].rearrange("p (c t) -> p c t", t=CH)
                OP("dve", lambda e: e.tensor_tensor(out=bc3, in0=b3[:, :, CH - 1:CH].to_broadcast([128, nchk, CH]), in1=b3, op=ALU.subtract),
                   reads=[b], writes=[bc])
                OP("dve", lambda e: e.tensor_tensor(out=b[:, :n], in0=bc[:, :n], in1=lf[:, :n], op=ALU.add), reads=[bc, lf], writes=[b])
                mid = CH // 2; last = 0
            else:
                mid = CH // 2 - 1; last = CH - 1
            bc3 = bc[:, :n].rearrange("p (c t) -> p c t", t=CH)
            OP("dve", lambda e: e.tensor_tensor(out=bc3, in0=b3, in1=b3[:, :, mid:mid + 1].to_broadcast([128, nchk, CH]), op=ALU.subtract),
               reads=[b], writes=[bc])
            OP("act", lambda e: e.activation(out=e1[:, :n], in_=bc[:, :n], func=AF.Exp), reads=[bc], writes=[e1])
            OP("act", lambda e: e.activation(out=e2[:, :n], in_=bc[:, :n], func=AF.Exp, scale=-1.0), reads=[bc], writes=[e2])
            OP("act", lambda e: e.activation(out=e3[:, :n], in_=b[:, :n], func=AF.Exp), reads=[b], writes=[e3])
            OP("dve", lambda e: e.tensor_tensor(out=bc3, in0=b3[:, :, last:last + 1].to_broadcast([128, nchk, CH]), in1=b3, op=ALU.subtract),
               reads=[b, e1, e2], writes=[bc])
            OP("act", lambda e: e.activation(out=e4[:, :n], in_=bc[:, :n], func=AF.Exp), reads=[bc], writes=[e4])
            OP("dve", lambda e: e.tensor_tensor(out=qp[:, :n], in0=qsrc_ap, in1=e1[:, :n], op=ALU.mult), reads=[qsrc_t, e1], writes=[qp])
            OP("pool", lambda e: e.tensor_tensor(out=qin[:, :n], in0=qsrc_ap, in1=e3[:, :n], op=ALU.mult), reads=[qsrc_t, e3], writes=[qin])
            OP("dve", lambda e: e.tensor_tensor(out=kp[:, :n], in0=key[:, :n], in1=e2[:, :n], op=ALU.mult), reads=[key, e2], writes=[kp])
            OP("pool", lambda e: e.tensor_tensor(out=kout[:, :n], in0=key[:, :n], in1=e4[:, :n], op=ALU.mult), reads=[key, e4], writes=[kout])
            return last

        zps_t = [None]

        def recur(l, d, h, n, bwd, last, o_dst):
            si = d * 4 + h
            e3 = W["e3"]
            nsub = n // 128
            subs = range(nsub - 1, -1, -1) if bwd else range(nsub)
            mask = maskB if bwd else maskF
            for j in subs:
                js = slice(j * 128, (j + 1) * 128)
                pt = npsb()
                OP("pe", lambda e: e.transpose(out=pt[:, 0:128], in_=kout[:, js], identity=ident[:]), reads=[kout, ident], writes=[pt])
                for c4 in range(4):
                    OP("act", lambda e, c4=c4: e.activation(out=kotm[:, c4, :], in_=pt[:, 0:128], func=AF.Identity, scale=rowm[:, c4:c4 + 1]),
                       reads=[pt, rowm], writes=[kotm])
                pa = nps()
                OP("pe", lambda e: e.matmul(pa[:, 0:128], lhsT=kp[:, js], rhs=qp[:, js], start=True, stop=True), reads=[kp, qp], writes=[pa])
                OP("dve", lambda e: e.tensor_tensor(out=attm[:], in0=pa[:, 0:128], in1=mask[:], op=ALU.mult), reads=[pa, mask], writes=[attm])
                po = nps()
                chunks = range(3, -1, -1) if bwd else range(4)
                for ci, c in enumerate(chunks):
                    cs = slice(c * CH, (c + 1) * CH)
                    tcs = slice(j * 128 + c * CH, j * 128 + (c + 1) * CH)
                    sbf = Sbf[(si * 4 + ci) % 8]
                    OP("act", lambda e, sbf=sbf: e.copy(out=sbf[:], in_=Sst[:, si, :]), reads=[Sst], writes=[sbf])
                    OP("pe", lambda e, sbf=sbf, cs=cs, tcs=tcs: e.matmul(po[:, cs], lhsT=sbf[:], rhs=qin[:, tcs], start=True, stop=False),
                       reads=[sbf, qin], writes=[po])
                    OP("pe", lambda e, cs=cs: e.matmul(po[:, cs], lhsT=vtm[:, j, h * 128:(h + 1) * 128], rhs=attm[:, cs], start=False, stop=True),
                       reads=[vtm, attm], writes=[po])
                    pS = nps()
                    OP("pe", lambda e, c=c, pS=pS: e.matmul(pS[:, 0:128], lhsT=kotm[:, c, :], rhs=vtm[:, j, h * 128:(h + 1) * 128], start=True, stop=True),
                       reads=[kotm, vtm], writes=[pS])
                    dcol = j * 128 + c * CH + last
                    OP("dve", lambda e, pS=pS, dcol=dcol: e.scalar_tensor_tensor(out=Sst[:, si, :], in0=Sst[:, si, :], scalar=e3[:, dcol:dcol + 1],
                                                                                 in1=pS[:, 0:128], op0=ALU.mult, op1=ALU.add),
                       reads=[Sst, e3, pS], writes=[Sst])
                OP("act", lambda e: e.copy(out=o_dst[:, h, js], in_=po[:, 0:128]), reads=[po], writes=[o_dst])


        def peer_layer(l, last_layer):
            pst = ExitStack()

            def sbp(name, shape, dt=F32):
                return Tile(pst.enter_context(nc.sbuf_tensor(f"p{l}_" + name, list(shape), dt)), name)
            wqb = sbp("wqb", [128, KC, 2048], BF16); skb = sbp("skb", [128, 16, 128], BF16)
            for k in range(KC):
                DMA("pool", wqb[:, k, :], wq_in[l, k * 128:(k + 1) * 128, :], writes=[wqb])
            DMA("pool", skb[:], skT_in[l], writes=[skb])
            uTl = uT_in[l].rearrange("(k p) e -> p k e", p=128)
            for eb in range(NEB):
                DMA("pool", U16[eb], uTl[:, :, eb * EB:(eb + 1) * EB], writes=[U16b])
            for r in range(16):
                DMA("pool", V16[r * 1024:(r + 1) * 1024, :], v_in[l, r * 1024:(r + 1) * 1024, :], writes=[V16b])
            V16v = V16.rearrange("(b q p) d -> b p q d", q=2, p=128)
            qT = sbp("qT", [128, 16, 128], BF16); sc = sbp("sc", [128, 16, 128]); scw = sbp("scw", [128, 128])
            top = sbp("top", [128, 16, 16]); cand = sbp("cand", [128, 8, 256]); cw2 = sbp("cw2", [128, 256]); c24 = sbp("c24", [128, 8, 24])
            tau = sbp("tau", [128, 8]); d16 = sbp("d16", [128, 8, 16]); Zs = sbp("Zs", [128, 8]); beta = sbp("beta", [128, 8])
            sA = sbp("sA", [128, 8, 128])
            Sblk = [sbp(f"Sblk{i}", [128, 8, 128]) for i in range(2)]; Exb = [sbp(f"Exb{i}", [128, 8, 128]) for i in range(2)]
            tmpg = [sbp(f"tmpg{i}", [128, 1024], BF16) for i in range(2)]
            Gq = [sbp(f"Gq{i}", [128, 4096], BF16) for i in range(4)]
            ub = [sbp(f"ub{i}", [128, KC, EB], BF16) for i in range(2)]; vb = [sbp(f"vb{i}", [128, 2, D], BF16) for i in range(2)]
            ga = [sbp(f"ga{i}", [128, EB]) for i in range(2)]; wt = [sbp(f"wt{i}", [128, EB], BF16) for i in range(2)]
            wT = [sbp(f"wT{i}", [128, 2, 128], BF16) for i in range(2)]
            otm = sbp("otm", [128, D]); tmpo = sbp("tmpo", [128, KC, 128])
            cnt["n"] = 4
            po = [psF[4], psF[5]]
            ti = 0
            ybuf = Buf("yout")
            for sname in (["x"] if last_layer else ["c", "x"]):
                S_ = streams[sname]
                s = S_["s"]; R = S_["R"]; Rb = S_["Rb"]; T = S_["T"]; nreg = S_["n"]
                for i in range(T // 128):
                    t0 = i * 128
                    rb = Rb[t0 // nreg]
                    xt_ = xtp[ti % 2]; hT_ = hTp[ti % 2]; ti += 1
                    DMA("sp", xt_[:, :, :128], rsview(R, t0, 128), reads=[rb], writes=[xt_])
                    norm_mod(128, A2, modv, 24, s, hT_, xin=xt_)
                    for jc in range(16):
                        ps = nps()
                        for k in range(KC):
                            OP("pe", lambda e, k=k, jc=jc, ps=ps: e.matmul(ps[:, :128], lhsT=wqb[:, k, jc * 128:(jc + 1) * 128], rhs=hT_[:, k, :128],
                                                                           start=(k == 0), stop=(k == KC - 1)), reads=[wqb, hT_], writes=[ps])
                        OP("act", lambda e, jc=jc, ps=ps: e.copy(out=qT[:, jc, :], in_=ps[:, :128]), reads=[ps], writes=[qT])
                    for g in range(4):
                        ps = nps()
                        for jj in range(4):
                            jc = g * 4 + jj
                            OP("pe", lambda e, jc=jc, jj=jj, ps=ps: e.matmul(ps[:, jj * 128:(jj + 1) * 128], lhsT=qT[:, jc, :], rhs=skb[:, jc, :], start=True, stop=True),
                               reads=[qT, skb], writes=[ps])
                        OP("act", lambda e, g=g, ps=ps: e.copy(out=sc[:, g * 4:(g + 1) * 4, :], in_=ps[:, :].rearrange("p (a b) -> p a b", b=128)), reads=[ps], writes=[sc])
                    for jc in range(16):
                        OP("dve", lambda e, jc=jc: e.max(out=top[:, jc, 0:8], in_=sc[:, jc, :]), reads=[sc], writes=[top])
                        OP("dve", lambda e, jc=jc: e.match_replace(out=scw[:], in_to_replace=top[:, jc, 0:8], in_values=sc[:, jc, :], imm_value=-1e30),
                           reads=[top, sc], writes=[scw])
                        OP("dve", lambda e, jc=jc: e.max(out=top[:, jc, 8:16], in_=scw[:]), reads=[scw], writes=[top])
                    top4 = top[:].rearrange("p (h two) r -> p h two r", two=2)
                    cand4 = cand[:].rearrange("p h (r s) -> p h r s", s=16)
                    OP("dve", lambda e: e.tensor_tensor(out=cand4, in0=top4[:, :, 0, :].unsqueeze(3).to_broadcast([128, 8, 16, 16]),
                                                         in1=top4[:, :, 1, :].unsqueeze(2).to_broadcast([128, 8, 16, 16]), op=ALU.add), reads=[top], writes=[cand])
                    for h in range(8):
                        OP("dve", lambda e, h=h: e.max(out=c24[:, h, 0:8], in_=cand[:, h, :]), reads=[cand], writes=[c24])
                        OP("dve", lambda e, h=h: e.match_replace(out=cw2[:], in_to_replace=c24[:, h, 0:8], in_values=cand[:, h, :], imm_value=-1e30),
                           reads=[c24, cand], writes=[cw2])
                        OP("dve", lambda e, h=h: e.max(out=c24[:, h, 8:16], in_=cw2[:]), reads=[cw2], writes=[c24])
                        OP("dve", lambda e, h=h: e.match_replace(out=cw2[:], in_to_replace=c24[:, h, 8:16], in_values=cw2[:], imm_value=-1e30),
                           reads=[c24, cw2], writes=[cw2])
                        OP("dve", lambda e, h=h: e.max(out=c24[:, h, 16:24], in_=cw2[:]), reads=[cw2], writes=[c24])
                    OP("dve", lambda e: e.tensor_tensor(out=tau[:], in0=c24[:, :, 15], in1=c24[:, :, 16], op=ALU.add), reads=[c24], writes=[tau])
                    OP("dve", lambda e: e.tensor_scalar(out=tau[:], in0=tau[:], scalar1=0.5, scalar2=None, op0=ALU.mult), reads=[tau], writes=[tau])
                    OP("dve", lambda e: e.tensor_tensor(out=d16[:], in0=c24[:, :, 0:16], in1=c24[:, :, 0:1].to_broadcast([128, 8, 16]), op=ALU.subtract),
                       reads=[c24], writes=[d16])
                    OP("act", lambda e: e.activation(out=d16[:], in_=d16[:], func=AF.Exp), reads=[d16], writes=[d16])
                    OP("dve", lambda e: e.tensor_reduce(out=Zs[:], in_=d16[:], axis=AX.X, op=ALU.add), reads=[d16], writes=[Zs])
                    OP("act", lambda e: e.activation(out=Zs[:], in_=Zs[:], func=AF.Ln), reads=[Zs], writes=[Zs])
                    OP("dve", lambda e: e.tensor_tensor(out=beta[:], in0=tau[:], in1=c24[:, :, 0], op=ALU.subtract), reads=[tau, c24], writes=[beta])
                    OP("dve", lambda e: e.tensor_tensor(out=beta[:], in0=beta[:], in1=Zs[:], op=ALU.subtract), reads=[beta, Zs], writes=[beta])
                    sc4 = sc[:].rearrange("p (h two) k -> p h two k", two=2)
                    OP("dve", lambda e: e.tensor_tensor(out=sA[:], in0=sc4[:, :, 0, :], in1=tau[:].unsqueeze(2).to_broadcast([128, 8, 128]), op=ALU.subtract),
                       reads=[sc, tau], writes=[sA])
                    it = 0
                    for qtr in range(4):
                        for h in range(8):
                            for ab in range(4):
                                a0 = qtr * 32 + ab * 8
                                sbk = Sblk[it % 2]; exk = Exb[it % 2]; tg = tmpg[it % 2]; it += 1
                                OP("dve", lambda e, h=h, a0=a0, sbk=sbk: e.tensor_tensor(out=sbk[:], in0=sA[:, h, a0:a0 + 8].unsqueeze(2).to_broadcast([128, 8, 128]),
                                                                                         in1=sc4[:, h, 1, :].unsqueeze(1).to_broadcast([128, 8, 128]), op=ALU.add),
                                   reads=[sA, sc], writes=[sbk])
                                OP("act", lambda e, h=h, sbk=sbk, exk=exk: e.activation(out=exk[:], in_=sbk[:], func=AF.Exp, bias=beta[:, h:h + 1]),
                                   reads=[sbk, beta], writes=[exk])
                                gsl = Gq[qtr][:, ab * 1024:(ab + 1) * 1024]
                                sb2 = sbk[:].rearrange("p a b -> p (a b)"); ex2 = exk[:].rearrange("p a b -> p (a b)")
                                if h == 0:
                                    OP("dve", lambda e, gsl=gsl, sb2=sb2, ex2=ex2: e.scalar_tensor_tensor(out=gsl, in0=sb2, scalar=0.0, in1=ex2, op0=ALU.is_ge, op1=ALU.mult),
                                       reads=[sbk, exk], writes=[Gq[qtr]])
                                else:
                                    OP("dve", lambda e, tg=tg, sb2=sb2, ex2=ex2: e.scalar_tensor_tensor(out=tg[:], in0=sb2, scalar=0.0, in1=ex2, op0=ALU.is_ge, op1=ALU.mult),
                                       reads=[sbk, exk], writes=[tg])
                                    OP("pool", lambda e, tg=tg, gsl=gsl: e.tensor_tensor(out=gsl, in0=gsl, in1=tg[:], op=ALU.add), reads=[tg, Gq[qtr]], writes=[Gq[qtr]])
                    for eb in range(NEB):
                        ub_ = ub[eb % 2]; vb_ = vb[eb % 2]; ga_ = ga[eb % 2]; wt_ = wt[eb % 2]; wT_ = wT[eb % 2]
                        DMA("sp", ub_[:], U16[eb], reads=[U16b], writes=[ub_])
                        DMA("sp", vb_[:], V16v[eb], reads=[V16b], writes=[vb_])
                        pa = nps()
                        for k in range(KC):
                            OP("pe", lambda e, k=k, pa=pa, ub_=ub_: e.matmul(pa[:, :EB], lhsT=hT_[:, k, :128], rhs=ub_[:, k, :], start=(k == 0), stop=(k == KC - 1)),
                               reads=[hT_, ub_], writes=[pa])
                        OP("act", lambda e, pa=pa, ga_=ga_: e.activation(out=ga_[:], in_=pa[:, :EB], func=AF.Gelu), reads=[pa], writes=[ga_])
                        gq = Gq[eb // 16]
                        OP("dve", lambda e, ga_=ga_, wt_=wt_, gq=gq, eb=eb: e.tensor_tensor(out=wt_[:], in0=ga_[:], in1=gq[:, (eb % 16) * EB:(eb % 16 + 1) * EB], op=ALU.mult),
                           reads=[ga_, gq], writes=[wt_])
                        pt = npsb()
                        for q in range(2):
                            OP("pe", lambda e, q=q, pt=pt, wt_=wt_: e.transpose(out=pt[:, q * 128:(q + 1) * 128], in_=wt_[:, q * 128:(q + 1) * 128], identity=ident[:]),
                               reads=[wt_, ident], writes=[pt])
                        OP("act", lambda e, pt=pt, wT_=wT_: e.copy(out=wT_[:].rearrange("p a b -> p (a b)"), in_=pt[:, 0:256]), reads=[pt], writes=[wT_])
                        for q in range(2):
                            for hf in range(2):
                                OP("pe", lambda e, q=q, hf=hf, wT_=wT_, vb_=vb_, eb=eb: e.matmul(po[hf][:, :], lhsT=wT_[:, q, :], rhs=vb_[:, q, hf * 512:(hf + 1) * 512],
                                                                                              start=(eb == 0 and q == 0), stop=(eb == NEB - 1 and q == 1)),
                                   reads=[wT_, vb_], writes=[po[hf]])
                    for hf in range(2):
                        OP("act", lambda e, hf=hf: e.copy(out=otm[:, hf * 512:(hf + 1) * 512], in_=po[hf][:, :]), reads=[po[hf]], writes=[otm])
                    for m in range(KC):
                        pt = nps()
                        OP("pe", lambda e, m=m, pt=pt: e.transpose(out=pt[:, :128], in_=otm[:, m * 128:(m + 1) * 128], identity=identF[:]), reads=[otm, identF], writes=[pt])
                        OP("dve", lambda e, m=m, pt=pt: e.scalar_tensor_tensor(out=xt_[:, m, :128], in0=pt[:, :128], scalar=modv[:, 40 + m, s:s + 1], in1=xt_[:, m, :128],
                                                                               op0=ALU.mult, op1=ALU.add), reads=[pt, modv, xt_], writes=[xt_])
                    if last_layer and stage == 99:
                        norm_mod(128, fgt, zerob, 0, 0, tmpo, xin=xt_)
                        DMA("sp", rsview(yT_out, t0, 128), tmpo[:], reads=[tmpo], writes=[ybuf])
                    else:
                        DMA("sp", rsview(R, t0, 128), xt_[:, :, :128], reads=[xt_], writes=[rb])
            cnt["n"] = 6
            fw.barrier()
            pst.close()

        for l in range(depth):
            last_layer = (l == depth - 1)
            mst = ExitStack()

            def sbm(name, shape, dt=F32):
                return Tile(mst.enter_context(nc.sbuf_tensor(f"m{l}_" + name, list(shape), dt)), name)
            winb = sbm("winb", [128, KC, 4096], BF16)
            woutb = sbm("woutb", [128, KC, D], BF16)
            wmb = [sbm(f"wmb{i}", [128, KC, 256]) for i in range(2)]
            Sbf = [sbm(f"Sbf{i}", [128, 128], BF16) for i in range(8)]
            W = {n_: sbm("w_" + n_, [128, NT]) for n_ in ["sg", "f", "key", "lf", "b", "bc", "e1", "e2", "e3", "e4"]}
            qp = sbm("qp", [128, NT], BF16); qin = sbm("qin", [128, NT], BF16); kp = sbm("kp", [128, NT], BF16); kout = sbm("kout", [128, NT], BF16)
            vtm = sbm("vtm", [128, NT // 128, 512], BF16); kotm = sbm("kotm", [128, 4, 128], BF16)
            attm = sbm("attm", [128, 128], BF16)
            osb = sbm("osb", [128, 4, NT]); ofl = sbm("ofl", [128, 4, NT]); sgl = sbm("sgl", [128, 4, NT])
            stg = sbm("stg", [128, 4, NT])
            mix = sbm("mix", [128, KC, NT], BF16)
            uh = sbm("uh", [128, 4, NT + 128]); bgl = sbm("bgl", [128, 4, NT]); cva = sbm("cva", [128, NT])
            zbl = sbm("zbl", [128, 4, NT]); ql = sbm("ql", [128, 4, NT])
            xt = xtp[0]; hT = hTp[0]
            DMA("sp", n1t[:], n1_in[l], writes=[n1t]); DMA("sp", n2t[:], n2_in[l], writes=[n2t])
            DMA("sp", bmt[:], bmod_in[l], writes=[bmt]); DMA("sp", cwt[:], cw_in[l], writes=[cwt])
            for k in range(KC):
                DMA("pool", winb[:, k, :], win_in[l, k * 128:(k + 1) * 128, :], writes=[winb])
            for k in range(KC):
                DMA("pool", woutb[:, k, :], wout_in[l, k * 128:(k + 1) * 128, :], writes=[woutb])
            pm = nps()
            for cb in range(24):
                wm = wmb[cb % 2]
                DMA("sp", wm[:], wmod_in[l].rearrange("(k p) c -> p k c", p=128)[:, :, cb * 256:(cb + 1) * 256], writes=[wm])
                for cc in range(2):
                    c = cb * 2 + cc
                    for k in range(KC):
                        OP("pe", lambda e, wm=wm, cc=cc, c=c, k=k: e.matmul(pm[:, 2 * c:2 * c + 2], lhsT=wm[:, k, cc * 128:(cc + 1) * 128],
                                                                            rhs=cond[:, k, :], start=(k == 0), stop=(k == KC - 1)),
                           reads=[wm, cond], writes=[pm])
            OP("dve", lambda e: e.tensor_tensor(out=modv[:], in0=pm[:, 0:96].rearrange("p (c s) -> p c s", s=2),
                                                 in1=bmt[:].unsqueeze(2).to_broadcast([128, 48, 2]), op=ALU.add), reads=[pm, bmt], writes=[modv])
            for (A, nt_, g) in [(A1, n1t, 1), (A2, n2t, 4)]:
                OP("dve", lambda e, A=A, g=g: e.tensor_scalar(out=A[:], in0=modv[:, g * 8:(g + 1) * 8, :], scalar1=1.0, scalar2=None, op0=ALU.add),
                   reads=[modv], writes=[A])
                OP("dve", lambda e, A=A, nt_=nt_: e.tensor_tensor(out=A[:], in0=A[:], in1=nt_[:].unsqueeze(2).to_broadcast([128, KC, 2]), op=ALU.mult),
                   reads=[A, nt_], writes=[A])

            for sname in ["c", "x"]:
                S_ = streams[sname]
                s = S_["s"]; n = S_["n"]; ntile = S_["nt"]; R = S_["R"]; Rb = S_["Rb"]; T = S_["T"]
                nsub = n // 128
                if sname == "c":
                    OP("dve", lambda e: e.memset(Sst[:], 0.0), writes=[Sst])
                only_states = (sname == "c" and last_layer)
                for i in range(ntile):
                    t0 = i * n
                    DMA("sp", xt[:, :, :n], rsview(R, t0, n), reads=[Rb[i]], writes=[xt])
                    norm_mod(n, A1, modv, 0, s, hT)
                    for j in range(nsub):
                        pv = nps()
                        for k in range(KC):
                            OP("pe", lambda e, k=k, j=j, pv=pv: e.matmul(pv[:, :], lhsT=hT[:, k, j * 128:(j + 1) * 128], rhs=winb[:, k, 0:512],
                                                                         start=(k == 0), stop=(k == KC - 1)), reads=[hT, winb], writes=[pv])
                        OP("act", lambda e, j=j, pv=pv: e.copy(out=vtm[:, j, :], in_=pv[:, :]), reads=[pv], writes=[vtm])
                    DMA("sp", VS[t0:t0 + n, :].rearrange("(j p) c -> p j c", p=128), vtm[:, :nsub, :], reads=[vtm], writes=[scr_b["VS"][i]])

                    def proj(cc):
                        ps = nps()
                        for k in range(KC):
                            OP("pe", lambda e, k=k, ps=ps: e.matmul(ps[:, :n], lhsT=winb[:, k, cc * 128:(cc + 1) * 128], rhs=hT[:, k, :n],
                                                                    start=(k == 0), stop=(k == KC - 1)), reads=[hT, winb], writes=[ps])
                        return ps
                    for h in range(4):
                        ps = proj(8 + h)
                        OP("act", lambda e, h=h, ps=ps: e.copy(out=stg[:, h, :n], in_=ps[:, :n]), reads=[ps], writes=[stg])
                    DMA("sp", s4view(ZB, t0, n), stg[:, :, :n], reads=[stg], writes=[scr_b["ZB"][i]])
                    for h in range(4):
                        psq = proj(12 + h)
                        OP("act", lambda e, h=h, psq=psq: e.copy(out=ql[:, h, :n], in_=psq[:, :n]), reads=[psq], writes=[ql])
                        psz = proj(4 + h)
                        zps_t[0] = psz
                        last = gate_math(psz[:, :n], ql[:, h, :n], ql, l, 0, h, n, False)
                        recur(l, 0, h, n, False, last, osb)
                    DMA("sp", s4view(QS, t0, n), ql[:, :, :n], reads=[ql], writes=[scr_b["QS"][i]])
                    DMA("sp", s4view(OF, t0, n), osb[:, :, :n], reads=[osb], writes=[scr_b["OF"][i]])
                    if only_states:
                        continue
                    for h in range(4):
                        ps = proj(16 + h)
                        OP("act", lambda e, h=h, ps=ps: e.activation(out=stg[:, h, :n], in_=ps[:, :n], func=AF.Silu), reads=[ps], writes=[stg])
                    DMA("sp", s4view(SG, t0, n), stg[:, :, :n], reads=[stg], writes=[scr_b["SG"][i]])
                    for c in range(4):
                        pc = proj(20 + c)
                        OP("act", lambda e, pc=pc: e.copy(out=cva[:, :n], in_=pc[:, :n]), reads=[pc], writes=[cva])
                        ph = proj(28 + c)
                        OP("dve", lambda e, c=c, ph=ph: e.tensor_tensor(out=stg[:, c, :n], in0=ph[:, :n], in1=cva[:, :n], op=ALU.mult),
                           reads=[ph, cva], writes=[stg])
                    DMA("sp", s4view(US, t0, n), stg[:, :, :n], reads=[stg], writes=[scr_b["US"][i]])
                    for c in range(4):
                        pb = proj(24 + c)
                        OP("act", lambda e, c=c, pb=pb: e.copy(out=stg[:, c, :n], in_=pb[:, :n]), reads=[pb], writes=[stg])
                    DMA("sp", s4view(BG, t0, n), stg[:, :, :n], reads=[stg], writes=[scr_b["BG"][i]])
                for i in range(ntile - 1, -1, -1):
                    t0 = i * n
                    DMA("sp", zbl[:, :, :n], s4view(ZB, t0, n), reads=[scr_b["ZB"][i]], writes=[zbl])
                    DMA("sp", ql[:, :, :n], s4view(QS, t0, n), reads=[scr_b["QS"][i]], writes=[ql])
                    DMA("sp", vtm[:, :nsub, :], VS[t0:t0 + n, :].rearrange("(j p) c -> p j c", p=128), reads=[scr_b["VS"][i]], writes=[vtm])
                    for h in range(4):
                        zps_t[0] = zbl
                        last = gate_math(zbl[:, h, :n], ql[:, h, :n], ql, l, 1, h, n, True)
                        recur(l, 1, h, n, True, last, osb)
                    if only_states:
                        continue
                    DMA("sp", ofl[:, :, :n], s4view(OF, t0, n), reads=[scr_b["OF"][i]], writes=[ofl])
                    DMA("sp", sgl[:, :, :n], s4view(SG, t0, n), reads=[scr_b["SG"][i]], writes=[sgl])
                    DMA("sp", bgl[:, :, :n], s4view(BG, t0, n), reads=[scr_b["BG"][i]], writes=[bgl])
                    OP("pool", lambda e: e.memset(uh[:], 0.0), writes=[uh])
                    lo = 64 if t0 > 0 else 0
                    hi = 64 if t0 + n < T else 0
                    rd = [scr_b["US"][i]] + ([scr_b["US"][i - 1]] if lo else []) + ([scr_b["US"][i + 1]] if hi else [])
                    DMA("sp", uh[:, :, 64 - lo:64 + n + hi], s4view(US, t0 - lo, n + lo + hi), reads=rd, writes=[uh])
                    DMA("sp", xt[:, :, :n], rsview(R, t0, n), reads=[Rb[i]], writes=[xt])
                    OP("dve", lambda e: e.tensor_tensor(out=osb[:, :, :n], in0=osb[:, :, :n], in1=ofl[:, :, :n], op=ALU.add), reads=[osb, ofl], writes=[osb])
                    OP("act", lambda e: e.activation(out=sq8[:, 0:4, :n], in_=osb[:, :, :n], func=AF.Square), reads=[osb], writes=[sq8])
                    for h in range(4):
                        ps = nps()
                        OP("pe", lambda e, h=h, ps=ps: e.matmul(ps[:, :n], lhsT=ones[:], rhs=sq8[:, h, :n], start=True, stop=True), reads=[ones, sq8], writes=[ps])
                        OP("act", lambda e, ps=ps: e.activation(out=rstd[:, :n], in_=ps[:, :n], func=AF.Sqrt, scale=1.0 / 128, bias=epsc[:]),
                           reads=[ps, epsc], writes=[rstd])
                        OP("dve", lambda e: e.reciprocal(out=rstd[:, :n], in_=rstd[:, :n]), reads=[rstd], writes=[rstd])
                        OP("dve", lambda e, h=h: e.tensor_tensor(out=osb[:, h, :n], in0=osb[:, h, :n], in1=rstd[:, :n], op=ALU.mult), reads=[osb, rstd], writes=[osb])
                        OP("dve", lambda e, h=h: e.tensor_tensor(out=mix[:, h, :n], in0=osb[:, h, :n], in1=sgl[:, h, :n], op=ALU.mult), reads=[osb, sgl], writes=[mix])
                    for c in range(4):
                        ctr = uh[:, c, 64:64 + n]
                        OP("dve", lambda e, c=c, ctr=ctr: e.tensor_scalar(out=cva[:, :n], in0=ctr, scalar1=cwt[:, c, 1:2], scalar2=None, op0=ALU.mult),
                           reads=[uh, cwt], writes=[cva])
                        if sname == "x" and c >= 2:
                            OP("dve", lambda e, c=c: e.scalar_tensor_tensor(out=cva[:, :n], in0=uh[:, c, 0:n], scalar=cwt[:, c, 0:1], in1=cva[:, :n],
                                                                            op0=ALU.mult, op1=ALU.add), reads=[uh, cwt, cva], writes=[cva])
                            OP("dve", lambda e, c=c: e.scalar_tensor_tensor(out=cva[:, :n], in0=uh[:, c, 128:128 + n], scalar=cwt[:, c, 2:3], in1=cva[:, :n],
                                                                            op0=ALU.mult, op1=ALU.add), reads=[uh, cwt, cva], writes=[cva])
                        elif sname == "x":
                            u3 = uh[:, c, 64:64 + n].rearrange("p (r w) -> p r w", w=64)
                            c3 = cva[:, :n].rearrange("p (r w) -> p r w", w=64)
                            OP("dve", lambda e, c=c, u3=u3, c3=c3: e.scalar_tensor_tensor(out=c3[:, :, 1:64], in0=u3[:, :, 0:63], scalar=cwt[:, c, 0:1], in1=c3[:, :, 1:64],
                                                                                        op0=ALU.mult, op1=ALU.add), reads=[uh, cwt, cva], writes=[cva])
                            OP("dve", lambda e, c=c, u3=u3, c3=c3: e.scalar_tensor_tensor(out=c3[:, :, 0:63], in0=u3[:, :, 1:64], scalar=cwt[:, c, 2:3], in1=c3[:, :, 0:63],
                                                                                        op0=ALU.mult, op1=ALU.add), reads=[uh, cwt, cva], writes=[cva])
                        else:
                            OP("dve", lambda e, c=c: e.scalar_tensor_tensor(out=cva[:, :n], in0=uh[:, c, 63:63 + n], scalar=cwt[:, c, 0:1], in1=cva[:, :n],
                                                                            op0=ALU.mult, op1=ALU.add), reads=[uh, cwt, cva], writes=[cva])
                            OP("dve", lambda e, c=c: e.scalar_tensor_tensor(out=cva[:, :n], in0=uh[:, c, 65:65 + n], scalar=cwt[:, c, 2:3], in1=cva[:, :n],
                                                                            op0=ALU.mult, op1=ALU.add), reads=[uh, cwt, cva], writes=[cva])
                        OP("dve", lambda e, c=c: e.tensor_tensor(out=mix[:, 4 + c, :n], in0=cva[:, :n], in1=bgl[:, c, :n], op=ALU.mult), reads=[cva, bgl], writes=[mix])
                    for m in range(KC):
                        ps = nps()
                        for fch in range(KC):
                            OP("pe", lambda e, m=m, fch=fch, ps=ps: e.matmul(ps[:, :n], lhsT=woutb[:, fch, m * 128:(m + 1) * 128], rhs=mix[:, fch, :n],
                                                                             start=(fch == 0), stop=(fch == KC - 1)), reads=[woutb, mix], writes=[ps])
                        OP("dve", lambda e, m=m, ps=ps: e.scalar_tensor_tensor(out=xt[:, m, :n], in0=ps[:, :n], scalar=modv[:, 16 + m, s:s + 1], in1=xt[:, m, :n],
                                                                               op0=ALU.mult, op1=ALU.add), reads=[ps, modv, xt], writes=[xt])
                    DMA("sp", rsview(R, t0, n), xt[:, :, :n], reads=[xt], writes=[Rb[i]])
            fw.barrier()
            mst.close()
            if stage == 1:
                break
            peer_layer(l, last_layer)
            if stage == 2:
                break

        if stage in (1, 2):
            for i in range(seq // NT):
                DMA("sp", xt[:, :, :NT], rsview(XR, i * NT, NT), reads=[XRb[i]], writes=[xt])
                DMA("sp", rsview(yT_out, i * NT, NT), xt[:, :, :NT], reads=[xt], writes=[dbuf("y")])
            if dbg:
                DMA("sp", xt[:, :, :ctxlen], rsview(CR, 0, ctxlen), reads=[CRb[0]], writes=[xt])
                DMA("sp", rsview(dbgc_out, 0, ctxlen), xt[:, :, :ctxlen], reads=[xt], writes=[dbuf("y2")])
        fw.finish()
        print("instructions:", fw.ninst, {k: v.count for k, v in fw.E.items()})
    return nc


def make_consts():
    bf = ml_dtypes.bfloat16
    ident = np.eye(128, dtype=np.float32).astype(bf)
    ones = np.ones((128, 128), np.float32).astype(bf)
    s = np.arange(128)[:, None]; t = np.arange(128)[None, :]
    same = (s // CH) == (t // CH)
    maskF = (same & (s <= t)).astype(np.float32).astype(bf)
    maskB = (same & (s >= t)).astype(np.float32).astype(bf)
    resetm = np.ones((128, NT), np.float32); resetm[:, ::CH] = 0.0
    rowm = (np.arange(128)[:, None] // CH == np.arange(4)[None, :]).astype(np.float32)
    return dict(identF=np.eye(128, dtype=np.float32), ident=ident, ones=ones, maskF=maskF, maskB=maskB, resetm=resetm, rowm=rowm)


def prep_shared(inp, depth):
    f = lambda a: np.ascontiguousarray(np.asarray(a, dtype=np.float32))
    sh = {}
    sh["w_mod"] = f(inp["w_mod"])
    sh["b_mod"] = f(np.asarray(inp["b_mod"]).reshape(depth, 48, 128).transpose(0, 2, 1))
    sh["n1"] = f(np.asarray(inp["norm1_g"]).reshape(depth, KC, 128).transpose(0, 2, 1))
    sh["n2"] = f(np.asarray(inp["norm2_g"]).reshape(depth, KC, 128).transpose(0, 2, 1))
    sh["fg"] = f(np.repeat(np.asarray(inp["final_g"]).reshape(KC, 128).T[:, :, None], 2, axis=2))
    sh["w_in"] = f(inp["w_in"]); sh["w_out"] = f(inp["w_out"])
    sh["cw"] = f(np.asarray(inp["conv_w"]).reshape(depth, 3, 4, 128).transpose(0, 3, 2, 1))
    sh["lbl"] = f(np.asarray(inp["lb_logits"]).reshape(depth, 2, 4, 128).transpose(3, 0, 1, 2))
    sh["wq"] = f(inp["peer_wq"])
    sh["skT"] = f(np.asarray(inp["peer_subkeys"]).reshape(depth, 16, 128, 128).transpose(0, 3, 1, 2))
    sh["uT"] = f(np.asarray(inp["peer_u"]).transpose(0, 2, 1))
    sh["v"] = f(inp["peer_v"])
    sh.update(make_consts())
    return sh


def prep_core(inp, b):
    f = lambda a: np.ascontiguousarray(np.asarray(a, dtype=np.float32))
    m = {}
    m["xT"] = f(np.asarray(inp["x"])[b].T)
    m["cxT"] = f(np.asarray(inp["ctx"])[b].T)
    cv = np.stack([np.asarray(inp["c"])[b], np.asarray(inp["c_ctx"])], axis=-1)
    m["cv"] = f(cv.reshape(KC, 128, 2).transpose(1, 0, 2))
    return m


def kernel(**inp):
    x = np.asarray(inp["x"])
    B, seq, _ = x.shape
    ctxlen = np.asarray(inp["ctx"]).shape[1]
    depth = np.asarray(inp["w_in"]).shape[0]
    nc = build(depth, seq, ctxlen)
    sh = prep_shared(inp, depth)
    in_maps = []
    for b in range(B):
        m = dict(sh); m.update(prep_core(inp, b)); in_maps.append(m)
    res = run_bass_kernel_spmd(nc, in_maps, core_ids=list(range(B)))
    out = np.stack([np.ascontiguousarray(res.results[b]["yT"].T) for b in range(B)], axis=0)
    return out.astype(np.float32)
```

```python
import numpy as np
import ml_dtypes
from contextlib import ExitStack
import concourse.bass as bass
import concourse.mybir as mybir
from concourse.bass_utils import run_bass_kernel_spmd

F32 = mybir.dt.float32
BF16 = mybir.dt.bfloat16
AF = mybir.ActivationFunctionType
ALU = mybir.AluOpType
AX = mybir.AxisListType

D = 1024
KC = 8
NT = 256
NP = 256
CH = 32
EPS = 1e-6
EPOCH = 30000
NDMA = 12
NEXP = 16384


class Tok:
    __slots__ = ("sem", "val", "eng")

    def __init__(self, sem, val, eng):
        self.sem = sem; self.val = val; self.eng = eng


class Buf:
    def __init__(self, name=""):
        self.name = name; self.w = None; self.r = {}


class Eng:
    def __init__(self, name, obj):
        self.name = name; self.obj = obj
        self.sems = []; self.count = 0; self.seen = {}
        self.dma_sems = []; self.dma_n = 0


class FW:
    def __init__(self, nc, stack):
        self.nc = nc; self.stack = stack
        self.E = {n: Eng(n, getattr(nc, o)) for n, o in
                  [("pe", "tensor"), ("dve", "vector"), ("act", "scalar"), ("pool", "gpsimd"), ("sp", "sync")]}
        self.ninst = 0

    def newsem(self, name):
        return self.stack.enter_context(self.nc.semaphore(name))

    def _wait(self, e, tok):
        if tok is None:
            return
        k = id(tok.sem)
        if e.seen.get(k, 0) >= tok.val:
            return
        e.obj.wait_ge(tok.sem, tok.val)
        e.seen[k] = tok.val

    def _deps(self, e, reads, writes):
        for b in reads:
            if b.w is not None:
                self._wait(e, b.w)
        for b in writes:
            if b.w is not None and b.w.eng != e.name:
                self._wait(e, b.w)
            for en, t in b.r.items():
                if en != e.name:
                    self._wait(e, t)

    def _commit(self, tok, reads, writes, rkey):
        for b in reads:
            b.r[rkey] = tok
        for b in writes:
            b.w = tok; b.r = {}

    def op(self, eng, fn, reads=(), writes=()):
        e = self.E[eng]
        self._deps(e, reads, writes)
        ep = e.count // EPOCH
        while len(e.sems) <= ep:
            e.sems.append(self.newsem(f"s_{eng}_{len(e.sems)}"))
        ins = fn(e.obj)
        val = e.count - ep * EPOCH + 1
        ins.then_inc(e.sems[ep], 1)
        e.count += 1
        self.ninst += 1
        tok = Tok(e.sems[ep], val, eng)
        self._commit(tok, reads, writes, eng)
        return tok

    def dma(self, eng, out, in_, reads=(), writes=(), **kw):
        e = self.E[eng]
        self._deps(e, reads, writes)
        j = e.dma_n % NDMA
        if len(e.dma_sems) <= j:
            e.dma_sems.append([self.newsem(f"d_{eng}_{j}"), 0])
        slot = e.dma_sems[j]
        if slot[1] > 0:
            self._wait(e, Tok(slot[0], slot[1], "dma"))
        slot[1] += 16
        e.obj.dma_start(out=out, in_=in_, **kw).then_inc(slot[0], 16)
        e.dma_n += 1
        self.ninst += 1
        tok = Tok(slot[0], slot[1], "dma")
        self._commit(tok, reads, writes, f"dma_{eng}_{j}")
        return tok

    def barrier(self):
        toks = []
        for en in self.E.values():
            if en.count > 0:
                ep = (en.count - 1) // EPOCH
                toks.append(Tok(en.sems[ep], en.count - ep * EPOCH, en.name))
            for slot in en.dma_sems:
                if slot[1] > 0:
                    toks.append(Tok(slot[0], slot[1], "dma"))
        for e in self.E.values():
            for t in toks:
                if t.eng != e.name:
                    self._wait(e, t)

    def finish(self):
        e = self.E["sp"]
        for en in self.E.values():
            for slot in en.dma_sems:
                if slot[1] > 0:
                    self._wait(e, Tok(slot[0], slot[1], "dma"))


class Tile:
    def __init__(self, t, name):
        self.t = t; self.b = Buf(name)

    def __getitem__(self, k):
        return self.t[k]


def build(depth, seq, ctxlen, stage=99, dbg=False):
    nc = bass.Bass("TRN2", target_bir_lowering=False)
    assert seq % NT == 0 and ctxlen <= NT and ctxlen % 128 == 0 and seq % NP == 0
    rows_per_tile = NT // 64

    def din(name, shape, dt=F32):
        return nc.dram_tensor(name, list(shape), dt, kind="ExternalInput").ap()

    def dscr(name, shape, dt=F32):
        return nc.dram_tensor(name, list(shape), dt).ap()

    xT_in = din("xT", [D, seq]); cxT_in = din("cxT", [D, ctxlen])
    cv_in = din("cv", [128, KC, 2])
    wmod_in = din("w_mod", [depth, D, 6 * D]); bmod_in = din("b_mod", [depth, 128, 48])
    n1_in = din("n1", [depth, 128, KC]); n2_in = din("n2", [depth, 128, KC]); fg_in = din("fg", [128, KC, 2])
    win_in = din("w_in", [depth, D, 4096]); wout_in = din("w_out", [depth, D, D])
    cw_in = din("cw", [depth, 128, 4, 3]); lbl_in = din("lbl", [128, depth, 2, 4])
    wq_in = din("wq", [depth, D, 2048]); skT_in = din("skT", [depth, 128, 16, 128])
    uT_in = din("uT", [depth, D, NEXP]); v_in = din("v", [depth, NEXP, D])
    ident_in = din("ident", [128, 128], BF16); ones_in = din("ones", [128, 128], BF16)
    maskF_in = din("maskF", [128, 128], BF16); maskB_in = din("maskB", [128, 128], BF16)
    identF_in = din("identF", [128, 128]); reset_in = din("resetm", [128, NT]); rowm_in = din("rowm", [128, 4])
    yT_out = nc.dram_tensor("yT", [D, seq], F32, kind="ExternalOutput").ap()
    dbg_out = nc.dram_tensor("dbg", [D, seq], F32, kind="ExternalOutput").ap() if dbg else None
    dbgc_out = nc.dram_tensor("dbgc", [D, ctxlen], F32, kind="ExternalOutput").ap() if dbg else None

    TM = max(seq, ctxlen)
    XR = dscr("XR", [D, seq]); CR = dscr("CR", [D, ctxlen])
    ZB = dscr("ZB", [512, TM]); QS = dscr("QS", [512, TM]); SG = dscr("SG", [512, TM])
    OF = dscr("OF", [512, TM]); US = dscr("US", [512, TM]); BG = dscr("BG", [512, TM])
    VS = dscr("VS", [TM, 512], BF16)
    U16 = dscr("U16", [NEXP // 512, 128, KC, 512], BF16); V16 = dscr("V16", [NEXP, D], BF16)
    U16b = Buf("U16"); V16b = Buf("V16")

    with ExitStack() as st:
        fw = FW(nc, st)

        def sb(name, shape, dt=F32):
            return Tile(st.enter_context(nc.sbuf_tensor("sb_" + name, list(shape), dt)), name)

        def dbuf(name):
            return Buf(name)

        PS = {"F": [], "B": []}
        cnt = {"f": 0, "b": 0}

        def alloc_psum(stack, tag, nf, nb):
            PS["F"] = [Tile(stack.enter_context(nc.psum_tensor(f"psF{tag}_{i}", [128, 512], F32)), f"psF{i}") for i in range(nf)]
            PS["B"] = [Tile(stack.enter_context(nc.psum_tensor(f"psB{tag}_{i}", [128, 1024], BF16)), f"psB{i}") for i in range(nb)]

        def nps():
            cnt["f"] += 1
            return PS["F"][cnt["f"] % cnt["n"]]

        def npsb():
            cnt["b"] += 1
            return PS["B"][cnt["b"] % 2]

        def OP(eng, fn, reads=(), writes=()):
            return fw.op(eng, fn, reads=[x.b if hasattr(x, "b") else x for x in reads],
                         writes=[x.b if hasattr(x, "b") else x for x in writes])

        def DMA(eng, out, in_, reads=(), writes=(), **kw):
            return fw.dma(eng, out, in_, reads=[x.b if hasattr(x, "b") else x for x in reads],
                          writes=[x.b if hasattr(x, "b") else x for x in writes], **kw)

        ident = sb("ident", [128, 128], BF16); ones = sb("ones", [128, 128], BF16)
        maskF = sb("maskF", [128, 128], BF16); maskB = sb("maskB", [128, 128], BF16)
        resetm = sb("resetm", [128, NT]); rowm = sb("rowm", [128, 4])
        DMA("sp", rowm[:], rowm_in[:, :], writes=[rowm])
        for tl, src in [(ident, ident_in), (ones, ones_in), (maskF, maskF_in), (maskB, maskB_in), (resetm, reset_in)]:
            DMA("sp", tl[:], src[:, :], writes=[tl])
        cond = sb("cond", [128, KC, 2])
        DMA("sp", cond[:], cv_in[:, :, :], writes=[cond])
        OP("act", lambda e: e.activation(out=cond[:], in_=cond[:], func=AF.Silu), reads=[cond], writes=[cond])
        epsc = sb("epsc", [128, 1])
        OP("dve", lambda e: e.memset(epsc[:], EPS), writes=[epsc])
        lbe = sb("lbe", [128, depth, 8]); lbs = sb("lbs", [128, 8]); lb = sb("lb", [128, depth, 8]); oml = sb("oml", [128, depth, 8])
        DMA("sp", lbe[:], lbl_in.rearrange("p l a b -> p l (a b)"), writes=[lbe])
        OP("act", lambda e: e.activation(out=lbe[:], in_=lbe[:], func=AF.Exp), reads=[lbe], writes=[lbe])
        OP("dve", lambda e: e.tensor_copy(out=lbs[:], in_=lbe[:, 0, :]), reads=[lbe], writes=[lbs])
        for l in range(1, depth):
            OP("dve", lambda e, l=l: e.tensor_tensor(out=lbs[:], in0=lbs[:], in1=lbe[:, l, :], op=ALU.add), reads=[lbs, lbe], writes=[lbs])
        OP("dve", lambda e: e.reciprocal(out=lbs[:], in_=lbs[:]), reads=[lbs], writes=[lbs])
        OP("dve", lambda e: e.memset(lb[:, 0, :], 0.0), writes=[lb])
        for l in range(1, depth):
            OP("dve", lambda e, l=l: e.tensor_tensor(out=lbe[:, l, :], in0=lbe[:, l, :], in1=lbs[:], op=ALU.mult), reads=[lbe, lbs], writes=[lbe])
            OP("dve", lambda e, l=l: e.tensor_tensor(out=lb[:, l, :], in0=lb[:, l - 1, :], in1=lbe[:, l, :], op=ALU.add), reads=[lb, lbe], writes=[lb])
        OP("dve", lambda e: e.tensor_scalar(out=oml[:], in0=lb[:], scalar1=-1.0, scalar2=1.0, op0=ALU.mult, op1=ALU.add), reads=[lb], writes=[oml])

        XRb = [dbuf(f"XR{i}") for i in range(seq // NT)]
        CRb = [dbuf("CR")]
        for i in range(seq // NT):
            DMA("sp", XR[:, i * NT:(i + 1) * NT], xT_in[:, i * NT:(i + 1) * NT], writes=[XRb[i]])
        DMA("sp", CR[:, :], cxT_in[:, :], writes=[CRb[0]])

        modv = sb("modv", [128, 48, 2])
        A1 = sb("A1", [128, KC, 2]); A2 = sb("A2", [128, KC, 2])
        n1t = sb("n1t", [128, KC]); n2t = sb("n2t", [128, KC]); bmt = sb("bmt", [128, 48]); cwt = sb("cwt", [128, 4, 3])
        fgt = sb("fgt", [128, KC, 2]); zerob = sb("zerob", [128, KC, 2])
        DMA("sp", fgt[:], fg_in[:, :, :], writes=[fgt])
        OP("dve", lambda e: e.memset(zerob[:], 0.0), writes=[zerob])
        identF = sb("identF", [128, 128])
        DMA("sp", identF[:], identF_in[:, :], writes=[identF])
        Sst = sb("Sst", [128, 8, 128])
        rstd = sb("rstd", [128, NT])

        streams = {
            "c": dict(T=ctxlen, R=CR, Rb=CRb, s=1, nt=1, n=ctxlen),
            "x": dict(T=seq, R=XR, Rb=XRb, s=0, nt=seq // NT, n=NT),
        }
        scr_b = {nm: [dbuf(f"{nm}{i}") for i in range(TM // min(NT, ctxlen) + 1)] for nm in ["ZB", "QS", "SG", "OF", "US", "BG", "VS"]}

        def rsview(R, t0, n):
            return R.rearrange("(k p) t -> p k t", p=128)[:, :, t0:t0 + n]

        def s4view(S, t0, n):
            return S.rearrange("(k p) t -> p k t", p=128)[:, :, t0:t0 + n]

        def norm_mod(n, A, Bt, Bm_col0, s, out_bf, xin, tmp8, sq8):
            OP("act", lambda e: e.activation(out=sq8[:, :, :n], in_=xin[:, :, :n], func=AF.Square), reads=[xin], writes=[sq8])
            ps = nps()
            for k in range(KC):
                OP("pe", lambda e, k=k: e.matmul(ps[:, :n], lhsT=ones[:], rhs=sq8[:, k, :n], start=(k == 0), stop=(k == KC - 1)),
                   reads=[ones, sq8], writes=[ps])
            OP("act", lambda e: e.activation(out=rstd[:, :n], in_=ps[:, :n], func=AF.Sqrt, scale=1.0 / D, bias=epsc[:]), reads=[ps, epsc], writes=[rstd])
            OP("dve", lambda e: e.reciprocal(out=rstd[:, :n], in_=rstd[:, :n]), reads=[rstd], writes=[rstd])
            OP("dve", lambda e: e.tensor_tensor(out=tmp8[:, :, :n], in0=xin[:, :, :n],
                                                 in1=rstd[:, :n].unsqueeze(1).to_broadcast([128, KC, n]), op=ALU.mult),
               reads=[xin, rstd], writes=[tmp8])
            for k in range(KC):
                OP("act", lambda e, k=k: e.activation(out=out_bf[:, k, :n], in_=tmp8[:, k, :n], func=AF.Identity,
                                                       scale=A[:, k, s:s + 1], bias=Bt[:, Bm_col0 + k, s:s + 1]),
                   reads=[tmp8, A, Bt], writes=[out_bf])

        def gate_math(zps, qsrc_ap, qsrc_t, l, d, h, n, bwd):
            nchk = n // CH
            sg, f, key, lf, b, bc, e1, e2, e3, e4 = (W[x] for x in ["sg", "f", "key", "lf", "b", "bc", "e1", "e2", "e3", "e4"])
            col = d * 4 + h
            OP("act", lambda e: e.activation(out=sg[:, :n], in_=zps, func=AF.Sigmoid), reads=[zps_t[0]], writes=[sg])
            OP("dve", lambda e: e.tensor_scalar(out=f[:, :n], in0=sg[:, :n], scalar1=oml[:, l, col:col + 1], scalar2=lb[:, l, col:col + 1],
                                                 op0=ALU.mult, op1=ALU.add), reads=[sg, oml, lb], writes=[f])
            OP("pool", lambda e: e.tensor_scalar(out=f[:, :n], in0=f[:, :n], scalar1=1e-20, scalar2=None, op0=ALU.max), reads=[f], writes=[f])
            OP("pool", lambda e: e.tensor_scalar(out=key[:, :n], in0=f[:, :n], scalar1=-1.0, scalar2=1.0, op0=ALU.mult, op1=ALU.add),
               reads=[f], writes=[key])
            OP("act", lambda e: e.activation(out=lf[:, :n], in_=f[:, :n], func=AF.Ln), reads=[f], writes=[lf])
            OP("dve", lambda e: e.tensor_tensor_scan(out=b[:, :n], data0=resetm[:, :n], data1=lf[:, :n], initial=0.0, op0=ALU.mult, op1=ALU.add),
               reads=[resetm, lf], writes=[b])
            b3 = b[:, :n].rearrange("p (c t) -> p c t", t=CH)
            if bwd:
                lf3 = lf[:, :n].rearrange("p (c t) -> p c t", t=CH)
                bc3 = bc[:, :n].rearrange("p (c t) -> p c t", t=CH)
                OP("dve", lambda e: e.tensor_tensor(out=bc3, in0=b3[:, :, CH - 1:CH].to_broadcast([128, nchk, CH]), in1=b3, op=ALU.subtract),
                   reads=[b], writes=[bc])
                OP("dve", lambda e: e.tensor_tensor(out=b[:, :n], in0=bc[:, :n], in1=lf[:, :n], op=ALU.add), reads=[bc, lf], writes=[b])
                mid = CH // 2; last = 0
            else:
                mid = CH // 2 - 1; last = CH - 1
            bc3 = bc[:, :n].rearrange("p (c t) -> p c t", t=CH)
            OP("dve", lambda e: e.tensor_tensor(out=bc3, in0=b3, in1=b3[:, :, mid:mid + 1].to_broadcast([128, nchk, CH]), op=ALU.subtract),
               reads=[b], writes=[bc])
            OP("act", lambda e: e.activation(out=e1[:, :n], in_=bc[:, :n], func=AF.Exp), reads=[bc], writes=[e1])
            OP("act", lambda e: e.activation(out=e2[:, :n], in_=bc[:, :n], func=AF.Exp, scale=-1.0), reads=[bc], writes=[e2])
            OP("act", lambda e: e.activation(out=e3[:, :n], in_=b[:, :n], func=AF.Exp), reads=[b], writes=[e3])
            OP("dve", lambda e: e.tensor_tensor(out=bc3, in0=b3[:, :, last:last + 1].to_broadcast([128, nchk, CH]), in1=b3, op=ALU.subtract),
               reads=[b, e1, e2], writes=[bc])
            OP("act", lambda e: e.activation(out=e4[:, :n], in_=bc[:, :n], func=AF.Exp), reads=[bc], writes=[e4])
            OP("dve", lambda e: e.tensor_tensor(out=qp[:, :n], in0=qsrc_ap, in1=e1[:, :n], op=ALU.mult), reads=[qsrc_t, e1], writes=[qp])
            OP("pool", lambda e: e.tensor_tensor(out=qin[:, :n], in0=qsrc_ap, in1=e3[:, :n], op=ALU.mult), reads=[qsrc_t, e3], writes=[qin])
            OP("dve", lambda e: e.tensor_tensor(out=kp[:, :n], in0=key[:, :n], in1=e2[:, :n], op=ALU.mult), reads=[key, e2], writes=[kp])
            OP("pool", lambda e: e.tensor_tensor(out=kout[:, :n], in0=key[:, :n], in1=e4[:, :n], op=ALU.mult), reads=[key, e4], writes=[kout])
            return last

        zps_t = [None]

        def recur(l, d, h, n, bwd, last, o_dst):
            si = d * 4 + h
            e3 = W["e3"]
            nsub = n // 128
            subs = range(nsub - 1, -1, -1) if bwd else range(nsub)
            mask = maskB if bwd else maskF
            for j in subs:
                js = slice(j * 128, (j + 1) * 128)
                pt = npsb()
                OP("pe", lambda e: e.transpose(out=pt[:, 0:128], in_=kout[:, js], identity=ident[:]), reads=[kout, ident], writes=[pt])
                for c4 in range(4):
                    OP("act", lambda e, c4=c4: e.activation(out=kotm[:, c4, :], in_=pt[:, 0:128], func=AF.Identity, scale=rowm[:, c4:c4 + 1]),
                       reads=[pt, rowm], writes=[kotm])
                pa = nps()
                OP("pe", lambda e: e.matmul(pa[:, 0:128], lhsT=kp[:, js], rhs=qp[:, js], start=True, stop=True), reads=[kp, qp], writes=[pa])
                OP("dve", lambda e: e.tensor_tensor(out=attm[:], in0=pa[:, 0:128], in1=mask[:], op=ALU.mult), reads=[pa, mask], writes=[attm])
                po = nps()
                chunks = range(3, -1, -1) if bwd else range(4)
                for ci, c in enumerate(chunks):
                    cs = slice(c * CH, (c + 1) * CH)
                    tcs = slice(j * 128 + c * CH, j * 128 + (c + 1) * CH)
                    sbf = Sbf[(si * 4 + ci) % 8]
                    OP("act", lambda e, sbf=sbf: e.copy(out=sbf[:], in_=Sst[:, si, :]), reads=[Sst], writes=[sbf])
                    OP("pe", lambda e, sbf=sbf, cs=cs, tcs=tcs: e.matmul(po[:, cs], lhsT=sbf[:], rhs=qin[:, tcs], start=True, stop=False),
                       reads=[sbf, qin], writes=[po])
                    OP("pe", lambda e, cs=cs: e.matmul(po[:, cs], lhsT=vtm[:, j, h * 128:(h + 1) * 128], rhs=attm[:, cs], start=False, stop=True),
                       reads=[vtm, attm], writes=[po])
                    pS = nps()
                    OP("pe", lambda e, c=c, pS=pS: e.matmul(pS[:, 0:128], lhsT=kotm[:, c, :], rhs=vtm[:, j, h * 128:(h + 1) * 128], start=True, stop=True),
                       reads=[kotm, vtm], writes=[pS])
                    dcol = j * 128 + c * CH + last
                    OP("dve", lambda e, pS=pS, dcol=dcol: e.scalar_tensor_tensor(out=Sst[:, si, :], in0=Sst[:, si, :], scalar=e3[:, dcol:dcol + 1],
                                                                                 in1=pS[:, 0:128], op0=ALU.mult, op1=ALU.add),
                       reads=[Sst, e3, pS], writes=[Sst])
                OP("act", lambda e: e.copy(out=o_dst[:, h, js], in_=po[:, 0:128]), reads=[po], writes=[o_dst])


        HB = 512
        NHB = NEXP // HB

        def peer_layer(l, last_layer):
            pst = ExitStack()

            def sbp(name, shape, dt=F32):
                return Tile(pst.enter_context(nc.sbuf_tensor(f"p{l}_" + name, list(shape), dt)), name)
            alloc_psum(pst, f"p{l}", 8, 0); cnt["n"] = 6
            po = [PS["F"][6], PS["F"][7]]
            wqb = sbp("wqb", [128, KC, 2048], BF16); skb = sbp("skb", [128, 16, 128], BF16)
            for k in range(KC):
                DMA("pool", wqb[:, k, :], wq_in[l, k * 128:(k + 1) * 128, :], writes=[wqb])
            DMA("pool", skb[:], skT_in[l], writes=[skb])
            uTl = uT_in[l].rearrange("(k p) e -> p k e", p=128)
            for hb in range(NHB):
                DMA("pool", U16[hb], uTl[:, :, hb * HB:(hb + 1) * HB], writes=[U16b])
            for r in range(16):
                DMA("pool", V16[r * 1024:(r + 1) * 1024, :], v_in[l, r * 1024:(r + 1) * 1024, :], writes=[V16b])
            V16v = V16.rearrange("(b q p) d -> b p q d", q=4, p=128)
            xtp = [sbp(f"xt{i}", [128, KC, 128]) for i in range(2)]; hTp = [sbp(f"hT{i}", [128, KC, 128], BF16) for i in range(2)]
            tmp8 = sbp("tmp8", [128, KC, 128]); sq8 = sbp("sq8", [128, KC, 128], BF16)
            qT = sbp("qT", [128, 16, 128], BF16); sc = sbp("sc", [128, 16, 128]); scw = sbp("scw", [128, 128])
            top = sbp("top", [128, 16, 16]); cand = sbp("cand", [128, 8, 256]); cw2 = sbp("cw2", [128, 256]); c24 = sbp("c24", [128, 8, 24])
            tau = sbp("tau", [128, 8]); d16 = sbp("d16", [128, 8, 16]); Zs = sbp("Zs", [128, 8]); beta = sbp("beta", [128, 8])
            sA = sbp("sA", [128, 8, 128])
            NB = 4
            Sblk = [sbp(f"Sblk{i}", [128, 8, 128]) for i in range(NB)]; Exb = [sbp(f"Exb{i}", [128, 8, 128], BF16) for i in range(NB)]
            tmpg = [sbp(f"tmpg{i}", [128, 1024], BF16) for i in range(NB)]
            NR = 4
            ub = [sbp(f"ub{i}", [128, KC, HB], BF16) for i in range(NR)]; vb = [sbp(f"vb{i}", [128, 4, D], BF16) for i in range(NR)]
            gaT = [sbp(f"gaT{i}", [128, 1024]) for i in range(2)]; WT = [sbp(f"WT{i}", [128, 8, 128], BF16) for i in range(2)]
            otm = sbp("otm", [128, D])
            zb16 = sbp("zb16", [128, 128], BF16)
            OP("dve", lambda e: e.memset(zb16[:], 0.0), writes=[zb16])
            tmpo_ap = cand[:].rearrange("p h c -> p (h c)")[:, 0:1024].rearrange("p (k n) -> p k n", n=128)

            class _Alias:
                def __init__(self, ap, tile_):
                    self.ap = ap; self.b = tile_.b

                def __getitem__(self, k):
                    return self.ap[k]
            tmpo = _Alias(tmpo_ap, cand)
            ti = 0
            ybuf = Buf("yout")
            for sname in (["x"] if last_layer else ["c", "x"]):
                S_ = streams[sname]
                s = S_["s"]; R = S_["R"]; Rb = S_["Rb"]; T = S_["T"]; nreg = S_["n"]
                for i in range(T // 128):
                    t0 = i * 128
                    rb = Rb[t0 // nreg]
                    xt_ = xtp[ti % 2]; hT_ = hTp[ti % 2]; ti += 1
                    DMA("sp", xt_[:, :, :128], rsview(R, t0, 128), reads=[rb], writes=[xt_])
                    norm_mod(128, A2, modv, 24, s, hT_, xt_, tmp8, sq8)
                    for jc in range(16):
                        ps = nps()
                        for k in range(KC):
                            OP("pe", lambda e, k=k, jc=jc, ps=ps: e.matmul(ps[:, :128], lhsT=wqb[:, k, jc * 128:(jc + 1) * 128], rhs=hT_[:, k, :128],
                                                                           start=(k == 0), stop=(k == KC - 1)), reads=[wqb, hT_], writes=[ps])
                        OP("act", lambda e, jc=jc, ps=ps: e.copy(out=qT[:, jc, :], in_=ps[:, :128]), reads=[ps], writes=[qT])
                    for g in range(4):
                        ps = nps()
                        for jj in range(4):
                            jc = g * 4 + jj
                            OP("pe", lambda e, jc=jc, jj=jj, ps=ps: e.matmul(ps[:, jj * 128:(jj + 1) * 128], lhsT=qT[:, jc, :], rhs=skb[:, jc, :], start=True, stop=True),
                               reads=[qT, skb], writes=[ps])
                        OP("act", lambda e, g=g, ps=ps: e.copy(out=sc[:, g * 4:(g + 1) * 4, :], in_=ps[:, :].rearrange("p (a b) -> p a b", b=128)), reads=[ps], writes=[sc])
                    for jc in range(16):
                        OP("dve", lambda e, jc=jc: e.max(out=top[:, jc, 0:8], in_=sc[:, jc, :]), reads=[sc], writes=[top])
                        OP("dve", lambda e, jc=jc: e.match_replace(out=scw[:], in_to_replace=top[:, jc, 0:8], in_values=sc[:, jc, :], imm_value=-1e30),
                           reads=[top, sc], writes=[scw])
                        OP("dve", lambda e, jc=jc: e.max(out=top[:, jc, 8:16], in_=scw[:]), reads=[scw], writes=[top])
                    top4 = top[:].rearrange("p (h two) r -> p h two r", two=2)
                    cand4 = cand[:].rearrange("p h (r s) -> p h r s", s=16)
                    OP("dve", lambda e: e.tensor_tensor(out=cand4, in0=top4[:, :, 0, :].unsqueeze(3).to_broadcast([128, 8, 16, 16]),
                                                         in1=top4[:, :, 1, :].unsqueeze(2).to_broadcast([128, 8, 16, 16]), op=ALU.add), reads=[top], writes=[cand])
                    for h in range(8):
                        OP("dve", lambda e, h=h: e.max(out=c24[:, h, 0:8], in_=cand[:, h, :]), reads=[cand], writes=[c24])
                        OP("dve", lambda e, h=h: e.match_replace(out=cw2[:], in_to_replace=c24[:, h, 0:8], in_values=cand[:, h, :], imm_value=-1e30),
                           reads=[c24, cand], writes=[cw2])
                        OP("dve", lambda e, h=h: e.max(out=c24[:, h, 8:16], in_=cw2[:]), reads=[cw2], writes=[c24])
                        OP("dve", lambda e, h=h: e.match_replace(out=cw2[:], in_to_replace=c24[:, h, 8:16], in_values=cw2[:], imm_value=-1e30),
                           reads=[c24, cw2], writes=[cw2])
                        OP("dve", lambda e, h=h: e.max(out=c24[:, h, 16:24], in_=cw2[:]), reads=[cw2], writes=[c24])
                    OP("dve", lambda e: e.tensor_tensor(out=tau[:], in0=c24[:, :, 15], in1=c24[:, :, 16], op=ALU.add), reads=[c24], writes=[tau])
                    OP("dve", lambda e: e.tensor_scalar(out=tau[:], in0=tau[:], scalar1=0.5, scalar2=None, op0=ALU.mult), reads=[tau], writes=[tau])
                    OP("dve", lambda e: e.tensor_tensor(out=d16[:], in0=c24[:, :, 0:16], in1=c24[:, :, 0:1].to_broadcast([128, 8, 16]), op=ALU.subtract),
                       reads=[c24], writes=[d16])
                    OP("act", lambda e: e.activation(out=d16[:], in_=d16[:], func=AF.Exp), reads=[d16], writes=[d16])
                    OP("dve", lambda e: e.tensor_reduce(out=Zs[:], in_=d16[:], axis=AX.X, op=ALU.add), reads=[d16], writes=[Zs])
                    OP("act", lambda e: e.activation(out=Zs[:], in_=Zs[:], func=AF.Ln), reads=[Zs], writes=[Zs])
                    OP("dve", lambda e: e.tensor_tensor(out=beta[:], in0=tau[:], in1=c24[:, :, 0], op=ALU.subtract), reads=[tau, c24], writes=[beta])
                    OP("dve", lambda e: e.tensor_tensor(out=beta[:], in0=beta[:], in1=Zs[:], op=ALU.subtract), reads=[beta, Zs], writes=[beta])
                    sc4 = sc[:].rearrange("p (h two) k -> p h two k", two=2)
                    OP("dve", lambda e: e.tensor_tensor(out=sA[:], in0=sc4[:, :, 0, :], in1=tau[:].unsqueeze(2).to_broadcast([128, 8, 128]), op=ALU.subtract),
                       reads=[sc, tau], writes=[sA])

                    NBK = NEXP // 1024
                    its = [(bk, h) for bk in range(NBK) for h in range(8)]
                    pg = {}; pa = {}

                    def ldu(hb):
                        DMA("sp", ub[hb % NR][:], U16[hb], reads=[U16b], writes=[ub[hb % NR]])

                    def ldv(hb):
                        DMA("sp", vb[hb % NR][:], V16v[hb], reads=[V16b], writes=[vb[hb % NR]])

                    def blk_start(bk):
                        pa[bk] = (nps(), nps()); pg[bk] = (nps(), nps())
                        for c in range(8):
                            ub_ = ub[(2 * bk + c // 4) % NR]; pp = pa[bk][c // 4]
                            for k in range(KC):
                                OP("pe", lambda e, k=k, c=c, ub_=ub_, pp=pp: e.matmul(pp[:, (c % 4) * 128:(c % 4 + 1) * 128], lhsT=ub_[:, k, (c % 4) * 128:(c % 4 + 1) * 128],
                                                                                     rhs=hT_[:, k, :128], start=(k == 0), stop=(k == KC - 1)),
                                   reads=[ub_, hT_], writes=[pp])
                        for hf in range(2):
                            OP("pe", lambda e, hf=hf: e.matmul(pg[bk][hf][:, :], lhsT=zb16[:], rhs=hT_[:, 0:4, :].rearrange("p a b -> p (a b)"), start=True, stop=False),
                               reads=[zb16, hT_], writes=[pg[bk][hf]])
                        g_ = gaT[bk % 2]
                        for hf in range(2):
                            OP("act", lambda e, hf=hf: e.activation(out=g_[:, hf * 512:(hf + 1) * 512], in_=pa[bk][hf][:, :], func=AF.Gelu),
                               reads=[pa[bk][hf]], writes=[g_])

                    def g1(n_):
                        bk, h = its[n_]
                        a0 = bk * 8
                        sbk = Sblk[n_ % NB]; exk = Exb[n_ % NB]
                        eng = "pool" if (n_ % 4 == 3) else "dve"
                        OP(eng, lambda e: e.tensor_tensor(out=sbk[:], in0=sA[:, h, a0:a0 + 8].unsqueeze(2).to_broadcast([128, 8, 128]),
                                                          in1=sc4[:, h, 1, :].unsqueeze(1).to_broadcast([128, 8, 128]), op=ALU.add),
                           reads=[sA, sc], writes=[sbk])
                        OP("act", lambda e: e.activation(out=exk[:], in_=sbk[:], func=AF.Exp, bias=beta[:, h:h + 1]), reads=[sbk, beta], writes=[exk])

                    def g2(n_):
                        bk, h = its[n_]
                        sbk = Sblk[n_ % NB]; exk = Exb[n_ % NB]; tg = tmpg[n_ % NB]
                        sb2 = sbk[:].rearrange("p a b -> p (a b)"); ex2 = exk[:].rearrange("p a b -> p (a b)")
                        OP("dve", lambda e: e.scalar_tensor_tensor(out=tg[:], in0=sb2, scalar=0.0, in1=ex2, op0=ALU.is_ge, op1=ALU.mult),
                           reads=[sbk, exk], writes=[tg])
                        for c in range(8):
                            pp = pg[bk][c // 4]
                            OP("pe", lambda e, c=c, pp=pp: e.matmul(pp[:, (c % 4) * 128:(c % 4 + 1) * 128], lhsT=tg[:, c * 128:(c + 1) * 128], rhs=ident[:],
                                                                   start=False, stop=(h == 7 and c % 4 == 3)), reads=[tg, ident], writes=[pp])

                    def blk_end(bk):
                        g_ = gaT[bk % 2]; w_ = WT[bk % 2]
                        for hf in range(2):
                            OP("dve", lambda e, hf=hf: e.tensor_tensor(out=w_[:, hf * 4:(hf + 1) * 4, :].rearrange("p a b -> p (a b)"), in0=g_[:, hf * 512:(hf + 1) * 512],
                                                                        in1=pg[bk][hf][:, :], op=ALU.mult), reads=[g_, pg[bk][hf]], writes=[w_])
                        for c in range(8):
                            vb_ = vb[(2 * bk + c // 4) % NR]
                            for hf in range(2):
                                OP("pe", lambda e, c=c, hf=hf, vb_=vb_: e.matmul(po[hf][:, :], lhsT=w_[:, c, :], rhs=vb_[:, c % 4, hf * 512:(hf + 1) * 512],
                                                                                start=(bk == 0 and c == 0), stop=(bk == NBK - 1 and c == 7)),
                                   reads=[w_, vb_], writes=[po[hf]])

                    ldu(0); ldu(1); ldv(0); ldv(1); ldv(2); ldv(3)
                    LAG = 2
                    pending_end = {}
                    for n_ in range(len(its) + LAG + 4):
                        if n_ < len(its):
                            bk, h = its[n_]
                            if h == 0:
                                blk_start(bk)
                                if 2 * bk + 2 < NHB:
                                    ldu(2 * bk + 2); ldu(2 * bk + 3)
                            g1(n_)
                        m_ = n_ - LAG
                        if 0 <= m_ < len(its):
                            g2(m_)
                            if its[m_][1] == 7:
                                pending_end[n_ + 3] = its[m_][0]
                        if n_ in pending_end:
                            bke = pending_end.pop(n_)
                            blk_end(bke)
                            if 2 * (bke + 2) < NHB:
                                ldv(2 * (bke + 2)); ldv(2 * (bke + 2) + 1)
                    for hf in range(2):
                        OP("act", lambda e, hf=hf: e.copy(out=otm[:, hf * 512:(hf + 1) * 512], in_=po[hf][:, :]), reads=[po[hf]], writes=[otm])
                    for m in range(KC):
                        pt = nps()
                        OP("pe", lambda e, m=m, pt=pt: e.transpose(out=pt[:, :128], in_=otm[:, m * 128:(m + 1) * 128], identity=identF[:]), reads=[otm, identF], writes=[pt])
                        OP("dve", lambda e, m=m, pt=pt: e.scalar_tensor_tensor(out=xt_[:, m, :128], in0=pt[:, :128], scalar=modv[:, 40 + m, s:s + 1], in1=xt_[:, m, :128],
                                                                               op0=ALU.mult, op1=ALU.add), reads=[pt, modv, xt_], writes=[xt_])
                    if last_layer and stage == 99:
                        norm_mod(128, fgt, zerob, 0, 0, tmpo, xt_, tmp8, sq8)
                        DMA("sp", rsview(yT_out, t0, 128), tmpo[:], reads=[tmpo], writes=[ybuf])
                    else:
                        DMA("sp", rsview(R, t0, 128), xt_[:, :, :128], reads=[xt_], writes=[rb])
            fw.barrier()
            pst.close()

        for l in range(depth):
            last_layer = (l == depth - 1)
            mst = ExitStack()

            def sbm(name, shape, dt=F32):
                return Tile(mst.enter_context(nc.sbuf_tensor(f"m{l}_" + name, list(shape), dt)), name)
            winb = sbm("winb", [128, KC, 4096], BF16)
            woutb = sbm("woutb", [128, KC, D], BF16)
            wmb = [sbm(f"wmb{i}", [128, KC, 256]) for i in range(2)]
            Sbf = [sbm(f"Sbf{i}", [128, 128], BF16) for i in range(8)]
            W = {n_: sbm("w_" + n_, [128, NT]) for n_ in ["sg", "f", "key", "lf", "b", "bc", "e1", "e2", "e3", "e4"]}
            qp = sbm("qp", [128, NT], BF16); qin = sbm("qin", [128, NT], BF16); kp = sbm("kp", [128, NT], BF16); kout = sbm("kout", [128, NT], BF16)
            vtm = sbm("vtm", [128, NT // 128, 512], BF16); kotm = sbm("kotm", [128, 4, 128], BF16)
            attm = sbm("attm", [128, 128], BF16)
            osb = sbm("osb", [128, 4, NT]); ofl = sbm("ofl", [128, 4, NT]); sgl = sbm("sgl", [128, 4, NT])
            stg = sbm("stg", [128, 4, NT])
            mix = sbm("mix", [128, KC, NT], BF16)
            uh = sbm("uh", [128, 4, NT + 128]); bgl = sbm("bgl", [128, 4, NT]); cva = sbm("cva", [128, NT])
            zbl = sbm("zbl", [128, 4, NT]); ql = sbm("ql", [128, 4, NT])
            xt = sbm("xt", [128, KC, NT]); tmp8 = sbm("tmp8", [128, KC, NT]); sq8 = sbm("sq8", [128, KC, NT], BF16)
            hT = sbm("hT", [128, KC, NT], BF16)
            alloc_psum(mst, f"m{l}", 6, 2); cnt["n"] = 6
            DMA("sp", n1t[:], n1_in[l], writes=[n1t]); DMA("sp", n2t[:], n2_in[l], writes=[n2t])
            DMA("sp", bmt[:], bmod_in[l], writes=[bmt]); DMA("sp", cwt[:], cw_in[l], writes=[cwt])
            for k in range(KC):
                DMA("pool", winb[:, k, :], win_in[l, k * 128:(k + 1) * 128, :], writes=[winb])
            for k in range(KC):
                DMA("pool", woutb[:, k, :], wout_in[l, k * 128:(k + 1) * 128, :], writes=[woutb])
            pm = nps()
            for cb in range(24):
                wm = wmb[cb % 2]
                DMA("sp", wm[:], wmod_in[l].rearrange("(k p) c -> p k c", p=128)[:, :, cb * 256:(cb + 1) * 256], writes=[wm])
                for cc in range(2):
                    c = cb * 2 + cc
                    for k in range(KC):
                        OP("pe", lambda e, wm=wm, cc=cc, c=c, k=k: e.matmul(pm[:, 2 * c:2 * c + 2], lhsT=wm[:, k, cc * 128:(cc + 1) * 128],
                                                                            rhs=cond[:, k, :], start=(k == 0), stop=(k == KC - 1)),
                           reads=[wm, cond], writes=[pm])
            OP("dve", lambda e: e.tensor_tensor(out=modv[:], in0=pm[:, 0:96].rearrange("p (c s) -> p c s", s=2),
                                                 in1=bmt[:].unsqueeze(2).to_broadcast([128, 48, 2]), op=ALU.add), reads=[pm, bmt], writes=[modv])
            for (A, nt_, g) in [(A1, n1t, 1), (A2, n2t, 4)]:
                OP("dve", lambda e, A=A, g=g: e.tensor_scalar(out=A[:], in0=modv[:, g * 8:(g + 1) * 8, :], scalar1=1.0, scalar2=None, op0=ALU.add),
                   reads=[modv], writes=[A])
                OP("dve", lambda e, A=A, nt_=nt_: e.tensor_tensor(out=A[:], in0=A[:], in1=nt_[:].unsqueeze(2).to_broadcast([128, KC, 2]), op=ALU.mult),
                   reads=[A, nt_], writes=[A])

            for sname in ["c", "x"]:
                S_ = streams[sname]
                s = S_["s"]; n = S_["n"]; ntile = S_["nt"]; R = S_["R"]; Rb = S_["Rb"]; T = S_["T"]
                nsub = n // 128
                if sname == "c":
                    OP("dve", lambda e: e.memset(Sst[:], 0.0), writes=[Sst])
                only_states = (sname == "c" and last_layer)
                for i in range(ntile):
                    t0 = i * n
                    DMA("sp", xt[:, :, :n], rsview(R, t0, n), reads=[Rb[i]], writes=[xt])
                    norm_mod(n, A1, modv, 0, s, hT, xt, tmp8, sq8)
                    for j in range(nsub):
                        pv = nps()
                        for k in range(KC):
                            OP("pe", lambda e, k=k, j=j, pv=pv: e.matmul(pv[:, :], lhsT=hT[:, k, j * 128:(j + 1) * 128], rhs=winb[:, k, 0:512],
                                                                         start=(k == 0), stop=(k == KC - 1)), reads=[hT, winb], writes=[pv])
                        OP("act", lambda e, j=j, pv=pv: e.copy(out=vtm[:, j, :], in_=pv[:, :]), reads=[pv], writes=[vtm])
                    DMA("sp", VS[t0:t0 + n, :].rearrange("(j p) c -> p j c", p=128), vtm[:, :nsub, :], reads=[vtm], writes=[scr_b["VS"][i]])

                    def proj(cc):
                        ps = nps()
                        for k in range(KC):
                            OP("pe", lambda e, k=k, ps=ps: e.matmul(ps[:, :n], lhsT=winb[:, k, cc * 128:(cc + 1) * 128], rhs=hT[:, k, :n],
                                                                    start=(k == 0), stop=(k == KC - 1)), reads=[hT, winb], writes=[ps])
                        return ps
                    for h in range(4):
                        ps = proj(8 + h)
                        OP("act", lambda e, h=h, ps=ps: e.copy(out=stg[:, h, :n], in_=ps[:, :n]), reads=[ps], writes=[stg])
                    DMA("sp", s4view(ZB, t0, n), stg[:, :, :n], reads=[stg], writes=[scr_b["ZB"][i]])
                    for h in range(4):
                        psq = proj(12 + h)
                        OP("act", lambda e, h=h, psq=psq: e.copy(out=ql[:, h, :n], in_=psq[:, :n]), reads=[psq], writes=[ql])
                        psz = proj(4 + h)
                        zps_t[0] = psz
                        last = gate_math(psz[:, :n], ql[:, h, :n], ql, l, 0, h, n, False)
                        recur(l, 0, h, n, False, last, osb)
                    DMA("sp", s4view(QS, t0, n), ql[:, :, :n], reads=[ql], writes=[scr_b["QS"][i]])
                    DMA("sp", s4view(OF, t0, n), osb[:, :, :n], reads=[osb], writes=[scr_b["OF"][i]])
                    if only_states:
                        continue
                    for h in range(4):
                        ps = proj(16 + h)
                        OP("act", lambda e, h=h, ps=ps: e.activation(out=stg[:, h, :n], in_=ps[:, :n], func=AF.Silu), reads=[ps], writes=[stg])
                    DMA("sp", s4view(SG, t0, n), stg[:, :, :n], reads=[stg], writes=[scr_b["SG"][i]])
                    for c in range(4):
                        pc = proj(20 + c)
                        OP("act", lambda e, pc=pc: e.copy(out=cva[:, :n], in_=pc[:, :n]), reads=[pc], writes=[cva])
                        ph = proj(28 + c)
                        OP("dve", lambda e, c=c, ph=ph: e.tensor_tensor(out=stg[:, c, :n], in0=ph[:, :n], in1=cva[:, :n], op=ALU.mult),
                           reads=[ph, cva], writes=[stg])
                    DMA("sp", s4view(US, t0, n), stg[:, :, :n], reads=[stg], writes=[scr_b["US"][i]])
                    for c in range(4):
                        pb = proj(24 + c)
                        OP("act", lambda e, c=c, pb=pb: e.copy(out=stg[:, c, :n], in_=pb[:, :n]), reads=[pb], writes=[stg])
                    DMA("sp", s4view(BG, t0, n), stg[:, :, :n], reads=[stg], writes=[scr_b["BG"][i]])
                for i in range(ntile - 1, -1, -1):
                    t0 = i * n
                    DMA("sp", zbl[:, :, :n], s4view(ZB, t0, n), reads=[scr_b["ZB"][i]], writes=[zbl])
                    DMA("sp", ql[:, :, :n], s4view(QS, t0, n), reads=[scr_b["QS"][i]], writes=[ql])
                    DMA("sp", vtm[:, :nsub, :], VS[t0:t0 + n, :].rearrange("(j p) c -> p j c", p=128), reads=[scr_b["VS"][i]], writes=[vtm])
                    for h in range(4):
                        zps_t[0] = zbl
                        last = gate_math(zbl[:, h, :n], ql[:, h, :n], ql, l, 1, h, n, True)
                        recur(l, 1, h, n, True, last, osb)
                    if only_states:
                        continue
                    DMA("sp", ofl[:, :, :n], s4view(OF, t0, n), reads=[scr_b["OF"][i]], writes=[ofl])
                    DMA("sp", sgl[:, :, :n], s4view(SG, t0, n), reads=[scr_b["SG"][i]], writes=[sgl])
                    DMA("sp", bgl[:, :, :n], s4view(BG, t0, n), reads=[scr_b["BG"][i]], writes=[bgl])
                    OP("pool", lambda e: e.memset(uh[:], 0.0), writes=[uh])
                    lo = 64 if t0 > 0 else 0
                    hi = 64 if t0 + n < T else 0
                    rd = [scr_b["US"][i]] + ([scr_b["US"][i - 1]] if lo else []) + ([scr_b["US"][i + 1]] if hi else [])
                    DMA("sp", uh[:, :, 64 - lo:64 + n + hi], s4view(US, t0 - lo, n + lo + hi), reads=rd, writes=[uh])
                    DMA("sp", xt[:, :, :n], rsview(R, t0, n), reads=[Rb[i]], writes=[xt])
                    OP("dve", lambda e: e.tensor_tensor(out=osb[:, :, :n], in0=osb[:, :, :n], in1=ofl[:, :, :n], op=ALU.add), reads=[osb, ofl], writes=[osb])
                    OP("act", lambda e: e.activation(out=sq8[:, 0:4, :n], in_=osb[:, :, :n], func=AF.Square), reads=[osb], writes=[sq8])
                    for h in range(4):
                        ps = nps()
                        OP("pe", lambda e, h=h, ps=ps: e.matmul(ps[:, :n], lhsT=ones[:], rhs=sq8[:, h, :n], start=True, stop=True), reads=[ones, sq8], writes=[ps])
                        OP("act", lambda e, ps=ps: e.activation(out=rstd[:, :n], in_=ps[:, :n], func=AF.Sqrt, scale=1.0 / 128, bias=epsc[:]),
                           reads=[ps, epsc], writes=[rstd])
                        OP("dve", lambda e: e.reciprocal(out=rstd[:, :n], in_=rstd[:, :n]), reads=[rstd], writes=[rstd])
                        OP("dve", lambda e, h=h: e.tensor_tensor(out=osb[:, h, :n], in0=osb[:, h, :n], in1=rstd[:, :n], op=ALU.mult), reads=[osb, rstd], writes=[osb])
                        OP("dve", lambda e, h=h: e.tensor_tensor(out=mix[:, h, :n], in0=osb[:, h, :n], in1=sgl[:, h, :n], op=ALU.mult), reads=[osb, sgl], writes=[mix])
                    for c in range(4):
                        ctr = uh[:, c, 64:64 + n]
                        OP("dve", lambda e, c=c, ctr=ctr: e.tensor_scalar(out=cva[:, :n], in0=ctr, scalar1=cwt[:, c, 1:2], scalar2=None, op0=ALU.mult),
                           reads=[uh, cwt], writes=[cva])
                        if sname == "x" and c >= 2:
                            OP("dve", lambda e, c=c: e.scalar_tensor_tensor(out=cva[:, :n], in0=uh[:, c, 0:n], scalar=cwt[:, c, 0:1], in1=cva[:, :n],
                                                                            op0=ALU.mult, op1=ALU.add), reads=[uh, cwt, cva], writes=[cva])
                            OP("dve", lambda e, c=c: e.scalar_tensor_tensor(out=cva[:, :n], in0=uh[:, c, 128:128 + n], scalar=cwt[:, c, 2:3], in1=cva[:, :n],
                                                                            op0=ALU.mult, op1=ALU.add), reads=[uh, cwt, cva], writes=[cva])
                        elif sname == "x":
                            u3 = uh[:, c, 64:64 + n].rearrange("p (r w) -> p r w", w=64)
                            c3 = cva[:, :n].rearrange("p (r w) -> p r w", w=64)
                            OP("dve", lambda e, c=c, u3=u3, c3=c3: e.scalar_tensor_tensor(out=c3[:, :, 1:64], in0=u3[:, :, 0:63], scalar=cwt[:, c, 0:1], in1=c3[:, :, 1:64],
                                                                                        op0=ALU.mult, op1=ALU.add), reads=[uh, cwt, cva], writes=[cva])
                            OP("dve", lambda e, c=c, u3=u3, c3=c3: e.scalar_tensor_tensor(out=c3[:, :, 0:63], in0=u3[:, :, 1:64], scalar=cwt[:, c, 2:3], in1=c3[:, :, 0:63],
                                                                                        op0=ALU.mult, op1=ALU.add), reads=[uh, cwt, cva], writes=[cva])
                        else:
                            OP("dve", lambda e, c=c: e.scalar_tensor_tensor(out=cva[:, :n], in0=uh[:, c, 63:63 + n], scalar=cwt[:, c, 0:1], in1=cva[:, :n],
                                                                            op0=ALU.mult, op1=ALU.add), reads=[uh, cwt, cva], writes=[cva])
                            OP("dve", lambda e, c=c: e.scalar_tensor_tensor(out=cva[:, :n], in0=uh[:, c, 65:65 + n], scalar=cwt[:, c, 2:3], in1=cva[:, :n],
                                                                            op0=ALU.mult, op1=ALU.add), reads=[uh, cwt, cva], writes=[cva])
                        OP("dve", lambda e, c=c: e.tensor_tensor(out=mix[:, 4 + c, :n], in0=cva[:, :n], in1=bgl[:, c, :n], op=ALU.mult), reads=[cva, bgl], writes=[mix])
                    for m in range(KC):
                        ps = nps()
                        for fch in range(KC):
                            OP("pe", lambda e, m=m, fch=fch, ps=ps: e.matmul(ps[:, :n], lhsT=woutb[:, fch, m * 128:(m + 1) * 128], rhs=mix[:, fch, :n],
                                                                             start=(fch == 0), stop=(fch == KC - 1)), reads=[woutb, mix], writes=[ps])
                        OP("dve", lambda e, m=m, ps=ps: e.scalar_tensor_tensor(out=xt[:, m, :n], in0=ps[:, :n], scalar=modv[:, 16 + m, s:s + 1], in1=xt[:, m, :n],
                                                                               op0=ALU.mult, op1=ALU.add), reads=[ps, modv, xt], writes=[xt])
                    DMA("sp", rsview(R, t0, n), xt[:, :, :n], reads=[xt], writes=[Rb[i]])
            fw.barrier()
            mst.close()
            if stage == 1:
                break
            peer_layer(l, last_layer)
            if stage == 2:
                break

        if stage in (1, 2):
            xt = sb("xo", [128, KC, NT])
            for i in range(seq // NT):
                DMA("sp", xt[:, :, :NT], rsview(XR, i * NT, NT), reads=[XRb[i]], writes=[xt])
                DMA("sp", rsview(yT_out, i * NT, NT), xt[:, :, :NT], reads=[xt], writes=[dbuf("y")])
            if dbg:
                DMA("sp", xt[:, :, :ctxlen], rsview(CR, 0, ctxlen), reads=[CRb[0]], writes=[xt])
                DMA("sp", rsview(dbgc_out, 0, ctxlen), xt[:, :, :ctxlen], reads=[xt], writes=[dbuf("y2")])
        fw.finish()
        print("instructions:", fw.ninst, {k: v.count for k, v in fw.E.items()})
    return nc


def make_consts():
    bf = ml_dtypes.bfloat16
    ident = np.eye(128, dtype=np.float32).astype(bf)
    ones = np.ones((128, 128), np.float32).astype(bf)
    s = np.arange(128)[:, None]; t = np.arange(128)[None, :]
    same = (s // CH) == (t // CH)
    maskF = (same & (s <= t)).astype(np.float32).astype(bf)
    maskB = (same & (s >= t)).astype(np.float32).astype(bf)
    resetm = np.ones((128, NT), np.float32); resetm[:, ::CH] = 0.0
    rowm = (np.arange(128)[:, None] // CH == np.arange(4)[None, :]).astype(np.float32)
    return dict(identF=np.eye(128, dtype=np.float32), ident=ident, ones=ones, maskF=maskF, maskB=maskB, resetm=resetm, rowm=rowm)


def prep_shared(inp, depth):
    f = lambda a: np.ascontiguousarray(np.asarray(a, dtype=np.float32))
    sh = {}
    sh["w_mod"] = f(inp["w_mod"])
    sh["b_mod"] = f(np.asarray(inp["b_mod"]).reshape(depth, 48, 128).transpose(0, 2, 1))
    sh["n1"] = f(np.asarray(inp["norm1_g"]).reshape(depth, KC, 128).transpose(0, 2, 1))
    sh["n2"] = f(np.asarray(inp["norm2_g"]).reshape(depth, KC, 128).transpose(0, 2, 1))
    sh["fg"] = f(np.repeat(np.asarray(inp["final_g"]).reshape(KC, 128).T[:, :, None], 2, axis=2))
    sh["w_in"] = f(inp["w_in"]); sh["w_out"] = f(inp["w_out"])
    sh["cw"] = f(np.asarray(inp["conv_w"]).reshape(depth, 3, 4, 128).transpose(0, 3, 2, 1))
    sh["lbl"] = f(np.asarray(inp["lb_logits"]).reshape(depth, 2, 4, 128).transpose(3, 0, 1, 2))
    sh["wq"] = f(inp["peer_wq"])
    sh["skT"] = f(np.asarray(inp["peer_subkeys"]).reshape(depth, 16, 128, 128).transpose(0, 3, 1, 2))
    sh["uT"] = f(np.asarray(inp["peer_u"]).transpose(0, 2, 1))
    sh["v"] = f(inp["peer_v"])
    sh.update(make_consts())
    return sh


def prep_core(inp, b):
    f = lambda a: np.ascontiguousarray(np.asarray(a, dtype=np.float32))
    m = {}
    m["xT"] = f(np.asarray(inp["x"])[b].T)
    m["cxT"] = f(np.asarray(inp["ctx"])[b].T)
    cv = np.stack([np.asarray(inp["c"])[b], np.asarray(inp["c_ctx"])], axis=-1)
    m["cv"] = f(cv.reshape(KC, 128, 2).transpose(1, 0, 2))
    return m


def kernel(**inp):
    x = np.asarray(inp["x"])
    B, seq, _ = x.shape
    ctxlen = np.asarray(inp["ctx"]).shape[1]
    depth = np.asarray(inp["w_in"]).shape[0]
    nc = build(depth, seq, ctxlen)
    sh = prep_shared(inp, depth)
    in_maps = []
    for b in range(B):
        m = dict(sh); m.update(prep_core(inp, b)); in_maps.append(m)
    res = run_bass_kernel_spmd(nc, in_maps, core_ids=list(range(B)))
    out = np.stack([np.ascontiguousarray(res.results[b]["yT"].T) for b in range(B)], axis=0)
    return out.astype(np.float32)
```

```python
import numpy as np
import ml_dtypes
from contextlib import ExitStack
import concourse.bass as bass
import concourse.mybir as mybir
from concourse.bass_utils import run_bass_kernel_spmd

F32 = mybir.dt.float32
BF16 = mybir.dt.bfloat16
AF = mybir.ActivationFunctionType
ALU = mybir.AluOpType
AX = mybir.AxisListType

D = 1024
KC = 8
NT = 256
NP = 256
CH = 32
EPS = 1e-6
EPOCH = 30000
NDMA = 12
NEXP = 16384


class Tok:
    __slots__ = ("sem", "val", "eng")

    def __init__(self, sem, val, eng):
        self.sem = sem; self.val = val; self.eng = eng


class Buf:
    def __init__(self, name=""):
        self.name = name; self.w = None; self.r = {}


class Eng:
    def __init__(self, name, obj):
        self.name = name; self.obj = obj
        self.sems = []; self.count = 0; self.seen = {}
        self.dma_sems = []; self.dma_n = 0


class FW:
    def __init__(self, nc, stack):
        self.nc = nc; self.stack = stack
        self.E = {n: Eng(n, getattr(nc, o)) for n, o in
                  [("pe", "tensor"), ("dve", "vector"), ("act", "scalar"), ("pool", "gpsimd"), ("sp", "sync")]}
        self.ninst = 0

    def newsem(self, name):
        return self.stack.enter_context(self.nc.semaphore(name))

    def _wait(self, e, tok):
        if tok is None:
            return
        k = id(tok.sem)
        if e.seen.get(k, 0) >= tok.val:
            return
        e.obj.wait_ge(tok.sem, tok.val)
        e.seen[k] = tok.val

    def _deps(self, e, reads, writes):
        for b in reads:
            if b.w is not None:
                self._wait(e, b.w)
        for b in writes:
            if b.w is not None and b.w.eng != e.name:
                self._wait(e, b.w)
            for en, t in b.r.items():
                if en != e.name:
                    self._wait(e, t)

    def _commit(self, tok, reads, writes, rkey):
        for b in reads:
            b.r[rkey] = tok
        for b in writes:
            b.w = tok; b.r = {}

    def op(self, eng, fn, reads=(), writes=()):
        e = self.E[eng]
        self._deps(e, reads, writes)
        ep = e.count // EPOCH
        while len(e.sems) <= ep:
            e.sems.append(self.newsem(f"s_{eng}_{len(e.sems)}"))
        ins = fn(e.obj)
        val = e.count - ep * EPOCH + 1
        ins.then_inc(e.sems[ep], 1)
        e.count += 1
        self.ninst += 1
        tok = Tok(e.sems[ep], val, eng)
        self._commit(tok, reads, writes, eng)
        return tok

    def dma(self, eng, out, in_, reads=(), writes=(), **kw):
        e = self.E[eng]
        self._deps(e, reads, writes)
        j = e.dma_n % NDMA
        if len(e.dma_sems) <= j:
            e.dma_sems.append([self.newsem(f"d_{eng}_{j}"), 0])
        slot = e.dma_sems[j]
        if slot[1] > 0:
            self._wait(e, Tok(slot[0], slot[1], "dma"))
        slot[1] += 16
        e.obj.dma_start(out=out, in_=in_, **kw).then_inc(slot[0], 16)
        e.dma_n += 1
        self.ninst += 1
        tok = Tok(slot[0], slot[1], "dma")
        self._commit(tok, reads, writes, f"dma_{eng}_{j}")
        return tok

    def raw(self, eng, fn, reads=(), writes=()):
        e = self.E[eng]
        self._deps(e, reads, writes)
        return fn(e.obj)

    def collective(self, kind, ins, outs, groups, reads=(), writes=()):
        e = self.E["pool"]
        self._deps(e, reads, writes)
        if not hasattr(self, "cc_sem"):
            self.cc_sem = self.newsem("cc_sem"); self.cc_n = 0
        self.cc_n += 1
        e.obj.collective_compute(kind, mybir.AluOpType.bypass, replica_groups=groups, ins=ins, outs=outs).then_inc(self.cc_sem, 1)
        tok = Tok(self.cc_sem, self.cc_n, "cc")
        self._commit(tok, reads, writes, "cc")
        return tok

    def barrier(self):
        toks = []
        for en in self.E.values():
            if en.count > 0:
                ep = (en.count - 1) // EPOCH
                toks.append(Tok(en.sems[ep], en.count - ep * EPOCH, en.name))
            for slot in en.dma_sems:
                if slot[1] > 0:
                    toks.append(Tok(slot[0], slot[1], "dma"))
        if hasattr(self, "cc_sem") and self.cc_n > 0:
            toks.append(Tok(self.cc_sem, self.cc_n, "cc"))
        for e in self.E.values():
            for t in toks:
                if t.eng != e.name:
                    self._wait(e, t)

    def finish(self):
        e = self.E["sp"]
        for en in self.E.values():
            for slot in en.dma_sems:
                if slot[1] > 0:
                    self._wait(e, Tok(slot[0], slot[1], "dma"))


class Tile:
    def __init__(self, t, name):
        self.t = t; self.b = Buf(name)

    def __getitem__(self, k):
        return self.t[k]


def build(depth, seq, ctxlen, stage=99, dbg=False, pair=True, ncores=8):
    nc = bass.Bass("TRN2", target_bir_lowering=False)
    assert seq % NT == 0 and ctxlen <= NT and ctxlen % 128 == 0 and seq % NP == 0
    rows_per_tile = NT // 64

    def din(name, shape, dt=F32):
        return nc.dram_tensor(name, list(shape), dt, kind="ExternalInput").ap()

    def dscr(name, shape, dt=F32):
        return nc.dram_tensor(name, list(shape), dt).ap()

    xT_in = din("xT", [D, seq]); cxT_in = din("cxT", [D, ctxlen])
    cv_in = din("cv", [128, KC, 2])
    wmod_in = din("w_mod", [depth, D, 6 * D]); bmod_in = din("b_mod", [depth, 128, 48])
    n1_in = din("n1", [depth, 128, KC]); n2_in = din("n2", [depth, 128, KC]); fg_in = din("fg", [128, KC, 2])
    win_in = din("w_in", [depth, D, 4096]); wout_in = din("w_out", [depth, D, D])
    cw_in = din("cw", [depth, 128, 4, 3]); lbl_in = din("lbl", [128, depth, 2, 4])
    wq_in = din("wq", [depth, D, 2048]); skT_in = din("skT", [depth, 128, 16, 128])
    uT_in = din("uT", [depth, D, NEXP]); v_in = din("v", [depth, NEXP, D])
    ident_in = din("ident", [128, 128], BF16); ones_in = din("ones", [128, 128], BF16)
    maskF_in = din("maskF", [128, 128], BF16); maskB_in = din("maskB", [128, 128], BF16)
    identF_in = din("identF", [128, 128]); reset_in = din("resetm", [128, NT]); rowm_in = din("rowm", [128, 4])
    half = seq // 2 if pair else seq
    I32 = mybir.dt.int32
    par_in = din("par", [1, 1], I32)
    yT_out = nc.dram_tensor("yT", [D, half if stage == 99 else seq], F32, kind="ExternalOutput").ap()
    dbg_out = nc.dram_tensor("dbg", [D, seq], F32, kind="ExternalOutput").ap() if dbg else None
    dbgc_out = nc.dram_tensor("dbgc", [D, ctxlen], F32, kind="ExternalOutput").ap() if dbg else None

    TM = max(seq, ctxlen)
    XRh = nc.dram_tensor("XR", [D, seq], F32); XR = XRh.ap(); CR = dscr("CR", [D, ctxlen])
    CW = min(512, half); NCW = half // CW
    HXs = [dscr(f"HX{j}", [D, CW]) for j in range(NCW)]; HGs = [dscr(f"HG{j}", [2 * D, CW]) for j in range(NCW)]
    HXb = [Buf(f"HX{j}") for j in range(NCW)]; HGb = [Buf(f"HG{j}") for j in range(NCW)]
    ZB = dscr("ZB", [512, TM]); QS = dscr("QS", [512, TM]); SG = dscr("SG", [512, TM])
    OF = dscr("OF", [512, TM]); US = dscr("US", [512, TM]); BG = dscr("BG", [512, TM])
    VS = dscr("VS", [TM, 512], BF16)
    U16 = dscr("U16", [NEXP // 512, 128, KC, 512], BF16); V16 = dscr("V16", [NEXP, D], BF16)
    U16b = Buf("U16"); V16b = Buf("V16")

    with ExitStack() as st:
        fw = FW(nc, st)

        def sb(name, shape, dt=F32):
            return Tile(st.enter_context(nc.sbuf_tensor("sb_" + name, list(shape), dt)), name)

        def dbuf(name):
            return Buf(name)

        PS = {"F": [], "B": []}
        cnt = {"f": 0, "b": 0}

        def alloc_psum(stack, tag, nf, nb):
            PS["F"] = [Tile(stack.enter_context(nc.psum_tensor(f"psF{tag}_{i}", [128, 512], F32)), f"psF{i}") for i in range(nf)]
            PS["B"] = [Tile(stack.enter_context(nc.psum_tensor(f"psB{tag}_{i}", [128, 1024], BF16)), f"psB{i}") for i in range(nb)]

        def nps():
            cnt["f"] += 1
            return PS["F"][cnt["f"] % cnt["n"]]

        def npsb():
            cnt["b"] += 1
            return PS["B"][cnt["b"] % 2]

        def OP(eng, fn, reads=(), writes=()):
            return fw.op(eng, fn, reads=[x.b if hasattr(x, "b") else x for x in reads],
                         writes=[x.b if hasattr(x, "b") else x for x in writes])

        def DMA(eng, out, in_, reads=(), writes=(), **kw):
            return fw.dma(eng, out, in_, reads=[x.b if hasattr(x, "b") else x for x in reads],
                          writes=[x.b if hasattr(x, "b") else x for x in writes], **kw)

        ident = sb("ident", [128, 128], BF16); ones = sb("ones", [128, 128], BF16)
        maskF = sb("maskF", [128, 128], BF16); maskB = sb("maskB", [128, 128], BF16)
        resetm = sb("resetm", [128, NT]); rowm = sb("rowm", [128, 4])
        DMA("sp", rowm[:], rowm_in[:, :], writes=[rowm])
        for tl, src in [(ident, ident_in), (ones, ones_in), (maskF, maskF_in), (maskB, maskB_in), (resetm, reset_in)]:
            DMA("sp", tl[:], src[:, :], writes=[tl])
        part = Tile(st.enter_context(nc.sbuf_tensor("sb_part", [1, 1], I32)), "part")
        preg = st.enter_context(nc.sync.register("preg"))
        DMA("sp", part[:], par_in[:, :], writes=[part])
        fw.raw("sp", lambda e: e.reg_load(preg, part[:1, :1]), reads=[part.b])
        pval = nc.sync.snap(preg)
        XRdyn = bass.AP(XRh, pval, [[seq, 128], [128 * seq, KC], [1, half]])
        cond = sb("cond", [128, KC, 2])
        DMA("sp", cond[:], cv_in[:, :, :], writes=[cond])
        OP("act", lambda e: e.activation(out=cond[:], in_=cond[:], func=AF.Silu), reads=[cond], writes=[cond])
        epsc = sb("epsc", [128, 1])
        OP("dve", lambda e: e.memset(epsc[:], EPS), writes=[epsc])
        lbe = sb("lbe", [128, depth, 8]); lbs = sb("lbs", [128, 8]); lb = sb("lb", [128, depth, 8]); oml = sb("oml", [128, depth, 8])
        DMA("sp", lbe[:], lbl_in.rearrange("p l a b -> p l (a b)"), writes=[lbe])
        OP("act", lambda e: e.activation(out=lbe[:], in_=lbe[:], func=AF.Exp), reads=[lbe], writes=[lbe])
        OP("dve", lambda e: e.tensor_copy(out=lbs[:], in_=lbe[:, 0, :]), reads=[lbe], writes=[lbs])
        for l in range(1, depth):
            OP("dve", lambda e, l=l: e.tensor_tensor(out=lbs[:], in0=lbs[:], in1=lbe[:, l, :], op=ALU.add), reads=[lbs, lbe], writes=[lbs])
        OP("dve", lambda e: e.reciprocal(out=lbs[:], in_=lbs[:]), reads=[lbs], writes=[lbs])
        OP("dve", lambda e: e.memset(lb[:, 0, :], 0.0), writes=[lb])
        for l in range(1, depth):
            OP("dve", lambda e, l=l: e.tensor_tensor(out=lbe[:, l, :], in0=lbe[:, l, :], in1=lbs[:], op=ALU.mult), reads=[lbe, lbs], writes=[lbe])
            OP("dve", lambda e, l=l: e.tensor_tensor(out=lb[:, l, :], in0=lb[:, l - 1, :], in1=lbe[:, l, :], op=ALU.add), reads=[lb, lbe], writes=[lb])
        OP("dve", lambda e: e.tensor_scalar(out=oml[:], in0=lb[:], scalar1=-1.0, scalar2=1.0, op0=ALU.mult, op1=ALU.add), reads=[lb], writes=[oml])

        XRb = [dbuf(f"XR{i}") for i in range(seq // NT)]
        CRb = [dbuf("CR")]
        for i in range(seq // NT):
            DMA("sp", XR[:, i * NT:(i + 1) * NT], xT_in[:, i * NT:(i + 1) * NT], writes=[XRb[i]])
        DMA("sp", CR[:, :], cxT_in[:, :], writes=[CRb[0]])

        modv = sb("modv", [128, 48, 2])
        A1 = sb("A1", [128, KC, 2]); A2 = sb("A2", [128, KC, 2])
        n1t = sb("n1t", [128, KC]); n2t = sb("n2t", [128, KC]); bmt = sb("bmt", [128, 48]); cwt = sb("cwt", [128, 4, 3])
        fgt = sb("fgt", [128, KC, 2]); zerob = sb("zerob", [128, KC, 2])
        DMA("sp", fgt[:], fg_in[:, :, :], writes=[fgt])
        OP("dve", lambda e: e.memset(zerob[:], 0.0), writes=[zerob])
        identF = sb("identF", [128, 128])
        DMA("sp", identF[:], identF_in[:, :], writes=[identF])
        Sst = sb("Sst", [128, 8, 128])
        rstd = sb("rstd", [128, NT])

        streams = {
            "c": dict(T=ctxlen, R=CR, Rb=CRb, s=1, nt=1, n=ctxlen),
            "x": dict(T=seq, R=XR, Rb=XRb, s=0, nt=seq // NT, n=NT),
        }
        scr_b = {nm: [dbuf(f"{nm}{i}") for i in range(TM // min(NT, ctxlen) + 1)] for nm in ["ZB", "QS", "SG", "OF", "US", "BG", "VS"]}

        def rsview(R, t0, n):
            return R.rearrange("(k p) t -> p k t", p=128)[:, :, t0:t0 + n]

        def s4view(S, t0, n):
            return S.rearrange("(k p) t -> p k t", p=128)[:, :, t0:t0 + n]

        def norm_mod(n, A, Bt, Bm_col0, s, out_bf, xin, tmp8, sq8):
            OP("act", lambda e: e.activation(out=sq8[:, :, :n], in_=xin[:, :, :n], func=AF.Square), reads=[xin], writes=[sq8])
            ps = nps()
            for k in range(KC):
                OP("pe", lambda e, k=k: e.matmul(ps[:, :n], lhsT=ones[:], rhs=sq8[:, k, :n], start=(k == 0), stop=(k == KC - 1)),
                   reads=[ones, sq8], writes=[ps])
            OP("act", lambda e: e.activation(out=rstd[:, :n], in_=ps[:, :n], func=AF.Sqrt, scale=1.0 / D, bias=epsc[:]), reads=[ps, epsc], writes=[rstd])
            OP("dve", lambda e: e.reciprocal(out=rstd[:, :n], in_=rstd[:, :n]), reads=[rstd], writes=[rstd])
            OP("dve", lambda e: e.tensor_tensor(out=tmp8[:, :, :n], in0=xin[:, :, :n],
                                                 in1=rstd[:, :n].unsqueeze(1).to_broadcast([128, KC, n]), op=ALU.mult),
               reads=[xin, rstd], writes=[tmp8])
            for k in range(KC):
                OP("act", lambda e, k=k: e.activation(out=out_bf[:, k, :n], in_=tmp8[:, k, :n], func=AF.Identity,
                                                       scale=A[:, k, s:s + 1], bias=Bt[:, Bm_col0 + k, s:s + 1]),
                   reads=[tmp8, A, Bt], writes=[out_bf])

        def gate_math(zps, qsrc_ap, qsrc_t, l, d, h, n, bwd):
            nchk = n // CH
            sg, f, key, lf, b, bc, e1, e2, e3, e4 = (W[x] for x in ["sg", "f", "key", "lf", "b", "bc", "e1", "e2", "e3", "e4"])
            col = d * 4 + h
            OP("act", lambda e: e.activation(out=sg[:, :n], in_=zps, func=AF.Sigmoid), reads=[zps_t[0]], writes=[sg])
            OP("dve", lambda e: e.tensor_scalar(out=f[:, :n], in0=sg[:, :n], scalar1=oml[:, l, col:col + 1], scalar2=lb[:, l, col:col + 1],
                                                 op0=ALU.mult, op1=ALU.add), reads=[sg, oml, lb], writes=[f])
            OP("pool", lambda e: e.tensor_scalar(out=f[:, :n], in0=f[:, :n], scalar1=1e-20, scalar2=None, op0=ALU.max), reads=[f], writes=[f])
            OP("pool", lambda e: e.tensor_scalar(out=key[:, :n], in0=f[:, :n], scalar1=-1.0, scalar2=1.0, op0=ALU.mult, op1=ALU.add),
               reads=[f], writes=[key])
            OP("act", lambda e: e.activation(out=lf[:, :n], in_=f[:, :n], func=AF.Ln), reads=[f], writes=[lf])
            OP("dve", lambda e: e.tensor_tensor_scan(out=b[:, :n], data0=resetm[:, :n], data1=lf[:, :n], initial=0.0, op0=ALU.mult, op1=ALU.add),
               reads=[resetm, lf], writes=[b])
            b3 = b[:, :n].rearrange("p (c t) -> p c t", t=CH)
            if bwd:
                lf3 = lf[:, :n].rearrange("p (c t) -> p c t", t=CH)
                bc3 = bc[:, :n].rearrange("p (c t) -> p c t", t=CH)
                OP("dve", lambda e: e.tensor_tensor(out=bc3, in0=b3[:, :, CH - 1:CH].to_broadcast([128, nchk, CH]), in1=b3, op=ALU.subtract),
                   reads=[b], writes=[bc])
                OP("dve", lambda e: e.tensor_tensor(out=b[:, :n], in0=bc[:, :n], in1=lf[:, :n], op=ALU.add), reads=[bc, lf], writes=[b])
                mid = CH // 2; last = 0
            else:
                mid = CH // 2 - 1; last = CH - 1
            bc3 = bc[:, :n].rearrange("p (c t) -> p c t", t=CH)
            OP("dve", lambda e: e.tensor_tensor(out=bc3, in0=b3, in1=b3[:, :, mid:mid + 1].to_broadcast([128, nchk, CH]), op=ALU.subtract),
               reads=[b], writes=[bc])
            OP("act", lambda e: e.activation(out=e1[:, :n], in_=bc[:, :n], func=AF.Exp), reads=[bc], writes=[e1])
            OP("act", lambda e: e.activation(out=e2[:, :n], in_=bc[:, :n], func=AF.Exp, scale=-1.0), reads=[bc], writes=[e2])
            OP("act", lambda e: e.activation(out=e3[:, :n], in_=b[:, :n], func=AF.Exp), reads=[b], writes=[e3])
            OP("dve", lambda e: e.tensor_tensor(out=bc3, in0=b3[:, :, last:last + 1].to_broadcast([128, nchk, CH]), in1=b3, op=ALU.subtract),
               reads=[b, e1, e2], writes=[bc])
            OP("act", lambda e: e.activation(out=e4[:, :n], in_=bc[:, :n], func=AF.Exp), reads=[bc], writes=[e4])
            OP("dve", lambda e: e.tensor_tensor(out=qp[:, :n], in0=qsrc_ap, in1=e1[:, :n], op=ALU.mult), reads=[qsrc_t, e1], writes=[qp])
            OP("pool", lambda e: e.tensor_tensor(out=qin[:, :n], in0=qsrc_ap, in1=e3[:, :n], op=ALU.mult), reads=[qsrc_t, e3], writes=[qin])
            OP("dve", lambda e: e.tensor_tensor(out=kp[:, :n], in0=key[:, :n], in1=e2[:, :n], op=ALU.mult), reads=[key, e2], writes=[kp])
            OP("pool", lambda e: e.tensor_tensor(out=kout[:, :n], in0=key[:, :n], in1=e4[:, :n], op=ALU.mult), reads=[key, e4], writes=[kout])
            return last

        zps_t = [None]

        def recur(l, d, h, n, bwd, last, o_dst):
            si = d * 4 + h
            e3 = W["e3"]
            nsub = n // 128
            subs = range(nsub - 1, -1, -1) if bwd else range(nsub)
            mask = maskB if bwd else maskF
            for j in subs:
                js = slice(j * 128, (j + 1) * 128)
                pt = npsb()
                OP("pe", lambda e: e.transpose(out=pt[:, 0:128], in_=kout[:, js], identity=ident[:]), reads=[kout, ident], writes=[pt])
                for c4 in range(4):
                    OP("act", lambda e, c4=c4: e.activation(out=kotm[:, c4, :], in_=pt[:, 0:128], func=AF.Identity, scale=rowm[:, c4:c4 + 1]),
                       reads=[pt, rowm], writes=[kotm])
                pa = nps()
                OP("pe", lambda e: e.matmul(pa[:, 0:128], lhsT=kp[:, js], rhs=qp[:, js], start=True, stop=True), reads=[kp, qp], writes=[pa])
                OP("dve", lambda e: e.tensor_tensor(out=attm[:], in0=pa[:, 0:128], in1=mask[:], op=ALU.mult), reads=[pa, mask], writes=[attm])
                po = nps()
                chunks = range(3, -1, -1) if bwd else range(4)
                for ci, c in enumerate(chunks):
                    cs = slice(c * CH, (c + 1) * CH)
                    tcs = slice(j * 128 + c * CH, j * 128 + (c + 1) * CH)
                    sbf = Sbf[(si * 4 + ci) % 8]
                    OP("act", lambda e, sbf=sbf: e.copy(out=sbf[:], in_=Sst[:, si, :]), reads=[Sst], writes=[sbf])
                    OP("pe", lambda e, sbf=sbf, cs=cs, tcs=tcs: e.matmul(po[:, cs], lhsT=sbf[:], rhs=qin[:, tcs], start=True, stop=False),
                       reads=[sbf, qin], writes=[po])
                    OP("pe", lambda e, cs=cs: e.matmul(po[:, cs], lhsT=vtm[:, j, h * 128:(h + 1) * 128], rhs=attm[:, cs], start=False, stop=True),
                       reads=[vtm, attm], writes=[po])
                    pS = nps()
                    OP("pe", lambda e, c=c, pS=pS: e.matmul(pS[:, 0:128], lhsT=kotm[:, c, :], rhs=vtm[:, j, h * 128:(h + 1) * 128], start=True, stop=True),
                       reads=[kotm, vtm], writes=[pS])
                    dcol = j * 128 + c * CH + last
                    OP("dve", lambda e, pS=pS, dcol=dcol: e.scalar_tensor_tensor(out=Sst[:, si, :], in0=Sst[:, si, :], scalar=e3[:, dcol:dcol + 1],
                                                                                 in1=pS[:, 0:128], op0=ALU.mult, op1=ALU.add),
                       reads=[Sst, e3, pS], writes=[Sst])
                OP("act", lambda e: e.copy(out=o_dst[:, h, js], in_=po[:, 0:128]), reads=[po], writes=[o_dst])


        HB = 512
        NHB = NEXP // HB

        def peer_layer(l, last_layer):
            pst = ExitStack()

            def sbp(name, shape, dt=F32):
                return Tile(pst.enter_context(nc.sbuf_tensor(f"p{l}_" + name, list(shape), dt)), name)
            alloc_psum(pst, f"p{l}", 8, 0); cnt["n"] = 6
            po = [PS["F"][6], PS["F"][7]]
            wqb = sbp("wqb", [128, KC, 2048], BF16); skb = sbp("skb", [128, 16, 128], BF16)
            for k in range(KC):
                DMA("pool", wqb[:, k, :], wq_in[l, k * 128:(k + 1) * 128, :], writes=[wqb])
            DMA("pool", skb[:], skT_in[l], writes=[skb])
            uTl = uT_in[l].rearrange("(k p) e -> p k e", p=128)
            for hb in range(NHB):
                DMA("pool", U16[hb], uTl[:, :, hb * HB:(hb + 1) * HB], writes=[U16b])
            for r in range(16):
                DMA("pool", V16[r * 1024:(r + 1) * 1024, :], v_in[l, r * 1024:(r + 1) * 1024, :], writes=[V16b])
            V16v = V16.rearrange("(b q p) d -> b p q d", q=4, p=128)
            xtp = [sbp(f"xt{i}", [128, KC, 128]) for i in range(2)]; hTp = [sbp(f"hT{i}", [128, KC, 128], BF16) for i in range(2)]
            tmp8 = sbp("tmp8", [128, KC, 128]); sq8 = sbp("sq8", [128, KC, 128], BF16)
            qT = sbp("qT", [128, 16, 128], BF16); sc = sbp("sc", [128, 16, 128]); scw = sbp("scw", [128, 128])
            top = sbp("top", [128, 16, 16]); cand = sbp("cand", [128, 8, 256]); cw2 = sbp("cw2", [128, 256]); c24 = sbp("c24", [128, 8, 24])
            tau = sbp("tau", [128, 8]); d16 = sbp("d16", [128, 8, 16]); Zs = sbp("Zs", [128, 8]); beta = sbp("beta", [128, 8])
            sA = sbp("sA", [128, 8, 128])
            NB = 4
            Sblk = [sbp(f"Sblk{i}", [128, 8, 128]) for i in range(NB)]; Exb = [sbp(f"Exb{i}", [128, 8, 128], BF16) for i in range(NB)]
            tmpg = [sbp(f"tmpg{i}", [128, 1024], BF16) for i in range(NB)]
            NR = 4
            ub = [sbp(f"ub{i}", [128, KC, HB], BF16) for i in range(NR)]; vb = [sbp(f"vb{i}", [128, 4, D], BF16) for i in range(NR)]
            gaT = [sbp(f"gaT{i}", [128, 1024]) for i in range(2)]; WT = [sbp(f"WT{i}", [128, 8, 128], BF16) for i in range(2)]
            otm = sbp("otm", [128, D])
            zb16 = sbp("zb16", [128, 128], BF16)
            OP("dve", lambda e: e.memset(zb16[:], 0.0), writes=[zb16])
            tmpo_ap = cand[:].rearrange("p h c -> p (h c)")[:, 0:1024].rearrange("p (k n) -> p k n", n=128)

            class _Alias:
                def __init__(self, ap, tile_):
                    self.ap = ap; self.b = tile_.b

                def __getitem__(self, k):
                    return self.ap[k]
            tmpo = _Alias(tmpo_ap, cand)
            ti = 0
            ybuf = Buf("yout")
            for sname in (["x"] if last_layer else ["c", "x"]):
                S_ = streams[sname]
                s = S_["s"]; R = S_["R"]; Rb = S_["Rb"]; T = S_["T"]; nreg = S_["n"]
                dyn = (sname == "x")
                for i in range((half if dyn else T) // 128):
                    t0 = i * 128
                    xt_ = xtp[ti % 2]; hT_ = hTp[ti % 2]; ti += 1
                    if dyn:
                        DMA("sp", xt_[:, :, :128], XRdyn[:, :, t0:t0 + 128], reads=list(Rb), writes=[xt_])
                    else:
                        rb = Rb[t0 // nreg]
                        DMA("sp", xt_[:, :, :128], rsview(R, t0, 128), reads=[rb], writes=[xt_])
                    norm_mod(128, A2, modv, 24, s, hT_, xt_, tmp8, sq8)
                    for jc in range(16):
                        ps = nps()
                        for k in range(KC):
                            OP("pe", lambda e, k=k, jc=jc, ps=ps: e.matmul(ps[:, :128], lhsT=wqb[:, k, jc * 128:(jc + 1) * 128], rhs=hT_[:, k, :128],
                                                                           start=(k == 0), stop=(k == KC - 1)), reads=[wqb, hT_], writes=[ps])
                        OP("act", lambda e, jc=jc, ps=ps: e.copy(out=qT[:, jc, :], in_=ps[:, :128]), reads=[ps], writes=[qT])
                    for g in range(4):
                        ps = nps()
                        for jj in range(4):
                            jc = g * 4 + jj
                            OP("pe", lambda e, jc=jc, jj=jj, ps=ps: e.matmul(ps[:, jj * 128:(jj + 1) * 128], lhsT=qT[:, jc, :], rhs=skb[:, jc, :], start=True, stop=True),
                               reads=[qT, skb], writes=[ps])
                        OP("act", lambda e, g=g, ps=ps: e.copy(out=sc[:, g * 4:(g + 1) * 4, :], in_=ps[:, :].rearrange("p (a b) -> p a b", b=128)), reads=[ps], writes=[sc])
                    for jc in range(16):
                        OP("dve", lambda e, jc=jc: e.max(out=top[:, jc, 0:8], in_=sc[:, jc, :]), reads=[sc], writes=[top])
                        OP("dve", lambda e, jc=jc: e.match_replace(out=scw[:], in_to_replace=top[:, jc, 0:8], in_values=sc[:, jc, :], imm_value=-1e30),
                           reads=[top, sc], writes=[scw])
                        OP("dve", lambda e, jc=jc: e.max(out=top[:, jc, 8:16], in_=scw[:]), reads=[scw], writes=[top])
                    top4 = top[:].rearrange("p (h two) r -> p h two r", two=2)
                    cand4 = cand[:].rearrange("p h (r s) -> p h r s", s=16)
                    OP("dve", lambda e: e.tensor_tensor(out=cand4, in0=top4[:, :, 0, :].unsqueeze(3).to_broadcast([128, 8, 16, 16]),
                                                         in1=top4[:, :, 1, :].unsqueeze(2).to_broadcast([128, 8, 16, 16]), op=ALU.add), reads=[top], writes=[cand])
                    for h in range(8):
                        OP("dve", lambda e, h=h: e.max(out=c24[:, h, 0:8], in_=cand[:, h, :]), reads=[cand], writes=[c24])
                        OP("dve", lambda e, h=h: e.match_replace(out=cw2[:], in_to_replace=c24[:, h, 0:8], in_values=cand[:, h, :], imm_value=-1e30),
                           reads=[c24, cand], writes=[cw2])
                        OP("dve", lambda e, h=h: e.max(out=c24[:, h, 8:16], in_=cw2[:]), reads=[cw2], writes=[c24])
                        OP("dve", lambda e, h=h: e.match_replace(out=cw2[:], in_to_replace=c24[:, h, 8:16], in_values=cw2[:], imm_value=-1e30),
                           reads=[c24, cw2], writes=[cw2])
                        OP("dve", lambda e, h=h: e.max(out=c24[:, h, 16:24], in_=cw2[:]), reads=[cw2], writes=[c24])
                    OP("dve", lambda e: e.tensor_tensor(out=tau[:], in0=c24[:, :, 15], in1=c24[:, :, 16], op=ALU.add), reads=[c24], writes=[tau])
                    OP("dve", lambda e: e.tensor_scalar(out=tau[:], in0=tau[:], scalar1=0.5, scalar2=None, op0=ALU.mult), reads=[tau], writes=[tau])
                    OP("dve", lambda e: e.tensor_tensor(out=d16[:], in0=c24[:, :, 0:16], in1=c24[:, :, 0:1].to_broadcast([128, 8, 16]), op=ALU.subtract),
                       reads=[c24], writes=[d16])
                    OP("act", lambda e: e.activation(out=d16[:], in_=d16[:], func=AF.Exp), reads=[d16], writes=[d16])
                    OP("dve", lambda e: e.tensor_reduce(out=Zs[:], in_=d16[:], axis=AX.X, op=ALU.add), reads=[d16], writes=[Zs])
                    OP("act", lambda e: e.activation(out=Zs[:], in_=Zs[:], func=AF.Ln), reads=[Zs], writes=[Zs])
                    OP("dve", lambda e: e.tensor_tensor(out=beta[:], in0=tau[:], in1=c24[:, :, 0], op=ALU.subtract), reads=[tau, c24], writes=[beta])
                    OP("dve", lambda e: e.tensor_tensor(out=beta[:], in0=beta[:], in1=Zs[:], op=ALU.subtract), reads=[beta, Zs], writes=[beta])
                    sc4 = sc[:].rearrange("p (h two) k -> p h two k", two=2)
                    OP("dve", lambda e: e.tensor_tensor(out=sA[:], in0=sc4[:, :, 0, :], in1=tau[:].unsqueeze(2).to_broadcast([128, 8, 128]), op=ALU.subtract),
                       reads=[sc, tau], writes=[sA])

                    NBK = NEXP // 1024
                    its = [(bk, h) for bk in range(NBK) for h in range(8)]
                    pg = {}; pa = {}

                    def ldu(hb):
                        DMA("sp", ub[hb % NR][:], U16[hb], reads=[U16b], writes=[ub[hb % NR]])

                    def ldv(hb):
                        DMA("sp", vb[hb % NR][:], V16v[hb], reads=[V16b], writes=[vb[hb % NR]])

                    def blk_start(bk):
                        pa[bk] = (nps(), nps()); pg[bk] = (nps(), nps())
                        for c in range(8):
                            ub_ = ub[(2 * bk + c // 4) % NR]; pp = pa[bk][c // 4]
                            for k in range(KC):
                                OP("pe", lambda e, k=k, c=c, ub_=ub_, pp=pp: e.matmul(pp[:, (c % 4) * 128:(c % 4 + 1) * 128], lhsT=ub_[:, k, (c % 4) * 128:(c % 4 + 1) * 128],
                                                                                     rhs=hT_[:, k, :128], start=(k == 0), stop=(k == KC - 1)),
                                   reads=[ub_, hT_], writes=[pp])
                        for hf in range(2):
                            OP("pe", lambda e, hf=hf: e.matmul(pg[bk][hf][:, :], lhsT=zb16[:], rhs=hT_[:, 0:4, :].rearrange("p a b -> p (a b)"), start=True, stop=False),
                               reads=[zb16, hT_], writes=[pg[bk][hf]])
                        g_ = gaT[bk % 2]
                        for hf in range(2):
                            OP("act", lambda e, hf=hf: e.activation(out=g_[:, hf * 512:(hf + 1) * 512], in_=pa[bk][hf][:, :], func=AF.Gelu),
                               reads=[pa[bk][hf]], writes=[g_])

                    def g1(n_):
                        bk, h = its[n_]
                        a0 = bk * 8
                        sbk = Sblk[n_ % NB]; exk = Exb[n_ % NB]
                        eng = "pool" if (n_ % 4 == 3) else "dve"
                        OP(eng, lambda e: e.tensor_tensor(out=sbk[:], in0=sA[:, h, a0:a0 + 8].unsqueeze(2).to_broadcast([128, 8, 128]),
                                                          in1=sc4[:, h, 1, :].unsqueeze(1).to_broadcast([128, 8, 128]), op=ALU.add),
                           reads=[sA, sc], writes=[sbk])
                        OP("act", lambda e: e.activation(out=exk[:], in_=sbk[:], func=AF.Exp, bias=beta[:, h:h + 1]), reads=[sbk, beta], writes=[exk])

                    def g2(n_):
                        bk, h = its[n_]
                        sbk = Sblk[n_ % NB]; exk = Exb[n_ % NB]; tg = tmpg[n_ % NB]
                        sb2 = sbk[:].rearrange("p a b -> p (a b)"); ex2 = exk[:].rearrange("p a b -> p (a b)")
                        OP("dve", lambda e: e.scalar_tensor_tensor(out=tg[:], in0=sb2, scalar=0.0, in1=ex2, op0=ALU.is_ge, op1=ALU.mult),
                           reads=[sbk, exk], writes=[tg])
                        for c in range(8):
                            pp = pg[bk][c // 4]
                            OP("pe", lambda e, c=c, pp=pp: e.matmul(pp[:, (c % 4) * 128:(c % 4 + 1) * 128], lhsT=tg[:, c * 128:(c + 1) * 128], rhs=ident[:],
                                                                   start=False, stop=(h == 7 and c % 4 == 3)), reads=[tg, ident], writes=[pp])

                    def blk_end(bk):
                        g_ = gaT[bk % 2]; w_ = WT[bk % 2]
                        for hf in range(2):
                            OP("dve", lambda e, hf=hf: e.tensor_tensor(out=w_[:, hf * 4:(hf + 1) * 4, :].rearrange("p a b -> p (a b)"), in0=g_[:, hf * 512:(hf + 1) * 512],
                                                                        in1=pg[bk][hf][:, :], op=ALU.mult), reads=[g_, pg[bk][hf]], writes=[w_])
                        for c in range(8):
                            vb_ = vb[(2 * bk + c // 4) % NR]
                            for hf in range(2):
                                OP("pe", lambda e, c=c, hf=hf, vb_=vb_: e.matmul(po[hf][:, :], lhsT=w_[:, c, :], rhs=vb_[:, c % 4, hf * 512:(hf + 1) * 512],
                                                                                start=(bk == 0 and c == 0), stop=(bk == NBK - 1 and c == 7)),
                                   reads=[w_, vb_], writes=[po[hf]])

                    ldu(0); ldu(1); ldv(0); ldv(1); ldv(2); ldv(3)
                    LAG = 2
                    pending_end = {}
                    for n_ in range(len(its) + LAG + 4):
                        if n_ < len(its):
                            bk, h = its[n_]
                            if h == 0:
                                blk_start(bk)
                                if 2 * bk + 2 < NHB:
                                    ldu(2 * bk + 2); ldu(2 * bk + 3)
                            g1(n_)
                        m_ = n_ - LAG
                        if 0 <= m_ < len(its):
                            g2(m_)
                            if its[m_][1] == 7:
                                pending_end[n_ + 3] = its[m_][0]
                        if n_ in pending_end:
                            bke = pending_end.pop(n_)
                            blk_end(bke)
                            if 2 * (bke + 2) < NHB:
                                ldv(2 * (bke + 2)); ldv(2 * (bke + 2) + 1)
                    for hf in range(2):
                        OP("act", lambda e, hf=hf: e.copy(out=otm[:, hf * 512:(hf + 1) * 512], in_=po[hf][:, :]), reads=[po[hf]], writes=[otm])
                    for m in range(KC):
                        pt = nps()
                        OP("pe", lambda e, m=m, pt=pt: e.transpose(out=pt[:, :128], in_=otm[:, m * 128:(m + 1) * 128], identity=identF[:]), reads=[otm, identF], writes=[pt])
                        OP("dve", lambda e, m=m, pt=pt: e.scalar_tensor_tensor(out=xt_[:, m, :128], in0=pt[:, :128], scalar=modv[:, 40 + m, s:s + 1], in1=xt_[:, m, :128],
                                                                               op0=ALU.mult, op1=ALU.add), reads=[pt, modv, xt_], writes=[xt_])
                    if last_layer and stage == 99:
                        norm_mod(128, fgt, zerob, 0, 0, tmpo, xt_, tmp8, sq8)
                        DMA("sp", rsview(yT_out, t0, 128), tmpo[:], reads=[tmpo], writes=[ybuf])
                    elif dyn:
                        DMA("sp", rsview(HXs[t0 // CW], t0 % CW, 128), xt_[:, :, :128], reads=[xt_], writes=[HXb[t0 // CW]])
                    else:
                        DMA("sp", rsview(R, t0, 128), xt_[:, :, :128], reads=[xt_], writes=[rb])
            if not (last_layer and stage == 99):
                if pair:
                    for j in range(NCW):
                        fw.collective("AllGather", [HXs[j].opt()], [HGs[j].opt()], [[0, 1], [2, 3], [4, 5], [6, 7]][:ncores // 2], reads=[HXb[j]], writes=[HGb[j]])
                    for j in range(NCW):
                        for r in range(2):
                            c0 = r * half + j * CW
                            DMA("sp", XR[:, c0:c0 + CW], HGs[j][r * D:(r + 1) * D, :], reads=[HGb[j]], writes=list(XRb))
                else:
                    for j in range(NCW):
                        DMA("sp", XR[:, j * CW:(j + 1) * CW], HXs[j][:, :], reads=[HXb[j]], writes=list(XRb))
            fw.barrier()
            pst.close()

        for l in range(depth):
            last_layer = (l == depth - 1)
            mst = ExitStack()

            def sbm(name, shape, dt=F32):
                return Tile(mst.enter_context(nc.sbuf_tensor(f"m{l}_" + name, list(shape), dt)), name)
            winb = sbm("winb", [128, KC, 4096], BF16)
            woutb = sbm("woutb", [128, KC, D], BF16)
            wmb = [sbm(f"wmb{i}", [128, KC, 256]) for i in range(2)]
            Sbf = [sbm(f"Sbf{i}", [128, 128], BF16) for i in range(8)]
            W = {n_: sbm("w_" + n_, [128, NT]) for n_ in ["sg", "f", "key", "lf", "b", "bc", "e1", "e2", "e3", "e4"]}
            qp = sbm("qp", [128, NT], BF16); qin = sbm("qin", [128, NT], BF16); kp = sbm("kp", [128, NT], BF16); kout = sbm("kout", [128, NT], BF16)
            vtm = sbm("vtm", [128, NT // 128, 512], BF16); kotm = sbm("kotm", [128, 4, 128], BF16)
            attm = sbm("attm", [128, 128], BF16)
            osb = sbm("osb", [128, 4, NT]); ofl = sbm("ofl", [128, 4, NT]); sgl = sbm("sgl", [128, 4, NT])
            stg = sbm("stg", [128, 4, NT])
            mix = sbm("mix", [128, KC, NT], BF16)
            uh = sbm("uh", [128, 4, NT + 128]); bgl = sbm("bgl", [128, 4, NT]); cva = sbm("cva", [128, NT])
            zbl = sbm("zbl", [128, 4, NT]); ql = sbm("ql", [128, 4, NT])
            xt = sbm("xt", [128, KC, NT]); tmp8 = sbm("tmp8", [128, KC, NT]); sq8 = sbm("sq8", [128, KC, NT], BF16)
            hT = sbm("hT", [128, KC, NT], BF16)
            alloc_psum(mst, f"m{l}", 6, 2); cnt["n"] = 6
            DMA("sp", n1t[:], n1_in[l], writes=[n1t]); DMA("sp", n2t[:], n2_in[l], writes=[n2t])
            DMA("sp", bmt[:], bmod_in[l], writes=[bmt]); DMA("sp", cwt[:], cw_in[l], writes=[cwt])
            for k in range(KC):
                DMA("pool", winb[:, k, :], win_in[l, k * 128:(k + 1) * 128, :], writes=[winb])
            for k in range(KC):
                DMA("pool", woutb[:, k, :], wout_in[l, k * 128:(k + 1) * 128, :], writes=[woutb])
            pm = nps()
            for cb in range(24):
                wm = wmb[cb % 2]
                DMA("sp", wm[:], wmod_in[l].rearrange("(k p) c -> p k c", p=128)[:, :, cb * 256:(cb + 1) * 256], writes=[wm])
                for cc in range(2):
                    c = cb * 2 + cc
                    for k in range(KC):
                        OP("pe", lambda e, wm=wm, cc=cc, c=c, k=k: e.matmul(pm[:, 2 * c:2 * c + 2], lhsT=wm[:, k, cc * 128:(cc + 1) * 128],
                                                                            rhs=cond[:, k, :], start=(k == 0), stop=(k == KC - 1)),
                           reads=[wm, cond], writes=[pm])
            OP("dve", lambda e: e.tensor_tensor(out=modv[:], in0=pm[:, 0:96].rearrange("p (c s) -> p c s", s=2),
                                                 in1=bmt[:].unsqueeze(2).to_broadcast([128, 48, 2]), op=ALU.add), reads=[pm, bmt], writes=[modv])
            for (A, nt_, g) in [(A1, n1t, 1), (A2, n2t, 4)]:
                OP("dve", lambda e, A=A, g=g: e.tensor_scalar(out=A[:], in0=modv[:, g * 8:(g + 1) * 8, :], scalar1=1.0, scalar2=None, op0=ALU.add),
                   reads=[modv], writes=[A])
                OP("dve", lambda e, A=A, nt_=nt_: e.tensor_tensor(out=A[:], in0=A[:], in1=nt_[:].unsqueeze(2).to_broadcast([128, KC, 2]), op=ALU.mult),
                   reads=[A, nt_], writes=[A])

            for sname in ["c", "x"]:
                S_ = streams[sname]
                s = S_["s"]; n = S_["n"]; ntile = S_["nt"]; R = S_["R"]; Rb = S_["Rb"]; T = S_["T"]
                nsub = n // 128
                if sname == "c":
                    OP("dve", lambda e: e.memset(Sst[:], 0.0), writes=[Sst])
                only_states = (sname == "c" and last_layer)
                for i in range(ntile):
                    t0 = i * n
                    DMA("sp", xt[:, :, :n], rsview(R, t0, n), reads=[Rb[i]], writes=[xt])
                    norm_mod(n, A1, modv, 0, s, hT, xt, tmp8, sq8)
                    for j in range(nsub):
                        pv = nps()
                        for k in range(KC):
                            OP("pe", lambda e, k=k, j=j, pv=pv: e.matmul(pv[:, :], lhsT=hT[:, k, j * 128:(j + 1) * 128], rhs=winb[:, k, 0:512],
                                                                         start=(k == 0), stop=(k == KC - 1)), reads=[hT, winb], writes=[pv])
                        OP("act", lambda e, j=j, pv=pv: e.copy(out=vtm[:, j, :], in_=pv[:, :]), reads=[pv], writes=[vtm])
                    DMA("sp", VS[t0:t0 + n, :].rearrange("(j p) c -> p j c", p=128), vtm[:, :nsub, :], reads=[vtm], writes=[scr_b["VS"][i]])

                    def proj(cc):
                        ps = nps()
                        for k in range(KC):
                            OP("pe", lambda e, k=k, ps=ps: e.matmul(ps[:, :n], lhsT=winb[:, k, cc * 128:(cc + 1) * 128], rhs=hT[:, k, :n],
                                                                    start=(k == 0), stop=(k == KC - 1)), reads=[hT, winb], writes=[ps])
                        return ps
                    for h in range(4):
                        ps = proj(8 + h)
                        OP("act", lambda e, h=h, ps=ps: e.copy(out=stg[:, h, :n], in_=ps[:, :n]), reads=[ps], writes=[stg])
                    DMA("sp", s4view(ZB, t0, n), stg[:, :, :n], reads=[stg], writes=[scr_b["ZB"][i]])
                    for h in range(4):
                        psq = proj(12 + h)
                        OP("act", lambda e, h=h, psq=psq: e.copy(out=ql[:, h, :n], in_=psq[:, :n]), reads=[psq], writes=[ql])
                        psz = proj(4 + h)
                        zps_t[0] = psz
                        last = gate_math(psz[:, :n], ql[:, h, :n], ql, l, 0, h, n, False)
                        recur(l, 0, h, n, False, last, osb)
                    DMA("sp", s4view(QS, t0, n), ql[:, :, :n], reads=[ql], writes=[scr_b["QS"][i]])
                    DMA("sp", s4view(OF, t0, n), osb[:, :, :n], reads=[osb], writes=[scr_b["OF"][i]])
                    if only_states:
                        continue
                    for h in range(4):
                        ps = proj(16 + h)
                        OP("act", lambda e, h=h, ps=ps: e.activation(out=stg[:, h, :n], in_=ps[:, :n], func=AF.Silu), reads=[ps], writes=[stg])
                    DMA("sp", s4view(SG, t0, n), stg[:, :, :n], reads=[stg], writes=[scr_b["SG"][i]])
                    for c in range(4):
                        pc = proj(20 + c)
                        OP("act", lambda e, pc=pc: e.copy(out=cva[:, :n], in_=pc[:, :n]), reads=[pc], writes=[cva])
                        ph = proj(28 + c)
                        OP("dve", lambda e, c=c, ph=ph: e.tensor_tensor(out=stg[:, c, :n], in0=ph[:, :n], in1=cva[:, :n], op=ALU.mult),
                           reads=[ph, cva], writes=[stg])
                    DMA("sp", s4view(US, t0, n), stg[:, :, :n], reads=[stg], writes=[scr_b["US"][i]])
                    for c in range(4):
                        pb = proj(24 + c)
                        OP("act", lambda e, c=c, pb=pb: e.copy(out=stg[:, c, :n], in_=pb[:, :n]), reads=[pb], writes=[stg])
                    DMA("sp", s4view(BG, t0, n), stg[:, :, :n], reads=[stg], writes=[scr_b["BG"][i]])
                for i in range(ntile - 1, -1, -1):
                    t0 = i * n
                    DMA("sp", zbl[:, :, :n], s4view(ZB, t0, n), reads=[scr_b["ZB"][i]], writes=[zbl])
                    DMA("sp", ql[:, :, :n], s4view(QS, t0, n), reads=[scr_b["QS"][i]], writes=[ql])
                    DMA("sp", vtm[:, :nsub, :], VS[t0:t0 + n, :].rearrange("(j p) c -> p j c", p=128), reads=[scr_b["VS"][i]], writes=[vtm])
                    for h in range(4):
                        zps_t[0] = zbl
                        last = gate_math(zbl[:, h, :n], ql[:, h, :n], ql, l, 1, h, n, True)
                        recur(l, 1, h, n, True, last, osb)
                    if only_states:
                        continue
                    DMA("sp", ofl[:, :, :n], s4view(OF, t0, n), reads=[scr_b["OF"][i]], writes=[ofl])
                    DMA("sp", sgl[:, :, :n], s4view(SG, t0, n), reads=[scr_b["SG"][i]], writes=[sgl])
                    DMA("sp", bgl[:, :, :n], s4view(BG, t0, n), reads=[scr_b["BG"][i]], writes=[bgl])
                    OP("pool", lambda e: e.memset(uh[:], 0.0), writes=[uh])
                    lo = 64 if t0 > 0 else 0
                    hi = 64 if t0 + n < T else 0
                    rd = [scr_b["US"][i]] + ([scr_b["US"][i - 1]] if lo else []) + ([scr_b["US"][i + 1]] if hi else [])
                    DMA("sp", uh[:, :, 64 - lo:64 + n + hi], s4view(US, t0 - lo, n + lo + hi), reads=rd, writes=[uh])
                    DMA("sp", xt[:, :, :n], rsview(R, t0, n), reads=[Rb[i]], writes=[xt])
                    OP("dve", lambda e: e.tensor_tensor(out=osb[:, :, :n], in0=osb[:, :, :n], in1=ofl[:, :, :n], op=ALU.add), reads=[osb, ofl], writes=[osb])
                    OP("act", lambda e: e.activation(out=sq8[:, 0:4, :n], in_=osb[:, :, :n], func=AF.Square), reads=[osb], writes=[sq8])
                    for h in range(4):
                        ps = nps()
                        OP("pe", lambda e, h=h, ps=ps: e.matmul(ps[:, :n], lhsT=ones[:], rhs=sq8[:, h, :n], start=True, stop=True), reads=[ones, sq8], writes=[ps])
                        OP("act", lambda e, ps=ps: e.activation(out=rstd[:, :n], in_=ps[:, :n], func=AF.Sqrt, scale=1.0 / 128, bias=epsc[:]),
                           reads=[ps, epsc], writes=[rstd])
                        OP("dve", lambda e: e.reciprocal(out=rstd[:, :n], in_=rstd[:, :n]), reads=[rstd], writes=[rstd])
                        OP("dve", lambda e, h=h: e.tensor_tensor(out=osb[:, h, :n], in0=osb[:, h, :n], in1=rstd[:, :n], op=ALU.mult), reads=[osb, rstd], writes=[osb])
                        OP("dve", lambda e, h=h: e.tensor_tensor(out=mix[:, h, :n], in0=osb[:, h, :n], in1=sgl[:, h, :n], op=ALU.mult), reads=[osb, sgl], writes=[mix])
                    for c in range(4):
                        ctr = uh[:, c, 64:64 + n]
                        OP("dve", lambda e, c=c, ctr=ctr: e.tensor_scalar(out=cva[:, :n], in0=ctr, scalar1=cwt[:, c, 1:2], scalar2=None, op0=ALU.mult),
                           reads=[uh, cwt], writes=[cva])
                        if sname == "x" and c >= 2:
                            OP("dve", lambda e, c=c: e.scalar_tensor_tensor(out=cva[:, :n], in0=uh[:, c, 0:n], scalar=cwt[:, c, 0:1], in1=cva[:, :n],
                                                                            op0=ALU.mult, op1=ALU.add), reads=[uh, cwt, cva], writes=[cva])
                            OP("dve", lambda e, c=c: e.scalar_tensor_tensor(out=cva[:, :n], in0=uh[:, c, 128:128 + n], scalar=cwt[:, c, 2:3], in1=cva[:, :n],
                                                                            op0=ALU.mult, op1=ALU.add), reads=[uh, cwt, cva], writes=[cva])
                        elif sname == "x":
                            u3 = uh[:, c, 64:64 + n].rearrange("p (r w) -> p r w", w=64)
                            c3 = cva[:, :n].rearrange("p (r w) -> p r w", w=64)
                            OP("dve", lambda e, c=c, u3=u3, c3=c3: e.scalar_tensor_tensor(out=c3[:, :, 1:64], in0=u3[:, :, 0:63], scalar=cwt[:, c, 0:1], in1=c3[:, :, 1:64],
                                                                                        op0=ALU.mult, op1=ALU.add), reads=[uh, cwt, cva], writes=[cva])
                            OP("dve", lambda e, c=c, u3=u3, c3=c3: e.scalar_tensor_tensor(out=c3[:, :, 0:63], in0=u3[:, :, 1:64], scalar=cwt[:, c, 2:3], in1=c3[:, :, 0:63],
                                                                                        op0=ALU.mult, op1=ALU.add), reads=[uh, cwt, cva], writes=[cva])
                        else:
                            OP("dve", lambda e, c=c: e.scalar_tensor_tensor(out=cva[:, :n], in0=uh[:, c, 63:63 + n], scalar=cwt[:, c, 0:1], in1=cva[:, :n],
                                                                            op0=ALU.mult, op1=ALU.add), reads=[uh, cwt, cva], writes=[cva])
                            OP("dve", lambda e, c=c: e.scalar_tensor_tensor(out=cva[:, :n], in0=uh[:, c, 65:65 + n], scalar=cwt[:, c, 2:3], in1=cva[:, :n],
                                                                            op0=ALU.mult, op1=ALU.add), reads=[uh, cwt, cva], writes=[cva])
                        OP("dve", lambda e, c=c: e.tensor_tensor(out=mix[:, 4 + c, :n], in0=cva[:, :n], in1=bgl[:, c, :n], op=ALU.mult), reads=[cva, bgl], writes=[mix])
                    for m in range(KC):
                        ps = nps()
                        for fch in range(KC):
                            OP("pe", lambda e, m=m, fch=fch, ps=ps: e.matmul(ps[:, :n], lhsT=woutb[:, fch, m * 128:(m + 1) * 128], rhs=mix[:, fch, :n],
                                                                             start=(fch == 0), stop=(fch == KC - 1)), reads=[woutb, mix], writes=[ps])
                        OP("dve", lambda e, m=m, ps=ps: e.scalar_tensor_tensor(out=xt[:, m, :n], in0=ps[:, :n], scalar=modv[:, 16 + m, s:s + 1], in1=xt[:, m, :n],
                                                                               op0=ALU.mult, op1=ALU.add), reads=[ps, modv, xt], writes=[xt])
                    DMA("sp", rsview(R, t0, n), xt[:, :, :n], reads=[xt], writes=[Rb[i]])
            fw.barrier()
            mst.close()
            if stage == 1:
                break
            peer_layer(l, last_layer)
            if stage == 2:
                break

        if stage in (1, 2):
            xt = sb("xo", [128, KC, NT])
            for i in range(seq // NT):
                DMA("sp", xt[:, :, :NT], rsview(XR, i * NT, NT), reads=[XRb[i]], writes=[xt])
                DMA("sp", rsview(yT_out, i * NT, NT), xt[:, :, :NT], reads=[xt], writes=[dbuf("y")])
            if dbg:
                DMA("sp", xt[:, :, :ctxlen], rsview(CR, 0, ctxlen), reads=[CRb[0]], writes=[xt])
                DMA("sp", rsview(dbgc_out, 0, ctxlen), xt[:, :, :ctxlen], reads=[xt], writes=[dbuf("y2")])
        fw.finish()
        print("instructions:", fw.ninst, {k: v.count for k, v in fw.E.items()})
    return nc


def make_consts():
    bf = ml_dtypes.bfloat16
    ident = np.eye(128, dtype=np.float32).astype(bf)
    ones = np.ones((128, 128), np.float32).astype(bf)
    s = np.arange(128)[:, None]; t = np.arange(128)[None, :]
    same = (s // CH) == (t // CH)
    maskF = (same & (s <= t)).astype(np.float32).astype(bf)
    maskB = (same & (s >= t)).astype(np.float32).astype(bf)
    resetm = np.ones((128, NT), np.float32); resetm[:, ::CH] = 0.0
    rowm = (np.arange(128)[:, None] // CH == np.arange(4)[None, :]).astype(np.float32)
    return dict(identF=np.eye(128, dtype=np.float32), ident=ident, ones=ones, maskF=maskF, maskB=maskB, resetm=resetm, rowm=rowm)


def prep_shared(inp, depth):
    f = lambda a: np.ascontiguousarray(np.asarray(a, dtype=np.float32))
    sh = {}
    sh["w_mod"] = f(inp["w_mod"])
    sh["b_mod"] = f(np.asarray(inp["b_mod"]).reshape(depth, 48, 128).transpose(0, 2, 1))
    sh["n1"] = f(np.asarray(inp["norm1_g"]).reshape(depth, KC, 128).transpose(0, 2, 1))
    sh["n2"] = f(np.asarray(inp["norm2_g"]).reshape(depth, KC, 128).transpose(0, 2, 1))
    sh["fg"] = f(np.repeat(np.asarray(inp["final_g"]).reshape(KC, 128).T[:, :, None], 2, axis=2))
    sh["w_in"] = f(inp["w_in"]); sh["w_out"] = f(inp["w_out"])
    sh["cw"] = f(np.asarray(inp["conv_w"]).reshape(depth, 3, 4, 128).transpose(0, 3, 2, 1))
    sh["lbl"] = f(np.asarray(inp["lb_logits"]).reshape(depth, 2, 4, 128).transpose(3, 0, 1, 2))
    sh["wq"] = f(inp["peer_wq"])
    sh["skT"] = f(np.asarray(inp["peer_subkeys"]).reshape(depth, 16, 128, 128).transpose(0, 3, 1, 2))
    sh["uT"] = f(np.asarray(inp["peer_u"]).transpose(0, 2, 1))
    sh["v"] = f(inp["peer_v"])
    sh.update(make_consts())
    return sh


def prep_core(inp, b, parity=0, half=0):
    f = lambda a: np.ascontiguousarray(np.asarray(a, dtype=np.float32))
    m = {"par": np.array([[parity * half]], np.int32)}
    m["xT"] = f(np.asarray(inp["x"])[b].T)
    m["cxT"] = f(np.asarray(inp["ctx"])[b].T)
    cv = np.stack([np.asarray(inp["c"])[b], np.asarray(inp["c_ctx"])], axis=-1)
    m["cv"] = f(cv.reshape(KC, 128, 2).transpose(1, 0, 2))
    return m


def kernel(**inp):
    x = np.asarray(inp["x"])
    B, seq, _ = x.shape
    ctxlen = np.asarray(inp["ctx"]).shape[1]
    depth = np.asarray(inp["w_in"]).shape[0]
    nc = build(depth, seq, ctxlen, ncores=2 * B)
    sh = prep_shared(inp, depth)
    half = seq // 2
    in_maps = []
    for c in range(2 * B):
        m = dict(sh); m.update(prep_core(inp, c // 2, c % 2, half)); in_maps.append(m)
    res = run_bass_kernel_spmd(nc, in_maps, core_ids=list(range(2 * B)))
    out = np.empty((B, seq, D), np.float32)
    for c in range(2 * B):
        out[c // 2, (c % 2) * half:(c % 2 + 1) * half, :] = res.results[c]["yT"].T
    return out
```

```python
import numpy as np
import ml_dtypes
from contextlib import ExitStack
import concourse.bass as bass
import concourse.mybir as mybir
from concourse.bass_utils import run_bass_kernel_spmd

F32 = mybir.dt.float32
BF16 = mybir.dt.bfloat16
AF = mybir.ActivationFunctionType
ALU = mybir.AluOpType
AX = mybir.AxisListType

D = 1024
KC = 8
NT = 256
NP = 256
CH = 32
EPS = 1e-6
EPOCH = 30000
NDMA = 12
NEXP = 16384


class Tok:
    __slots__ = ("sem", "val", "eng")

    def __init__(self, sem, val, eng):
        self.sem = sem; self.val = val; self.eng = eng


class Buf:
    def __init__(self, name=""):
        self.name = name; self.w = None; self.r = {}


class Eng:
    def __init__(self, name, obj):
        self.name = name; self.obj = obj
        self.sems = []; self.count = 0; self.seen = {}
        self.dma_sems = []; self.dma_n = 0


class FW:
    def __init__(self, nc, stack):
        self.nc = nc; self.stack = stack
        self.E = {n: Eng(n, getattr(nc, o)) for n, o in
                  [("pe", "tensor"), ("dve", "vector"), ("act", "scalar"), ("pool", "gpsimd"), ("sp", "sync")]}
        self.ninst = 0

    def newsem(self, name):
        return self.stack.enter_context(self.nc.semaphore(name))

    def _wait(self, e, tok):
        if tok is None:
            return
        k = id(tok.sem)
        if e.seen.get(k, 0) >= tok.val:
            return
        e.obj.wait_ge(tok.sem, tok.val)
        e.seen[k] = tok.val

    def _deps(self, e, reads, writes):
        for b in reads:
            if b.w is not None:
                self._wait(e, b.w)
        for b in writes:
            if b.w is not None and b.w.eng != e.name:
                self._wait(e, b.w)
            for en, t in b.r.items():
                if en != e.name:
                    self._wait(e, t)

    def _commit(self, tok, reads, writes, rkey):
        for b in reads:
            b.r[rkey] = tok
        for b in writes:
            b.w = tok; b.r = {}

    def op(self, eng, fn, reads=(), writes=()):
        e = self.E[eng]
        self._deps(e, reads, writes)
        ep = e.count // EPOCH
        while len(e.sems) <= ep:
            e.sems.append(self.newsem(f"s_{eng}_{len(e.sems)}"))
        ins = fn(e.obj)
        val = e.count - ep * EPOCH + 1
        ins.then_inc(e.sems[ep], 1)
        e.count += 1
        self.ninst += 1
        tok = Tok(e.sems[ep], val, eng)
        self._commit(tok, reads, writes, eng)
        return tok

    def dma(self, eng, out, in_, reads=(), writes=(), **kw):
        e = self.E[eng]
        self._deps(e, reads, writes)
        j = e.dma_n % NDMA
        if len(e.dma_sems) <= j:
            e.dma_sems.append([self.newsem(f"d_{eng}_{j}"), 0])
        slot = e.dma_sems[j]
        if slot[1] > 0:
            self._wait(e, Tok(slot[0], slot[1], "dma"))
        slot[1] += 16
        e.obj.dma_start(out=out, in_=in_, **kw).then_inc(slot[0], 16)
        e.dma_n += 1
        self.ninst += 1
        tok = Tok(slot[0], slot[1], "dma")
        self._commit(tok, reads, writes, f"dma_{eng}_{j}")
        return tok

    def raw(self, eng, fn, reads=(), writes=()):
        e = self.E[eng]
        self._deps(e, reads, writes)
        return fn(e.obj)

    def collective(self, kind, ins, outs, groups, reads=(), writes=()):
        e = self.E["pool"]
        self._deps(e, reads, writes)
        if not hasattr(self, "cc_sem"):
            self.cc_sem = self.newsem("cc_sem"); self.cc_n = 0
        self.cc_n += 1
        e.obj.collective_compute(kind, mybir.AluOpType.bypass, replica_groups=groups, ins=ins, outs=outs).then_inc(self.cc_sem, 1)
        tok = Tok(self.cc_sem, self.cc_n, "cc")
        self._commit(tok, reads, writes, "cc")
        return tok

    def barrier(self):
        toks = []
        for en in self.E.values():
            if en.count > 0:
                ep = (en.count - 1) // EPOCH
                toks.append(Tok(en.sems[ep], en.count - ep * EPOCH, en.name))
            for slot in en.dma_sems:
                if slot[1] > 0:
                    toks.append(Tok(slot[0], slot[1], "dma"))
        if hasattr(self, "cc_sem") and self.cc_n > 0:
            toks.append(Tok(self.cc_sem, self.cc_n, "cc"))
        for e in self.E.values():
            for t in toks:
                if t.eng != e.name:
                    self._wait(e, t)

    def finish(self):
        e = self.E["sp"]
        for en in self.E.values():
            for slot in en.dma_sems:
                if slot[1] > 0:
                    self._wait(e, Tok(slot[0], slot[1], "dma"))


class Tile:
    def __init__(self, t, name):
        self.t = t; self.b = Buf(name)

    def __getitem__(self, k):
        return self.t[k]


def build(depth, seq, ctxlen, stage=99, dbg=False, pair=True, ncores=8):
    nc = bass.Bass("TRN2", target_bir_lowering=False)
    assert seq % NT == 0 and ctxlen <= NT and ctxlen % 128 == 0 and seq % NP == 0
    rows_per_tile = NT // 64

    def din(name, shape, dt=F32):
        return nc.dram_tensor(name, list(shape), dt, kind="ExternalInput").ap()

    def dscr(name, shape, dt=F32):
        return nc.dram_tensor(name, list(shape), dt).ap()

    xT_in = din("xT", [D, seq]); cxT_in = din("cxT", [D, ctxlen])
    cv_in = din("cv", [128, KC, 2])
    wmod_in = din("w_mod", [depth, D, 6 * D]); bmod_in = din("b_mod", [depth, 128, 48])
    n1_in = din("n1", [depth, 128, KC]); n2_in = din("n2", [depth, 128, KC]); fg_in = din("fg", [128, KC, 2])
    win_in = din("w_in", [depth, D, 4096]); wout_in = din("w_out", [depth, D, D])
    cw_in = din("cw", [depth, 128, 4, 3]); lbl_in = din("lbl", [128, depth, 2, 4])
    wq_in = din("wq", [depth, D, 2048]); skT_in = din("skT", [depth, 128, 16, 128])
    uT_in = din("uT", [depth, D, NEXP]); v_in = din("v", [depth, NEXP, D])
    ident_in = din("ident", [128, 128], BF16); ones_in = din("ones", [128, 128], BF16)
    maskF_in = din("maskF", [128, 128], BF16); maskB_in = din("maskB", [128, 128], BF16)
    identF_in = din("identF", [128, 128]); reset_in = din("resetm", [128, NT]); rowm_in = din("rowm", [128, 4])
    half = seq // 2 if pair else seq
    I32 = mybir.dt.int32
    par_in = din("par", [1, 1], I32)
    yT_out = nc.dram_tensor("yT", [D, half if stage == 99 else seq], F32, kind="ExternalOutput").ap()
    dbg_out = nc.dram_tensor("dbg", [D, seq], F32, kind="ExternalOutput").ap() if dbg else None
    dbgc_out = nc.dram_tensor("dbgc", [D, ctxlen], F32, kind="ExternalOutput").ap() if dbg else None

    TM = max(seq, ctxlen)
    XRh = nc.dram_tensor("XR", [D, seq], F32); XR = XRh.ap(); CR = dscr("CR", [D, ctxlen])
    CW = min(512, half); NCW = half // CW
    HXs = [dscr(f"HX{j}", [D, CW]) for j in range(NCW)]; HGs = [dscr(f"HG{j}", [2 * D, CW]) for j in range(NCW)]
    HXb = [Buf(f"HX{j}") for j in range(NCW)]; HGb = [Buf(f"HG{j}") for j in range(NCW)]
    ZB = dscr("ZB", [512, TM]); QS = dscr("QS", [512, TM]); SG = dscr("SG", [512, TM])
    OF = dscr("OF", [512, TM]); US = dscr("US", [512, TM]); BG = dscr("BG", [512, TM])
    VS = dscr("VS", [TM, 512], BF16)
    U16 = dscr("U16", [NEXP // 512, 128, KC, 512], BF16); V16 = dscr("V16", [NEXP, D], BF16)
    U16b = Buf("U16"); V16b = Buf("V16")

    with ExitStack() as st:
        fw = FW(nc, st)

        def sb(name, shape, dt=F32):
            return Tile(st.enter_context(nc.sbuf_tensor("sb_" + name, list(shape), dt)), name)

        def dbuf(name):
            return Buf(name)

        PS = {"F": [], "B": []}
        cnt = {"f": 0, "b": 0}

        def alloc_psum(stack, tag, nf, nb):
            PS["F"] = [Tile(stack.enter_context(nc.psum_tensor(f"psF{tag}_{i}", [128, 512], F32)), f"psF{i}") for i in range(nf)]
            PS["B"] = [Tile(stack.enter_context(nc.psum_tensor(f"psB{tag}_{i}", [128, 1024], BF16)), f"psB{i}") for i in range(nb)]

        def nps():
            cnt["f"] += 1
            return PS["F"][cnt["f"] % cnt["n"]]

        def npsb():
            cnt["b"] += 1
            return PS["B"][cnt["b"] % 2]

        def OP(eng, fn, reads=(), writes=()):
            return fw.op(eng, fn, reads=[x.b if hasattr(x, "b") else x for x in reads],
                         writes=[x.b if hasattr(x, "b") else x for x in writes])

        def DMA(eng, out, in_, reads=(), writes=(), **kw):
            return fw.dma(eng, out, in_, reads=[x.b if hasattr(x, "b") else x for x in reads],
                          writes=[x.b if hasattr(x, "b") else x for x in writes], **kw)

        ident = sb("ident", [128, 128], BF16); ones = sb("ones", [128, 128], BF16)
        maskF = sb("maskF", [128, 128], BF16); maskB = sb("maskB", [128, 128], BF16)
        resetm = sb("resetm", [128, NT]); rowm = sb("rowm", [128, 4])
        DMA("sp", rowm[:], rowm_in[:, :], writes=[rowm])
        for tl, src in [(ident, ident_in), (ones, ones_in), (maskF, maskF_in), (maskB, maskB_in), (resetm, reset_in)]:
            DMA("sp", tl[:], src[:, :], writes=[tl])
        part = Tile(st.enter_context(nc.sbuf_tensor("sb_part", [1, 1], I32)), "part")
        preg = st.enter_context(nc.sync.register("preg"))
        DMA("sp", part[:], par_in[:, :], writes=[part])
        fw.raw("sp", lambda e: e.reg_load(preg, part[:1, :1]), reads=[part.b])
        pval = nc.sync.snap(preg)
        XRdyn = bass.AP(XRh, pval, [[seq, 128], [128 * seq, KC], [1, half]])
        cond = sb("cond", [128, KC, 2])
        DMA("sp", cond[:], cv_in[:, :, :], writes=[cond])
        OP("act", lambda e: e.activation(out=cond[:], in_=cond[:], func=AF.Silu), reads=[cond], writes=[cond])
        epsc = sb("epsc", [128, 1])
        OP("dve", lambda e: e.memset(epsc[:], EPS), writes=[epsc])
        lbe = sb("lbe", [128, depth, 8]); lbs = sb("lbs", [128, 8]); lb = sb("lb", [128, depth, 8]); oml = sb("oml", [128, depth, 8])
        DMA("sp", lbe[:], lbl_in.rearrange("p l a b -> p l (a b)"), writes=[lbe])
        OP("act", lambda e: e.activation(out=lbe[:], in_=lbe[:], func=AF.Exp), reads=[lbe], writes=[lbe])
        OP("dve", lambda e: e.tensor_copy(out=lbs[:], in_=lbe[:, 0, :]), reads=[lbe], writes=[lbs])
        for l in range(1, depth):
            OP("dve", lambda e, l=l: e.tensor_tensor(out=lbs[:], in0=lbs[:], in1=lbe[:, l, :], op=ALU.add), reads=[lbs, lbe], writes=[lbs])
        OP("dve", lambda e: e.reciprocal(out=lbs[:], in_=lbs[:]), reads=[lbs], writes=[lbs])
        OP("dve", lambda e: e.memset(lb[:, 0, :], 0.0), writes=[lb])
        for l in range(1, depth):
            OP("dve", lambda e, l=l: e.tensor_tensor(out=lbe[:, l, :], in0=lbe[:, l, :], in1=lbs[:], op=ALU.mult), reads=[lbe, lbs], writes=[lbe])
            OP("dve", lambda e, l=l: e.tensor_tensor(out=lb[:, l, :], in0=lb[:, l - 1, :], in1=lbe[:, l, :], op=ALU.add), reads=[lb, lbe], writes=[lb])
        OP("dve", lambda e: e.tensor_scalar(out=oml[:], in0=lb[:], scalar1=-1.0, scalar2=1.0, op0=ALU.mult, op1=ALU.add), reads=[lb], writes=[oml])

        XRb = [dbuf(f"XR{i}") for i in range(seq // NT)]
        CRb = [dbuf("CR")]
        for i in range(seq // NT):
            DMA("sp", XR[:, i * NT:(i + 1) * NT], xT_in[:, i * NT:(i + 1) * NT], writes=[XRb[i]])
        DMA("sp", CR[:, :], cxT_in[:, :], writes=[CRb[0]])

        modv = sb("modv", [128, 48, 2])
        A1 = sb("A1", [128, KC, 2]); A2 = sb("A2", [128, KC, 2])
        n1t = sb("n1t", [128, KC]); n2t = sb("n2t", [128, KC]); bmt = sb("bmt", [128, 48]); cwt = sb("cwt", [128, 4, 3])
        fgt = sb("fgt", [128, KC, 2]); zerob = sb("zerob", [128, KC, 2])
        DMA("sp", fgt[:], fg_in[:, :, :], writes=[fgt])
        OP("dve", lambda e: e.memset(zerob[:], 0.0), writes=[zerob])
        identF = sb("identF", [128, 128])
        DMA("sp", identF[:], identF_in[:, :], writes=[identF])
        Sst = sb("Sst", [128, 8, 128])
        rstd = sb("rstd", [128, NT])

        streams = {
            "c": dict(T=ctxlen, R=CR, Rb=CRb, s=1, nt=1, n=ctxlen),
            "x": dict(T=seq, R=XR, Rb=XRb, s=0, nt=seq // NT, n=NT),
        }
        scr_b = {nm: [dbuf(f"{nm}{i}") for i in range(TM // min(NT, ctxlen) + 1)] for nm in ["ZB", "QS", "SG", "OF", "US", "BG", "VS"]}

        def rsview(R, t0, n):
            return R.rearrange("(k p) t -> p k t", p=128)[:, :, t0:t0 + n]

        def s4view(S, t0, n):
            return S.rearrange("(k p) t -> p k t", p=128)[:, :, t0:t0 + n]

        def norm_mod(n, A, Bt, Bm_col0, s, out_bf, xin, tmp8, sq8):
            OP("act", lambda e: e.activation(out=sq8[:, :, :n], in_=xin[:, :, :n], func=AF.Square), reads=[xin], writes=[sq8])
            ps = nps()
            for k in range(KC):
                OP("pe", lambda e, k=k: e.matmul(ps[:, :n], lhsT=ones[:], rhs=sq8[:, k, :n], start=(k == 0), stop=(k == KC - 1)),
                   reads=[ones, sq8], writes=[ps])
            OP("act", lambda e: e.activation(out=rstd[:, :n], in_=ps[:, :n], func=AF.Sqrt, scale=1.0 / D, bias=epsc[:]), reads=[ps, epsc], writes=[rstd])
            OP("dve", lambda e: e.reciprocal(out=rstd[:, :n], in_=rstd[:, :n]), reads=[rstd], writes=[rstd])
            OP("dve", lambda e: e.tensor_tensor(out=tmp8[:, :, :n], in0=xin[:, :, :n],
                                                 in1=rstd[:, :n].unsqueeze(1).to_broadcast([128, KC, n]), op=ALU.mult),
               reads=[xin, rstd], writes=[tmp8])
            for k in range(KC):
                OP("act", lambda e, k=k: e.activation(out=out_bf[:, k, :n], in_=tmp8[:, k, :n], func=AF.Identity,
                                                       scale=A[:, k, s:s + 1], bias=Bt[:, Bm_col0 + k, s:s + 1]),
                   reads=[tmp8, A, Bt], writes=[out_bf])

        def gate_math(zps, qsrc_ap, qsrc_t, l, d, h, n, bwd):
            nchk = n // CH
            sg, f, key, lf, b, bc, e1, e2, e3, e4 = (W[x] for x in ["sg", "f", "key", "lf", "b", "bc", "e1", "e2", "e3", "e4"])
            col = d * 4 + h
            OP("act", lambda e: e.activation(out=sg[:, :n], in_=zps, func=AF.Sigmoid), reads=[zps_t[0]], writes=[sg])
            OP("dve", lambda e: e.tensor_scalar(out=f[:, :n], in0=sg[:, :n], scalar1=oml[:, l, col:col + 1], scalar2=lb[:, l, col:col + 1],
                                                 op0=ALU.mult, op1=ALU.add), reads=[sg, oml, lb], writes=[f])
            OP("pool", lambda e: e.tensor_scalar(out=f[:, :n], in0=f[:, :n], scalar1=1e-20, scalar2=None, op0=ALU.max), reads=[f], writes=[f])
            OP("pool", lambda e: e.tensor_scalar(out=key[:, :n], in0=f[:, :n], scalar1=-1.0, scalar2=1.0, op0=ALU.mult, op1=ALU.add),
               reads=[f], writes=[key])
            OP("act", lambda e: e.activation(out=lf[:, :n], in_=f[:, :n], func=AF.Ln), reads=[f], writes=[lf])
            OP("dve", lambda e: e.tensor_tensor_scan(out=b[:, :n], data0=resetm[:, :n], data1=lf[:, :n], initial=0.0, op0=ALU.mult, op1=ALU.add),
               reads=[resetm, lf], writes=[b])
            b3 = b[:, :n].rearrange("p (c t) -> p c t", t=CH)
            if bwd:
                lf3 = lf[:, :n].rearrange("p (c t) -> p c t", t=CH)
                bc3 = bc[:, :n].rearrange("p (c t) -> p c t", t=CH)
                OP("dve", lambda e: e.tensor_tensor(out=bc3, in0=b3[:, :, CH - 1:CH].to_broadcast([128, nchk, CH]), in1=b3, op=ALU.subtract),
                   reads=[b], writes=[bc])
                OP("dve", lambda e: e.tensor_tensor(out=b[:, :n], in0=bc[:, :n], in1=lf[:, :n], op=ALU.add), reads=[bc, lf], writes=[b])
                mid = CH // 2; last = 0
            else:
                mid = CH // 2 - 1; last = CH - 1
            bc3 = bc[:, :n].rearrange("p (c t) -> p c t", t=CH)
            OP("dve", lambda e: e.tensor_tensor(out=bc3, in0=b3, in1=b3[:, :, mid:mid + 1].to_broadcast([128, nchk, CH]), op=ALU.subtract),
               reads=[b], writes=[bc])
            OP("act", lambda e: e.activation(out=e1[:, :n], in_=bc[:, :n], func=AF.Exp), reads=[bc], writes=[e1])
            OP("act", lambda e: e.activation(out=e2[:, :n], in_=bc[:, :n], func=AF.Exp, scale=-1.0), reads=[bc], writes=[e2])
            OP("act", lambda e: e.activation(out=e3[:, :n], in_=b[:, :n], func=AF.Exp), reads=[b], writes=[e3])
            OP("dve", lambda e: e.tensor_tensor(out=bc3, in0=b3[:, :, last:last + 1].to_broadcast([128, nchk, CH]), in1=b3, op=ALU.subtract),
               reads=[b, e1, e2], writes=[bc])
            OP("act", lambda e: e.activation(out=e4[:, :n], in_=bc[:, :n], func=AF.Exp), reads=[bc], writes=[e4])
            OP("dve", lambda e: e.tensor_tensor(out=qp[:, :n], in0=qsrc_ap, in1=e1[:, :n], op=ALU.mult), reads=[qsrc_t, e1], writes=[qp])
            OP("pool", lambda e: e.tensor_tensor(out=qin[:, :n], in0=qsrc_ap, in1=e3[:, :n], op=ALU.mult), reads=[qsrc_t, e3], writes=[qin])
            OP("dve", lambda e: e.tensor_tensor(out=kp[:, :n], in0=key[:, :n], in1=e2[:, :n], op=ALU.mult), reads=[key, e2], writes=[kp])
            OP("pool", lambda e: e.tensor_tensor(out=kout[:, :n], in0=key[:, :n], in1=e4[:, :n], op=ALU.mult), reads=[key, e4], writes=[kout])
            return last

        zps_t = [None]

        def recur(l, d, h, n, bwd, last, o_dst):
            si = d * 4 + h
            e3 = W["e3"]
            nsub = n // 128
            subs = range(nsub - 1, -1, -1) if bwd else range(nsub)
            mask = maskB if bwd else maskF
            for j in subs:
                js = slice(j * 128, (j + 1) * 128)
                pt = npsb()
                OP("pe", lambda e: e.transpose(out=pt[:, 0:128], in_=kout[:, js], identity=ident[:]), reads=[kout, ident], writes=[pt])
                for c4 in range(4):
                    OP("act", lambda e, c4=c4: e.activation(out=kotm[:, c4, :], in_=pt[:, 0:128], func=AF.Identity, scale=rowm[:, c4:c4 + 1]),
                       reads=[pt, rowm], writes=[kotm])
                pa = nps()
                OP("pe", lambda e: e.matmul(pa[:, 0:128], lhsT=kp[:, js], rhs=qp[:, js], start=True, stop=True), reads=[kp, qp], writes=[pa])
                OP("dve", lambda e: e.tensor_tensor(out=attm[:], in0=pa[:, 0:128], in1=mask[:], op=ALU.mult), reads=[pa, mask], writes=[attm])
                po = nps()
                chunks = range(3, -1, -1) if bwd else range(4)
                for ci, c in enumerate(chunks):
                    cs = slice(c * CH, (c + 1) * CH)
                    tcs = slice(j * 128 + c * CH, j * 128 + (c + 1) * CH)
                    sbf = Sbf[(si * 4 + ci) % 8]
                    OP("act", lambda e, sbf=sbf: e.copy(out=sbf[:], in_=Sst[:, si, :]), reads=[Sst], writes=[sbf])
                    OP("pe", lambda e, sbf=sbf, cs=cs, tcs=tcs: e.matmul(po[:, cs], lhsT=sbf[:], rhs=qin[:, tcs], start=True, stop=False),
                       reads=[sbf, qin], writes=[po])
                    OP("pe", lambda e, cs=cs: e.matmul(po[:, cs], lhsT=vtm[:, j, h * 128:(h + 1) * 128], rhs=attm[:, cs], start=False, stop=True),
                       reads=[vtm, attm], writes=[po])
                    pS = nps()
                    OP("pe", lambda e, c=c, pS=pS: e.matmul(pS[:, 0:128], lhsT=kotm[:, c, :], rhs=vtm[:, j, h * 128:(h + 1) * 128], start=True, stop=True),
                       reads=[kotm, vtm], writes=[pS])
                    dcol = j * 128 + c * CH + last
                    OP("dve", lambda e, pS=pS, dcol=dcol: e.scalar_tensor_tensor(out=Sst[:, si, :], in0=Sst[:, si, :], scalar=e3[:, dcol:dcol + 1],
                                                                                 in1=pS[:, 0:128], op0=ALU.mult, op1=ALU.add),
                       reads=[Sst, e3, pS], writes=[Sst])
                OP("act", lambda e: e.copy(out=o_dst[:, h, js], in_=po[:, 0:128]), reads=[po], writes=[o_dst])


        HB = 512
        NHB = NEXP // HB

        def peer_layer(l, last_layer):
            pst = ExitStack()

            def sbp(name, shape, dt=F32):
                return Tile(pst.enter_context(nc.sbuf_tensor(f"p{l}_" + name, list(shape), dt)), name)
            alloc_psum(pst, f"p{l}", 8, 0); cnt["n"] = 6
            po = [PS["F"][6], PS["F"][7]]
            wqb = sbp("wqb", [128, KC, 2048], BF16); skb = sbp("skb", [128, 16, 128], BF16)
            for k in range(KC):
                DMA("pool", wqb[:, k, :], wq_in[l, k * 128:(k + 1) * 128, :], writes=[wqb])
            DMA("pool", skb[:], skT_in[l], writes=[skb])
            uTl = uT_in[l].rearrange("(k p) e -> p k e", p=128)
            for hb in range(NHB):
                DMA("pool", U16[hb], uTl[:, :, hb * HB:(hb + 1) * HB], writes=[U16b])
            for r in range(16):
                DMA("pool", V16[r * 1024:(r + 1) * 1024, :], v_in[l, r * 1024:(r + 1) * 1024, :], writes=[V16b])
            V16v = V16.rearrange("(b q p) d -> b p q d", q=4, p=128)
            xtp = [sbp(f"xt{i}", [128, KC, 128]) for i in range(2)]; hTp = [sbp(f"hT{i}", [128, KC, 128], BF16) for i in range(2)]
            tmp8 = sbp("tmp8", [128, KC, 128]); sq8 = sbp("sq8", [128, KC, 128], BF16)
            qT = sbp("qT", [128, 16, 128], BF16); sc = sbp("sc", [128, 16, 128]); scw = sbp("scw", [128, 128])
            top = sbp("top", [128, 16, 16]); cand = sbp("cand", [128, 8, 256]); cw2 = sbp("cw2", [128, 256]); c24 = sbp("c24", [128, 8, 24])
            tau = sbp("tau", [128, 8]); d16 = sbp("d16", [128, 8, 16]); Zs = sbp("Zs", [128, 8]); beta = sbp("beta", [128, 8])
            sA = sbp("sA", [128, 8, 128])
            NB = 3
            Sblk = [sbp(f"Sblk{i}", [128, 8, 128]) for i in range(NB)]; Exb = [sbp(f"Exb{i}", [128, 8, 128], BF16) for i in range(NB)]
            tmpg = [sbp(f"tmpg{i}", [128, 1024], BF16) for i in range(NB)]
            NR = 4
            ub = [sbp(f"ub{i}", [128, KC, HB], BF16) for i in range(NR)]; vb = [sbp(f"vb{i}", [128, 4, D], BF16) for i in range(NR)]
            gaT = [sbp(f"gaT{i}", [128, 1024]) for i in range(2)]; WT = [sbp(f"WT{i}", [128, 8, 128], BF16) for i in range(2)]
            zb16 = sbp("zb16", [128, 128], BF16)
            OP("dve", lambda e: e.memset(zb16[:], 0.0), writes=[zb16])
            tmpo_ap = cand[:].rearrange("p h c -> p (h c)")[:, 0:1024].rearrange("p (k n) -> p k n", n=128)

            class _Alias:
                def __init__(self, ap, tile_):
                    self.ap = ap; self.b = tile_.b

                def __getitem__(self, k):
                    return self.ap[k]
            tmpo = _Alias(tmpo_ap, cand)
            otm = _Alias(Sblk[0][:].rearrange("p a b -> p (a b)"), Sblk[0])
            ti = 0
            ybuf = Buf("yout")
            for sname in (["x"] if last_layer else ["c", "x"]):
                S_ = streams[sname]
                s = S_["s"]; R = S_["R"]; Rb = S_["Rb"]; T = S_["T"]; nreg = S_["n"]
                dyn = (sname == "x")
                for i in range((half if dyn else T) // 128):
                    t0 = i * 128
                    xt_ = xtp[ti % 2]; hT_ = hTp[ti % 2]; ti += 1
                    if dyn:
                        DMA("sp", xt_[:, :, :128], XRdyn[:, :, t0:t0 + 128], reads=list(Rb), writes=[xt_])
                    else:
                        rb = Rb[t0 // nreg]
                        DMA("sp", xt_[:, :, :128], rsview(R, t0, 128), reads=[rb], writes=[xt_])
                    norm_mod(128, A2, modv, 24, s, hT_, xt_, tmp8, sq8)
                    for jc in range(16):
                        ps = nps()
                        for k in range(KC):
                            OP("pe", lambda e, k=k, jc=jc, ps=ps: e.matmul(ps[:, :128], lhsT=wqb[:, k, jc * 128:(jc + 1) * 128], rhs=hT_[:, k, :128],
                                                                           start=(k == 0), stop=(k == KC - 1)), reads=[wqb, hT_], writes=[ps])
                        OP("act", lambda e, jc=jc, ps=ps: e.copy(out=qT[:, jc, :], in_=ps[:, :128]), reads=[ps], writes=[qT])
                    for g in range(4):
                        ps = nps()
                        for jj in range(4):
                            jc = g * 4 + jj
                            OP("pe", lambda e, jc=jc, jj=jj, ps=ps: e.matmul(ps[:, jj * 128:(jj + 1) * 128], lhsT=qT[:, jc, :], rhs=skb[:, jc, :], start=True, stop=True),
                               reads=[qT, skb], writes=[ps])
                        OP("act", lambda e, g=g, ps=ps: e.copy(out=sc[:, g * 4:(g + 1) * 4, :], in_=ps[:, :].rearrange("p (a b) -> p a b", b=128)), reads=[ps], writes=[sc])
                    for jc in range(16):
                        OP("dve", lambda e, jc=jc: e.max(out=top[:, jc, 0:8], in_=sc[:, jc, :]), reads=[sc], writes=[top])
                        OP("dve", lambda e, jc=jc: e.match_replace(out=scw[:], in_to_replace=top[:, jc, 0:8], in_values=sc[:, jc, :], imm_value=-1e30),
                           reads=[top, sc], writes=[scw])
                        OP("dve", lambda e, jc=jc: e.max(out=top[:, jc, 8:16], in_=scw[:]), reads=[scw], writes=[top])
                    top4 = top[:].rearrange("p (h two) r -> p h two r", two=2)
                    cand4 = cand[:].rearrange("p h (r s) -> p h r s", s=16)
                    OP("dve", lambda e: e.tensor_tensor(out=cand4, in0=top4[:, :, 0, :].unsqueeze(3).to_broadcast([128, 8, 16, 16]),
                                                         in1=top4[:, :, 1, :].unsqueeze(2).to_broadcast([128, 8, 16, 16]), op=ALU.add), reads=[top], writes=[cand])
                    for h in range(8):
                        OP("dve", lambda e, h=h: e.max(out=c24[:, h, 0:8], in_=cand[:, h, :]), reads=[cand], writes=[c24])
                        OP("dve", lambda e, h=h: e.match_replace(out=cw2[:], in_to_replace=c24[:, h, 0:8], in_values=cand[:, h, :], imm_value=-1e30),
                           reads=[c24, cand], writes=[cw2])
                        OP("dve", lambda e, h=h: e.max(out=c24[:, h, 8:16], in_=cw2[:]), reads=[cw2], writes=[c24])
                        OP("dve", lambda e, h=h: e.match_replace(out=cw2[:], in_to_replace=c24[:, h, 8:16], in_values=cw2[:], imm_value=-1e30),
                           reads=[c24, cw2], writes=[cw2])
                        OP("dve", lambda e, h=h: e.max(out=c24[:, h, 16:24], in_=cw2[:]), reads=[cw2], writes=[c24])
                    OP("dve", lambda e: e.tensor_tensor(out=tau[:], in0=c24[:, :, 15], in1=c24[:, :, 16], op=ALU.add), reads=[c24], writes=[tau])
                    OP("dve", lambda e: e.tensor_scalar(out=tau[:], in0=tau[:], scalar1=0.5, scalar2=None, op0=ALU.mult), reads=[tau], writes=[tau])
                    OP("dve", lambda e: e.tensor_tensor(out=d16[:], in0=c24[:, :, 0:16], in1=c24[:, :, 0:1].to_broadcast([128, 8, 16]), op=ALU.subtract),
                       reads=[c24], writes=[d16])
                    OP("act", lambda e: e.activation(out=d16[:], in_=d16[:], func=AF.Exp), reads=[d16], writes=[d16])
                    OP("dve", lambda e: e.tensor_reduce(out=Zs[:], in_=d16[:], axis=AX.X, op=ALU.add), reads=[d16], writes=[Zs])
                    OP("act", lambda e: e.activation(out=Zs[:], in_=Zs[:], func=AF.Ln), reads=[Zs], writes=[Zs])
                    OP("dve", lambda e: e.tensor_tensor(out=beta[:], in0=tau[:], in1=c24[:, :, 0], op=ALU.subtract), reads=[tau, c24], writes=[beta])
                    OP("dve", lambda e: e.tensor_tensor(out=beta[:], in0=beta[:], in1=Zs[:], op=ALU.subtract), reads=[beta, Zs], writes=[beta])
                    sc4 = sc[:].rearrange("p (h two) k -> p h two k", two=2)
                    OP("dve", lambda e: e.tensor_tensor(out=sA[:], in0=sc4[:, :, 0, :], in1=tau[:].unsqueeze(2).to_broadcast([128, 8, 128]), op=ALU.subtract),
                       reads=[sc, tau], writes=[sA])

                    NBK = NEXP // 1024
                    its = [(bk, h) for bk in range(NBK) for h in range(8)]
                    pg = {}; pa = {}

                    def ldu(hb):
                        DMA("sp", ub[hb % NR][:], U16[hb], reads=[U16b], writes=[ub[hb % NR]])

                    def ldv(hb):
                        DMA("sp", vb[hb % NR][:], V16v[hb], reads=[V16b], writes=[vb[hb % NR]])

                    def blk_start(bk):
                        pa[bk] = (nps(), nps()); pg[bk] = (nps(), nps())
                        for c in range(8):
                            ub_ = ub[(2 * bk + c // 4) % NR]; pp = pa[bk][c // 4]
                            for k in range(KC):
                                OP("pe", lambda e, k=k, c=c, ub_=ub_, pp=pp: e.matmul(pp[:, (c % 4) * 128:(c % 4 + 1) * 128], lhsT=ub_[:, k, (c % 4) * 128:(c % 4 + 1) * 128],
                                                                                     rhs=hT_[:, k, :128], start=(k == 0), stop=(k == KC - 1)),
                                   reads=[ub_, hT_], writes=[pp])
                        for hf in range(2):
                            OP("pe", lambda e, hf=hf: e.matmul(pg[bk][hf][:, :], lhsT=zb16[:], rhs=hT_[:, 0:4, :].rearrange("p a b -> p (a b)"), start=True, stop=False),
                               reads=[zb16, hT_], writes=[pg[bk][hf]])
                        g_ = gaT[bk % 2]
                        for hf in range(2):
                            OP("act", lambda e, hf=hf: e.activation(out=g_[:, hf * 512:(hf + 1) * 512], in_=pa[bk][hf][:, :], func=AF.Gelu),
                               reads=[pa[bk][hf]], writes=[g_])

                    def g1(n_):
                        bk, h = its[n_]
                        a0 = bk * 8
                        sbk = Sblk[n_ % NB]; exk = Exb[n_ % NB]
                        eng = "pool" if (n_ % 2 == 1) else "dve"
                        OP(eng, lambda e: e.tensor_tensor(out=sbk[:], in0=sA[:, h, a0:a0 + 8].unsqueeze(2).to_broadcast([128, 8, 128]),
                                                          in1=sc4[:, h, 1, :].unsqueeze(1).to_broadcast([128, 8, 128]), op=ALU.add),
                           reads=[sA, sc], writes=[sbk])
                        OP("act", lambda e: e.activation(out=exk[:], in_=sbk[:], func=AF.Exp, bias=beta[:, h:h + 1]), reads=[sbk, beta], writes=[exk])

                    def g2(n_):
                        bk, h = its[n_]
                        sbk = Sblk[n_ % NB]; exk = Exb[n_ % NB]; tg = tmpg[n_ % NB]
                        sb2 = sbk[:].rearrange("p a b -> p (a b)"); ex2 = exk[:].rearrange("p a b -> p (a b)")
                        OP("dve", lambda e: e.scalar_tensor_tensor(out=tg[:], in0=sb2, scalar=0.0, in1=ex2, op0=ALU.is_ge, op1=ALU.mult),
                           reads=[sbk, exk], writes=[tg])
                        for c in range(8):
                            pp = pg[bk][c // 4]
                            OP("pe", lambda e, c=c, pp=pp: e.matmul(pp[:, (c % 4) * 128:(c % 4 + 1) * 128], lhsT=tg[:, c * 128:(c + 1) * 128], rhs=ident[:],
                                                                   start=False, stop=(h == 7 and c % 4 == 3)), reads=[tg, ident], writes=[pp])

                    def blk_end(bk):
                        g_ = gaT[bk % 2]; w_ = WT[bk % 2]
                        for hf in range(2):
                            OP("dve", lambda e, hf=hf: e.tensor_tensor(out=w_[:, hf * 4:(hf + 1) * 4, :].rearrange("p a b -> p (a b)"), in0=g_[:, hf * 512:(hf + 1) * 512],
                                                                        in1=pg[bk][hf][:, :], op=ALU.mult), reads=[g_, pg[bk][hf]], writes=[w_])
                        for c in range(8):
                            vb_ = vb[(2 * bk + c // 4) % NR]
                            for hf in range(2):
                                OP("pe", lambda e, c=c, hf=hf, vb_=vb_: e.matmul(po[hf][:, :], lhsT=w_[:, c, :], rhs=vb_[:, c % 4, hf * 512:(hf + 1) * 512],
                                                                                start=(bk == 0 and c == 0), stop=(bk == NBK - 1 and c == 7)),
                                   reads=[w_, vb_], writes=[po[hf]])

                    ldu(0); ldu(1); ldv(0); ldv(1); ldv(2); ldv(3)
                    LAG = 2
                    pending_end = {}
                    for n_ in range(len(its) + LAG + 4):
                        if n_ < len(its):
                            bk, h = its[n_]
                            if h == 0:
                                blk_start(bk)
                                if 2 * bk + 2 < NHB:
                                    ldu(2 * bk + 2); ldu(2 * bk + 3)
                            g1(n_)
                        m_ = n_ - LAG
                        if 0 <= m_ < len(its):
                            g2(m_)
                            if its[m_][1] == 7:
                                pending_end[n_ + 3] = its[m_][0]
                        if n_ in pending_end:
                            bke = pending_end.pop(n_)
                            blk_end(bke)
                            if 2 * (bke + 2) < NHB:
                                ldv(2 * (bke + 2)); ldv(2 * (bke + 2) + 1)
                    for hf in range(2):
                        OP("act", lambda e, hf=hf: e.copy(out=otm[:, hf * 512:(hf + 1) * 512], in_=po[hf][:, :]), reads=[po[hf]], writes=[otm])
                    for m in range(KC):
                        pt = nps()
                        OP("pe", lambda e, m=m, pt=pt: e.transpose(out=pt[:, :128], in_=otm[:, m * 128:(m + 1) * 128], identity=identF[:]), reads=[otm, identF], writes=[pt])
                        OP("dve", lambda e, m=m, pt=pt: e.scalar_tensor_tensor(out=xt_[:, m, :128], in0=pt[:, :128], scalar=modv[:, 40 + m, s:s + 1], in1=xt_[:, m, :128],
                                                                               op0=ALU.mult, op1=ALU.add), reads=[pt, modv, xt_], writes=[xt_])
                    if last_layer and stage == 99:
                        norm_mod(128, fgt, zerob, 0, 0, tmpo, xt_, tmp8, sq8)
                        DMA("sp", rsview(yT_out, t0, 128), tmpo[:], reads=[tmpo], writes=[ybuf])
                    elif dyn:
                        DMA("sp", rsview(HXs[t0 // CW], t0 % CW, 128), xt_[:, :, :128], reads=[xt_], writes=[HXb[t0 // CW]])
                    else:
                        DMA("sp", rsview(R, t0, 128), xt_[:, :, :128], reads=[xt_], writes=[rb])
            if not (last_layer and stage == 99):
                if pair:
                    for j in range(NCW):
                        fw.collective("AllGather", [HXs[j].opt()], [HGs[j].opt()], [[0, 1], [2, 3], [4, 5], [6, 7]][:ncores // 2], reads=[HXb[j]], writes=[HGb[j]])
                    for j in range(NCW):
                        for r in range(2):
                            c0 = r * half + j * CW
                            DMA("sp", XR[:, c0:c0 + CW], HGs[j][r * D:(r + 1) * D, :], reads=[HGb[j]], writes=list(XRb))
                else:
                    for j in range(NCW):
                        DMA("sp", XR[:, j * CW:(j + 1) * CW], HXs[j][:, :], reads=[HXb[j]], writes=list(XRb))
            fw.barrier()
            pst.close()

        for l in range(depth):
            last_layer = (l == depth - 1)
            mst = ExitStack()

            def sbm(name, shape, dt=F32):
                return Tile(mst.enter_context(nc.sbuf_tensor(f"m{l}_" + name, list(shape), dt)), name)
            winb = sbm("winb", [128, KC, 4096], BF16)
            woutb = sbm("woutb", [128, KC, D], BF16)
            wmb = [sbm(f"wmb{i}", [128, KC, 256]) for i in range(2)]
            Sbf = [sbm(f"Sbf{i}", [128, 128], BF16) for i in range(8)]
            W = {n_: sbm("w_" + n_, [128, NT]) for n_ in ["sg", "f", "key", "lf", "b", "bc", "e1", "e2", "e3", "e4"]}
            qp = sbm("qp", [128, NT], BF16); qin = sbm("qin", [128, NT], BF16); kp = sbm("kp", [128, NT], BF16); kout = sbm("kout", [128, NT], BF16)
            vtm = sbm("vtm", [128, NT // 128, 512], BF16); kotm = sbm("kotm", [128, 4, 128], BF16)
            attm = sbm("attm", [128, 128], BF16)
            osb = sbm("osb", [128, 4, NT]); ofl = sbm("ofl", [128, 4, NT]); sgl = sbm("sgl", [128, 4, NT])
            stg = sbm("stg", [128, 4, NT])
            mix = sbm("mix", [128, KC, NT], BF16)
            uh = sbm("uh", [128, 4, NT + 128]); bgl = sbm("bgl", [128, 4, NT]); cva = sbm("cva", [128, NT])
            zbl = sbm("zbl", [128, 4, NT]); ql = sbm("ql", [128, 4, NT])
            xt = sbm("xt", [128, KC, NT]); tmp8 = sbm("tmp8", [128, KC, NT]); sq8 = sbm("sq8", [128, KC, NT], BF16)
            hT = sbm("hT", [128, KC, NT], BF16)
            alloc_psum(mst, f"m{l}", 6, 2); cnt["n"] = 6
            DMA("sp", n1t[:], n1_in[l], writes=[n1t]); DMA("sp", n2t[:], n2_in[l], writes=[n2t])
            DMA("sp", bmt[:], bmod_in[l], writes=[bmt]); DMA("sp", cwt[:], cw_in[l], writes=[cwt])
            for k in range(KC):
                DMA("pool", winb[:, k, :], win_in[l, k * 128:(k + 1) * 128, :], writes=[winb])
            for k in range(KC):
                DMA("pool", woutb[:, k, :], wout_in[l, k * 128:(k + 1) * 128, :], writes=[woutb])
            pm = nps()
            for cb in range(24):
                wm = wmb[cb % 2]
                DMA("sp", wm[:], wmod_in[l].rearrange("(k p) c -> p k c", p=128)[:, :, cb * 256:(cb + 1) * 256], writes=[wm])
                for cc in range(2):
                    c = cb * 2 + cc
                    for k in range(KC):
                        OP("pe", lambda e, wm=wm, cc=cc, c=c, k=k: e.matmul(pm[:, 2 * c:2 * c + 2], lhsT=wm[:, k, cc * 128:(cc + 1) * 128],
                                                                            rhs=cond[:, k, :], start=(k == 0), stop=(k == KC - 1)),
                           reads=[wm, cond], writes=[pm])
            OP("dve", lambda e: e.tensor_tensor(out=modv[:], in0=pm[:, 0:96].rearrange("p (c s) -> p c s", s=2),
                                                 in1=bmt[:].unsqueeze(2).to_broadcast([128, 48, 2]), op=ALU.add), reads=[pm, bmt], writes=[modv])
            for (A, nt_, g) in [(A1, n1t, 1), (A2, n2t, 4)]:
                OP("dve", lambda e, A=A, g=g: e.tensor_scalar(out=A[:], in0=modv[:, g * 8:(g + 1) * 8, :], scalar1=1.0, scalar2=None, op0=ALU.add),
                   reads=[modv], writes=[A])
                OP("dve", lambda e, A=A, nt_=nt_: e.tensor_tensor(out=A[:], in0=A[:], in1=nt_[:].unsqueeze(2).to_broadcast([128, KC, 2]), op=ALU.mult),
                   reads=[A, nt_], writes=[A])

            for sname in ["c", "x"]:
                S_ = streams[sname]
                s = S_["s"]; n = S_["n"]; ntile = S_["nt"]; R = S_["R"]; Rb = S_["Rb"]; T = S_["T"]
                nsub = n // 128
                if sname == "c":
                    OP("dve", lambda e: e.memset(Sst[:], 0.0), writes=[Sst])
                only_states = (sname == "c" and last_layer)
                for i in range(ntile):
                    t0 = i * n
                    DMA("sp", xt[:, :, :n], rsview(R, t0, n), reads=[Rb[i]], writes=[xt])
                    norm_mod(n, A1, modv, 0, s, hT, xt, tmp8, sq8)
                    for j in range(nsub):
                        pv = nps()
                        for k in range(KC):
                            OP("pe", lambda e, k=k, j=j, pv=pv: e.matmul(pv[:, :], lhsT=hT[:, k, j * 128:(j + 1) * 128], rhs=winb[:, k, 0:512],
                                                                         start=(k == 0), stop=(k == KC - 1)), reads=[hT, winb], writes=[pv])
                        OP("act", lambda e, j=j, pv=pv: e.copy(out=vtm[:, j, :], in_=pv[:, :]), reads=[pv], writes=[vtm])
                    DMA("sp", VS[t0:t0 + n, :].rearrange("(j p) c -> p j c", p=128), vtm[:, :nsub, :], reads=[vtm], writes=[scr_b["VS"][i]])

                    def proj(cc):
                        ps = nps()
                        for k in range(KC):
                            OP("pe", lambda e, k=k, ps=ps: e.matmul(ps[:, :n], lhsT=winb[:, k, cc * 128:(cc + 1) * 128], rhs=hT[:, k, :n],
                                                                    start=(k == 0), stop=(k == KC - 1)), reads=[hT, winb], writes=[ps])
                        return ps
                    for h in range(4):
                        ps = proj(8 + h)
                        OP("act", lambda e, h=h, ps=ps: e.copy(out=stg[:, h, :n], in_=ps[:, :n]), reads=[ps], writes=[stg])
                    DMA("sp", s4view(ZB, t0, n), stg[:, :, :n], reads=[stg], writes=[scr_b["ZB"][i]])
                    for h in range(4):
                        psq = proj(12 + h)
                        OP("act", lambda e, h=h, psq=psq: e.copy(out=ql[:, h, :n], in_=psq[:, :n]), reads=[psq], writes=[ql])
                        psz = proj(4 + h)
                        zps_t[0] = psz
                        last = gate_math(psz[:, :n], ql[:, h, :n], ql, l, 0, h, n, False)
                        recur(l, 0, h, n, False, last, osb)
                    DMA("sp", s4view(QS, t0, n), ql[:, :, :n], reads=[ql], writes=[scr_b["QS"][i]])
                    DMA("sp", s4view(OF, t0, n), osb[:, :, :n], reads=[osb], writes=[scr_b["OF"][i]])
                    if only_states:
                        continue
                    for h in range(4):
                        ps = proj(16 + h)
                        OP("act", lambda e, h=h, ps=ps: e.activation(out=stg[:, h, :n], in_=ps[:, :n], func=AF.Silu), reads=[ps], writes=[stg])
                    DMA("sp", s4view(SG, t0, n), stg[:, :, :n], reads=[stg], writes=[scr_b["SG"][i]])
                    for c in range(4):
                        pc = proj(20 + c)
                        OP("act", lambda e, pc=pc: e.copy(out=cva[:, :n], in_=pc[:, :n]), reads=[pc], writes=[cva])
                        ph = proj(28 + c)
                        OP("dve", lambda e, c=c, ph=ph: e.tensor_tensor(out=stg[:, c, :n], in0=ph[:, :n], in1=cva[:, :n], op=ALU.mult),
                           reads=[ph, cva], writes=[stg])
                    DMA("sp", s4view(US, t0, n), stg[:, :, :n], reads=[stg], writes=[scr_b["US"][i]])
                    for c in range(4):
                        pb = proj(24 + c)
                        OP("act", lambda e, c=c, pb=pb: e.copy(out=stg[:, c, :n], in_=pb[:, :n]), reads=[pb], writes=[stg])
                    DMA("sp", s4view(BG, t0, n), stg[:, :, :n], reads=[stg], writes=[scr_b["BG"][i]])
                for i in range(ntile - 1, -1, -1):
                    t0 = i * n
                    DMA("sp", zbl[:, :, :n], s4view(ZB, t0, n), reads=[scr_b["ZB"][i]], writes=[zbl])
                    DMA("sp", ql[:, :, :n], s4view(QS, t0, n), reads=[scr_b["QS"][i]], writes=[ql])
                    DMA("sp", vtm[:, :nsub, :], VS[t0:t0 + n, :].rearrange("(j p) c -> p j c", p=128), reads=[scr_b["VS"][i]], writes=[vtm])
                    for h in range(4):
                        zps_t[0] = zbl
                        last = gate_math(zbl[:, h, :n], ql[:, h, :n], ql, l, 1, h, n, True)
                        recur(l, 1, h, n, True, last, osb)
                    if only_states:
                        continue
                    DMA("sp", ofl[:, :, :n], s4view(OF, t0, n), reads=[scr_b["OF"][i]], writes=[ofl])
                    DMA("sp", sgl[:, :, :n], s4view(SG, t0, n), reads=[scr_b["SG"][i]], writes=[sgl])
                    DMA("sp", bgl[:, :, :n], s4view(BG, t0, n), reads=[scr_b["BG"][i]], writes=[bgl])
                    OP("pool", lambda e: e.memset(uh[:], 0.0), writes=[uh])
                    lo = 64 if t0 > 0 else 0
                    hi = 64 if t0 + n < T else 0
                    rd = [scr_b["US"][i]] + ([scr_b["US"][i - 1]] if lo else []) + ([scr_b["US"][i + 1]] if hi else [])
                    DMA("sp", uh[:, :, 64 - lo:64 + n + hi], s4view(US, t0 - lo, n + lo + hi), reads=rd, writes=[uh])
                    DMA("sp", xt[:, :, :n], rsview(R, t0, n), reads=[Rb[i]], writes=[xt])
                    OP("dve", lambda e: e.tensor_tensor(out=osb[:, :, :n], in0=osb[:, :, :n], in1=ofl[:, :, :n], op=ALU.add), reads=[osb, ofl], writes=[osb])
                    OP("act", lambda e: e.activation(out=sq8[:, 0:4, :n], in_=osb[:, :, :n], func=AF.Square), reads=[osb], writes=[sq8])
                    for h in range(4):
                        ps = nps()
                        OP("pe", lambda e, h=h, ps=ps: e.matmul(ps[:, :n], lhsT=ones[:], rhs=sq8[:, h, :n], start=True, stop=True), reads=[ones, sq8], writes=[ps])
                        OP("act", lambda e, ps=ps: e.activation(out=rstd[:, :n], in_=ps[:, :n], func=AF.Sqrt, scale=1.0 / 128, bias=epsc[:]),
                           reads=[ps, epsc], writes=[rstd])
                        OP("dve", lambda e: e.reciprocal(out=rstd[:, :n], in_=rstd[:, :n]), reads=[rstd], writes=[rstd])
                        OP("dve", lambda e, h=h: e.tensor_tensor(out=osb[:, h, :n], in0=osb[:, h, :n], in1=rstd[:, :n], op=ALU.mult), reads=[osb, rstd], writes=[osb])
                        OP("dve", lambda e, h=h: e.tensor_tensor(out=mix[:, h, :n], in0=osb[:, h, :n], in1=sgl[:, h, :n], op=ALU.mult), reads=[osb, sgl], writes=[mix])
                    for c in range(4):
                        ctr = uh[:, c, 64:64 + n]
                        OP("dve", lambda e, c=c, ctr=ctr: e.tensor_scalar(out=cva[:, :n], in0=ctr, scalar1=cwt[:, c, 1:2], scalar2=None, op0=ALU.mult),
                           reads=[uh, cwt], writes=[cva])
                        if sname == "x" and c >= 2:
                            OP("dve", lambda e, c=c: e.scalar_tensor_tensor(out=cva[:, :n], in0=uh[:, c, 0:n], scalar=cwt[:, c, 0:1], in1=cva[:, :n],
                                                                            op0=ALU.mult, op1=ALU.add), reads=[uh, cwt, cva], writes=[cva])
                            OP("dve", lambda e, c=c: e.scalar_tensor_tensor(out=cva[:, :n], in0=uh[:, c, 128:128 + n], scalar=cwt[:, c, 2:3], in1=cva[:, :n],
                                                                            op0=ALU.mult, op1=ALU.add), reads=[uh, cwt, cva], writes=[cva])
                        elif sname == "x":
                            u3 = uh[:, c, 64:64 + n].rearrange("p (r w) -> p r w", w=64)
                            c3 = cva[:, :n].rearrange("p (r w) -> p r w", w=64)
                            OP("dve", lambda e, c=c, u3=u3, c3=c3: e.scalar_tensor_tensor(out=c3[:, :, 1:64], in0=u3[:, :, 0:63], scalar=cwt[:, c, 0:1], in1=c3[:, :, 1:64],
                                                                                        op0=ALU.mult, op1=ALU.add), reads=[uh, cwt, cva], writes=[cva])
                            OP("dve", lambda e, c=c, u3=u3, c3=c3: e.scalar_tensor_tensor(out=c3[:, :, 0:63], in0=u3[:, :, 1:64], scalar=cwt[:, c, 2:3], in1=c3[:, :, 0:63],
                                                                                        op0=ALU.mult, op1=ALU.add), reads=[uh, cwt, cva], writes=[cva])
                        else:
                            OP("dve", lambda e, c=c: e.scalar_tensor_tensor(out=cva[:, :n], in0=uh[:, c, 63:63 + n], scalar=cwt[:, c, 0:1], in1=cva[:, :n],
                                                                            op0=ALU.mult, op1=ALU.add), reads=[uh, cwt, cva], writes=[cva])
                            OP("dve", lambda e, c=c: e.scalar_tensor_tensor(out=cva[:, :n], in0=uh[:, c, 65:65 + n], scalar=cwt[:, c, 2:3], in1=cva[:, :n],
                                                                            op0=ALU.mult, op1=ALU.add), reads=[uh, cwt, cva], writes=[cva])
                        OP("dve", lambda e, c=c: e.tensor_tensor(out=mix[:, 4 + c, :n], in0=cva[:, :n], in1=bgl[:, c, :n], op=ALU.mult), reads=[cva, bgl], writes=[mix])
                    for m in range(KC):
                        ps = nps()
                        for fch in range(KC):
                            OP("pe", lambda e, m=m, fch=fch, ps=ps: e.matmul(ps[:, :n], lhsT=woutb[:, fch, m * 128:(m + 1) * 128], rhs=mix[:, fch, :n],
                                                                             start=(fch == 0), stop=(fch == KC - 1)), reads=[woutb, mix], writes=[ps])
                        OP("dve", lambda e, m=m, ps=ps: e.scalar_tensor_tensor(out=xt[:, m, :n], in0=ps[:, :n], scalar=modv[:, 16 + m, s:s + 1], in1=xt[:, m, :n],
                                                                               op0=ALU.mult, op1=ALU.add), reads=[ps, modv, xt], writes=[xt])
                    DMA("sp", rsview(R, t0, n), xt[:, :, :n], reads=[xt], writes=[Rb[i]])
            fw.barrier()
            mst.close()
            if stage == 1:
                break
            peer_layer(l, last_layer)
            if stage == 2:
                break

        if stage in (1, 2):
            xt = sb("xo", [128, KC, NT])
            for i in range(seq // NT):
                DMA("sp", xt[:, :, :NT], rsview(XR, i * NT, NT), reads=[XRb[i]], writes=[xt])
                DMA("sp", rsview(yT_out, i * NT, NT), xt[:, :, :NT], reads=[xt], writes=[dbuf("y")])
            if dbg:
                DMA("sp", xt[:, :, :ctxlen], rsview(CR, 0, ctxlen), reads=[CRb[0]], writes=[xt])
                DMA("sp", rsview(dbgc_out, 0, ctxlen), xt[:, :, :ctxlen], reads=[xt], writes=[dbuf("y2")])
        fw.finish()
        print("instructions:", fw.ninst, {k: v.count for k, v in fw.E.items()})
    return nc


def make_consts():
    bf = ml_dtypes.bfloat16
    ident = np.eye(128, dtype=np.float32).astype(bf)
    ones = np.ones((128, 128), np.float32).astype(bf)
    s = np.arange(128)[:, None]; t = np.arange(128)[None, :]
    same = (s // CH) == (t // CH)
    maskF = (same & (s <= t)).astype(np.float32).astype(bf)
    maskB = (same & (s >= t)).astype(np.float32).astype(bf)
    resetm = np.ones((128, NT), np.float32); resetm[:, ::CH] = 0.0
    rowm = (np.arange(128)[:, None] // CH == np.arange(4)[None, :]).astype(np.float32)
    return dict(identF=np.eye(128, dtype=np.float32), ident=ident, ones=ones, maskF=maskF, maskB=maskB, resetm=resetm, rowm=rowm)


def prep_shared(inp, depth):
    f = lambda a: np.ascontiguousarray(np.asarray(a, dtype=np.float32))
    sh = {}
    sh["w_mod"] = f(inp["w_mod"])
    sh["b_mod"] = f(np.asarray(inp["b_mod"]).reshape(depth, 48, 128).transpose(0, 2, 1))
    sh["n1"] = f(np.asarray(inp["norm1_g"]).reshape(depth, KC, 128).transpose(0, 2, 1))
    sh["n2"] = f(np.asarray(inp["norm2_g"]).reshape(depth, KC, 128).transpose(0, 2, 1))
    sh["fg"] = f(np.repeat(np.asarray(inp["final_g"]).reshape(KC, 128).T[:, :, None], 2, axis=2))
    sh["w_in"] = f(inp["w_in"]); sh["w_out"] = f(inp["w_out"])
    sh["cw"] = f(np.asarray(inp["conv_w"]).reshape(depth, 3, 4, 128).transpose(0, 3, 2, 1))
    sh["lbl"] = f(np.asarray(inp["lb_logits"]).reshape(depth, 2, 4, 128).transpose(3, 0, 1, 2))
    sh["wq"] = f(inp["peer_wq"])
    sh["skT"] = f(np.asarray(inp["peer_subkeys"]).reshape(depth, 16, 128, 128).transpose(0, 3, 1, 2))
    sh["uT"] = f(np.asarray(inp["peer_u"]).transpose(0, 2, 1))
    sh["v"] = f(inp["peer_v"])
    sh.update(make_consts())
    return sh


def prep_core(inp, b, parity=0, half=0):
    f = lambda a: np.ascontiguousarray(np.asarray(a, dtype=np.float32))
    m = {"par": np.array([[parity * half]], np.int32)}
    m["xT"] = f(np.asarray(inp["x"])[b].T)
    m["cxT"] = f(np.asarray(inp["ctx"])[b].T)
    cv = np.stack([np.asarray(inp["c"])[b], np.asarray(inp["c_ctx"])], axis=-1)
    m["cv"] = f(cv.reshape(KC, 128, 2).transpose(1, 0, 2))
    return m


def kernel(**inp):
    x = np.asarray(inp["x"])
    B, seq, _ = x.shape
    ctxlen = np.asarray(inp["ctx"]).shape[1]
    depth = np.asarray(inp["w_in"]).shape[0]
    nc = build(depth, seq, ctxlen, ncores=2 * B)
    sh = prep_shared(inp, depth)
    half = seq // 2
    in_maps = []
    for c in range(2 * B):
        m = dict(sh); m.update(prep_core(inp, c // 2, c % 2, half)); in_maps.append(m)
    res = run_bass_kernel_spmd(nc, in_maps, core_ids=list(range(2 * B)))
    out = np.empty((B, seq, D), np.float32)
    for c in range(2 * B):
        out[c // 2, (c % 2) * half:(c % 2 + 1) * half, :] = res.results[c]["yT"].T
    return out
```

```python
import numpy as np
import ml_dtypes
from contextlib import ExitStack
import concourse.bass as bass
import concourse.mybir as mybir
from concourse.bass_utils import run_bass_kernel_spmd

F32 = mybir.dt.float32
BF16 = mybir.dt.bfloat16
AF = mybir.ActivationFunctionType
ALU = mybir.AluOpType
AX = mybir.AxisListType

D = 1024
KC = 8
NT = 256
NP = 256
CH = 32
EPS = 1e-6
EPOCH = 30000
NDMA = 12
NEXP = 16384


class Tok:
    __slots__ = ("sem", "val", "eng")

    def __init__(self, sem, val, eng):
        self.sem = sem; self.val = val; self.eng = eng


class Buf:
    def __init__(self, name=""):
        self.name = name; self.w = None; self.r = {}


class Eng:
    def __init__(self, name, obj):
        self.name = name; self.obj = obj
        self.sems = []; self.count = 0; self.seen = {}
        self.dma_sems = []; self.dma_n = 0


class FW:
    def __init__(self, nc, stack):
        self.nc = nc; self.stack = stack
        self.E = {n: Eng(n, getattr(nc, o)) for n, o in
                  [("pe", "tensor"), ("dve", "vector"), ("act", "scalar"), ("pool", "gpsimd"), ("sp", "sync")]}
        self.ninst = 0

    def newsem(self, name):
        return self.stack.enter_context(self.nc.semaphore(name))

    def _wait(self, e, tok):
        if tok is None:
            return
        k = id(tok.sem)
        if e.seen.get(k, 0) >= tok.val:
            return
        e.obj.wait_ge(tok.sem, tok.val)
        e.seen[k] = tok.val

    def _deps(self, e, reads, writes):
        for b in reads:
            if b.w is not None:
                self._wait(e, b.w)
        for b in writes:
            if b.w is not None and b.w.eng != e.name:
                self._wait(e, b.w)
            for en, t in b.r.items():
                if en != e.name:
                    self._wait(e, t)

    def _commit(self, tok, reads, writes, rkey):
        for b in reads:
            b.r[rkey] = tok
        for b in writes:
            b.w = tok; b.r = {}

    def op(self, eng, fn, reads=(), writes=()):
        e = self.E[eng]
        self._deps(e, reads, writes)
        ep = e.count // EPOCH
        while len(e.sems) <= ep:
            e.sems.append(self.newsem(f"s_{eng}_{len(e.sems)}"))
        ins = fn(e.obj)
        val = e.count - ep * EPOCH + 1
        ins.then_inc(e.sems[ep], 1)
        e.count += 1
        self.ninst += 1
        tok = Tok(e.sems[ep], val, eng)
        self._commit(tok, reads, writes, eng)
        return tok

    def dma(self, eng, out, in_, reads=(), writes=(), **kw):
        e = self.E[eng]
        self._deps(e, reads, writes)
        j = e.dma_n % NDMA
        if len(e.dma_sems) <= j:
            e.dma_sems.append([self.newsem(f"d_{eng}_{j}"), 0])
        slot = e.dma_sems[j]
        if slot[1] > 0:
            self._wait(e, Tok(slot[0], slot[1], "dma"))
        slot[1] += 16
        e.obj.dma_start(out=out, in_=in_, **kw).then_inc(slot[0], 16)
        e.dma_n += 1
        self.ninst += 1
        tok = Tok(slot[0], slot[1], "dma")
        self._commit(tok, reads, writes, f"dma_{eng}_{j}")
        return tok

    def raw(self, eng, fn, reads=(), writes=()):
        e = self.E[eng]
        self._deps(e, reads, writes)
        return fn(e.obj)

    def collective(self, kind, ins, outs, groups, reads=(), writes=()):
        e = self.E["pool"]
        self._deps(e, reads, writes)
        if not hasattr(self, "cc_sem"):
            self.cc_sem = self.newsem("cc_sem"); self.cc_n = 0
        self.cc_n += 1
        e.obj.collective_compute(kind, mybir.AluOpType.bypass, replica_groups=groups, ins=ins, outs=outs).then_inc(self.cc_sem, 1)
        tok = Tok(self.cc_sem, self.cc_n, "cc")
        self._commit(tok, reads, writes, "cc")
        return tok

    def barrier(self):
        toks = []
        for en in self.E.values():
            if en.count > 0:
                ep = (en.count - 1) // EPOCH
                toks.append(Tok(en.sems[ep], en.count - ep * EPOCH, en.name))
            for slot in en.dma_sems:
                if slot[1] > 0:
                    toks.append(Tok(slot[0], slot[1], "dma"))
        if hasattr(self, "cc_sem") and self.cc_n > 0:
            toks.append(Tok(self.cc_sem, self.cc_n, "cc"))
        for e in self.E.values():
            for t in toks:
                if t.eng != e.name:
                    self._wait(e, t)

    def finish(self):
        e = self.E["sp"]
        for en in self.E.values():
            for slot in en.dma_sems:
                if slot[1] > 0:
                    self._wait(e, Tok(slot[0], slot[1], "dma"))


class Tile:
    def __init__(self, t, name):
        self.t = t; self.b = Buf(name)

    def __getitem__(self, k):
        return self.t[k]


def build(depth, seq, ctxlen, stage=99, dbg=False, pair=True, ncores=8):
    nc = bass.Bass("TRN2", target_bir_lowering=False)
    assert seq % NT == 0 and ctxlen <= NT and ctxlen % 128 == 0 and seq % NP == 0
    rows_per_tile = NT // 64

    def din(name, shape, dt=F32):
        return nc.dram_tensor(name, list(shape), dt, kind="ExternalInput").ap()

    def dscr(name, shape, dt=F32):
        return nc.dram_tensor(name, list(shape), dt).ap()

    xT_in = din("xT", [D, seq]); cxT_in = din("cxT", [D, ctxlen])
    cv_in = din("cv", [128, KC, 2])
    wmod_in = din("w_mod", [depth, D, 6 * D]); bmod_in = din("b_mod", [depth, 128, 48])
    n1_in = din("n1", [depth, 128, KC]); n2_in = din("n2", [depth, 128, KC]); fg_in = din("fg", [128, KC, 2])
    win_in = din("w_in", [depth, D, 4096]); wout_in = din("w_out", [depth, D, D])
    cw_in = din("cw", [depth, 128, 4, 3]); lbl_in = din("lbl", [128, depth, 2, 4])
    wq_in = din("wq", [depth, D, 2048]); skT_in = din("skT", [depth, 128, 16, 128])
    uT_in = din("uT", [depth, D, NEXP]); v_in = din("v", [depth, NEXP, D])
    ident_in = din("ident", [128, 128], BF16); ones_in = din("ones", [128, 128], BF16)
    maskF_in = din("maskF", [128, 128], BF16); maskB_in = din("maskB", [128, 128], BF16)
    identF_in = din("identF", [128, 128]); reset_in = din("resetm", [128, NT]); rowm_in = din("rowm", [128, 4])
    half = seq // 2 if pair else seq
    I32 = mybir.dt.int32
    par_in = din("par", [1, 1], I32)
    yT_out = nc.dram_tensor("yT", [D, half if stage == 99 else seq], F32, kind="ExternalOutput").ap()
    dbg_out = nc.dram_tensor("dbg", [D, seq], F32, kind="ExternalOutput").ap() if dbg else None
    dbgc_out = nc.dram_tensor("dbgc", [D, ctxlen], F32, kind="ExternalOutput").ap() if dbg else None

    TM = max(seq, ctxlen)
    XRh = nc.dram_tensor("XR", [D, seq], F32); XR = XRh.ap(); CR = dscr("CR", [D, ctxlen])
    CW = min(512, half); NCW = half // CW
    HXs = [dscr(f"HX{j}", [D, CW]) for j in range(NCW)]; HGs = [dscr(f"HG{j}", [2 * D, CW]) for j in range(NCW)]
    HXb = [Buf(f"HX{j}") for j in range(NCW)]; HGb = [Buf(f"HG{j}") for j in range(NCW)]
    ZB = dscr("ZB", [512, TM]); QS = dscr("QS", [512, TM]); SG = dscr("SG", [512, TM])
    OF = dscr("OF", [512, TM]); US = dscr("US", [512, TM]); BG = dscr("BG", [512, TM])
    VS = dscr("VS", [TM, 512], BF16)
    U16 = dscr("U16", [NEXP // 512, 128, KC, 512], BF16); V16 = dscr("V16", [NEXP, D], BF16)
    U16b = Buf("U16"); V16b = Buf("V16")

    with ExitStack() as st:
        fw = FW(nc, st)

        def sb(name, shape, dt=F32):
            return Tile(st.enter_context(nc.sbuf_tensor("sb_" + name, list(shape), dt)), name)

        def dbuf(name):
            return Buf(name)

        PS = {"F": [], "B": []}
        cnt = {"f": 0, "b": 0}

        def alloc_psum(stack, tag, nf, nb):
            PS["F"] = [Tile(stack.enter_context(nc.psum_tensor(f"psF{tag}_{i}", [128, 512], F32)), f"psF{i}") for i in range(nf)]
            PS["B"] = [Tile(stack.enter_context(nc.psum_tensor(f"psB{tag}_{i}", [128, 1024], BF16)), f"psB{i}") for i in range(nb)]

        def nps():
            cnt["f"] += 1
            return PS["F"][cnt["f"] % cnt["n"]]

        def npsb():
            cnt["b"] += 1
            return PS["B"][cnt["b"] % 2]

        def OP(eng, fn, reads=(), writes=()):
            return fw.op(eng, fn, reads=[x.b if hasattr(x, "b") else x for x in reads],
                         writes=[x.b if hasattr(x, "b") else x for x in writes])

        def DMA(eng, out, in_, reads=(), writes=(), **kw):
            return fw.dma(eng, out, in_, reads=[x.b if hasattr(x, "b") else x for x in reads],
                          writes=[x.b if hasattr(x, "b") else x for x in writes], **kw)

        ident = sb("ident", [128, 128], BF16); ones = sb("ones", [128, 128], BF16)
        maskF = sb("maskF", [128, 128], BF16); maskB = sb("maskB", [128, 128], BF16)
        resetm = sb("resetm", [128, NT]); rowm = sb("rowm", [128, 4])
        DMA("sp", rowm[:], rowm_in[:, :], writes=[rowm])
        for tl, src in [(ident, ident_in), (ones, ones_in), (maskF, maskF_in), (maskB, maskB_in), (resetm, reset_in)]:
            DMA("sp", tl[:], src[:, :], writes=[tl])
        part = Tile(st.enter_context(nc.sbuf_tensor("sb_part", [1, 1], I32)), "part")
        preg = st.enter_context(nc.sync.register("preg"))
        DMA("sp", part[:], par_in[:, :], writes=[part])
        fw.raw("sp", lambda e: e.reg_load(preg, part[:1, :1]), reads=[part.b])
        pval = nc.sync.snap(preg)
        XRdyn = bass.AP(XRh, pval, [[seq, 128], [128 * seq, KC], [1, half]])
        cond = sb("cond", [128, KC, 2])
        DMA("sp", cond[:], cv_in[:, :, :], writes=[cond])
        OP("act", lambda e: e.activation(out=cond[:], in_=cond[:], func=AF.Silu), reads=[cond], writes=[cond])
        epsc = sb("epsc", [128, 1])
        OP("dve", lambda e: e.memset(epsc[:], EPS), writes=[epsc])
        lbe = sb("lbe", [128, depth, 8]); lbs = sb("lbs", [128, 8]); lb = sb("lb", [128, depth, 8]); oml = sb("oml", [128, depth, 8])
        DMA("sp", lbe[:], lbl_in.rearrange("p l a b -> p l (a b)"), writes=[lbe])
        OP("act", lambda e: e.activation(out=lbe[:], in_=lbe[:], func=AF.Exp), reads=[lbe], writes=[lbe])
        OP("dve", lambda e: e.tensor_copy(out=lbs[:], in_=lbe[:, 0, :]), reads=[lbe], writes=[lbs])
        for l in range(1, depth):
            OP("dve", lambda e, l=l: e.tensor_tensor(out=lbs[:], in0=lbs[:], in1=lbe[:, l, :], op=ALU.add), reads=[lbs, lbe], writes=[lbs])
        OP("dve", lambda e: e.reciprocal(out=lbs[:], in_=lbs[:]), reads=[lbs], writes=[lbs])
        OP("dve", lambda e: e.memset(lb[:, 0, :], 0.0), writes=[lb])
        for l in range(1, depth):
            OP("dve", lambda e, l=l: e.tensor_tensor(out=lbe[:, l, :], in0=lbe[:, l, :], in1=lbs[:], op=ALU.mult), reads=[lbe, lbs], writes=[lbe])
            OP("dve", lambda e, l=l: e.tensor_tensor(out=lb[:, l, :], in0=lb[:, l - 1, :], in1=lbe[:, l, :], op=ALU.add), reads=[lb, lbe], writes=[lb])
        OP("dve", lambda e: e.tensor_scalar(out=oml[:], in0=lb[:], scalar1=-1.0, scalar2=1.0, op0=ALU.mult, op1=ALU.add), reads=[lb], writes=[oml])

        XRb = [dbuf(f"XR{i}") for i in range(seq // NT)]
        CRb = [dbuf("CR")]
        for i in range(seq // NT):
            DMA("sp", XR[:, i * NT:(i + 1) * NT], xT_in[:, i * NT:(i + 1) * NT], writes=[XRb[i]])
        DMA("sp", CR[:, :], cxT_in[:, :], writes=[CRb[0]])

        modv = sb("modv", [128, 48, 2])
        A1 = sb("A1", [128, KC, 2]); A2 = sb("A2", [128, KC, 2])
        n1t = sb("n1t", [128, KC]); n2t = sb("n2t", [128, KC]); bmt = sb("bmt", [128, 48]); cwt = sb("cwt", [128, 4, 3])
        fgt = sb("fgt", [128, KC, 2]); zerob = sb("zerob", [128, KC, 2])
        DMA("sp", fgt[:], fg_in[:, :, :], writes=[fgt])
        OP("dve", lambda e: e.memset(zerob[:], 0.0), writes=[zerob])
        identF = sb("identF", [128, 128])
        DMA("sp", identF[:], identF_in[:, :], writes=[identF])
        Sst = sb("Sst", [128, 8, 128])
        rstd = sb("rstd", [128, NT])

        streams = {
            "c": dict(T=ctxlen, R=CR, Rb=CRb, s=1, nt=1, n=ctxlen),
            "x": dict(T=seq, R=XR, Rb=XRb, s=0, nt=seq // NT, n=NT),
        }
        scr_b = {nm: [dbuf(f"{nm}{i}") for i in range(TM // min(NT, ctxlen) + 1)] for nm in ["ZB", "QS", "SG", "OF", "US", "BG", "VS"]}

        def rsview(R, t0, n):
            return R.rearrange("(k p) t -> p k t", p=128)[:, :, t0:t0 + n]

        def s4view(S, t0, n):
            return S.rearrange("(k p) t -> p k t", p=128)[:, :, t0:t0 + n]

        def norm_mod(n, A, Bt, Bm_col0, s, out_bf, xin, tmp8, sq8):
            OP("act", lambda e: e.activation(out=sq8[:, :, :n], in_=xin[:, :, :n], func=AF.Square), reads=[xin], writes=[sq8])
            ps = nps()
            for k in range(KC):
                OP("pe", lambda e, k=k: e.matmul(ps[:, :n], lhsT=ones[:], rhs=sq8[:, k, :n], start=(k == 0), stop=(k == KC - 1)),
                   reads=[ones, sq8], writes=[ps])
            OP("act", lambda e: e.activation(out=rstd[:, :n], in_=ps[:, :n], func=AF.Sqrt, scale=1.0 / D, bias=epsc[:]), reads=[ps, epsc], writes=[rstd])
            OP("dve", lambda e: e.reciprocal(out=rstd[:, :n], in_=rstd[:, :n]), reads=[rstd], writes=[rstd])
            OP("dve", lambda e: e.tensor_tensor(out=tmp8[:, :, :n], in0=xin[:, :, :n],
                                                 in1=rstd[:, :n].unsqueeze(1).to_broadcast([128, KC, n]), op=ALU.mult),
               reads=[xin, rstd], writes=[tmp8])
            for k in range(KC):
                OP("act", lambda e, k=k: e.activation(out=out_bf[:, k, :n], in_=tmp8[:, k, :n], func=AF.Identity,
                                                       scale=A[:, k, s:s + 1], bias=Bt[:, Bm_col0 + k, s:s + 1]),
                   reads=[tmp8, A, Bt], writes=[out_bf])

        def gate_math(zps, qsrc_ap, qsrc_t, l, d, h, n, bwd):
            nchk = n // CH
            sg, f, key, lf, b, bc, e1, e2, e3, e4 = (W[x] for x in ["sg", "f", "key", "lf", "b", "bc", "e1", "e2", "e3", "e4"])
            col = d * 4 + h
            OP("act", lambda e: e.activation(out=sg[:, :n], in_=zps, func=AF.Sigmoid), reads=[zps_t[0]], writes=[sg])
            OP("dve", lambda e: e.tensor_scalar(out=f[:, :n], in0=sg[:, :n], scalar1=oml[:, l, col:col + 1], scalar2=lb[:, l, col:col + 1],
                                                 op0=ALU.mult, op1=ALU.add), reads=[sg, oml, lb], writes=[f])
            OP("pool", lambda e: e.tensor_scalar(out=f[:, :n], in0=f[:, :n], scalar1=1e-20, scalar2=None, op0=ALU.max), reads=[f], writes=[f])
            OP("pool", lambda e: e.tensor_scalar(out=key[:, :n], in0=f[:, :n], scalar1=-1.0, scalar2=1.0, op0=ALU.mult, op1=ALU.add),
               reads=[f], writes=[key])
            OP("act", lambda e: e.activation(out=lf[:, :n], in_=f[:, :n], func=AF.Ln), reads=[f], writes=[lf])
            OP("dve", lambda e: e.tensor_tensor_scan(out=b[:, :n], data0=resetm[:, :n], data1=lf[:, :n], initial=0.0, op0=ALU.mult, op1=ALU.add),
               reads=[resetm, lf], writes=[b])
            b3 = b[:, :n].rearrange("p (c t) -> p c t", t=CH)
            if bwd:
                lf3 = lf[:, :n].rearrange("p (c t) -> p c t", t=CH)
                bc3 = bc[:, :n].rearrange("p (c t) -> p c t", t=CH)
                OP("dve", lambda e: e.tensor_tensor(out=bc3, in0=b3[:, :, CH - 1:CH].to_broadcast([128, nchk, CH]), in1=b3, op=ALU.subtract),
                   reads=[b], writes=[bc])
                OP("dve", lambda e: e.tensor_tensor(out=b[:, :n], in0=bc[:, :n], in1=lf[:, :n], op=ALU.add), reads=[bc, lf], writes=[b])
                mid = CH // 2; last = 0
            else:
                mid = CH // 2 - 1; last = CH - 1
            bc3 = bc[:, :n].rearrange("p (c t) -> p c t", t=CH)
            OP("dve", lambda e: e.tensor_tensor(out=bc3, in0=b3, in1=b3[:, :, mid:mid + 1].to_broadcast([128, nchk, CH]), op=ALU.subtract),
               reads=[b], writes=[bc])
            OP("act", lambda e: e.activation(out=e1[:, :n], in_=bc[:, :n], func=AF.Exp), reads=[bc], writes=[e1])
            OP("act", lambda e: e.activation(out=e2[:, :n], in_=bc[:, :n], func=AF.Exp, scale=-1.0), reads=[bc], writes=[e2])
            OP("act", lambda e: e.activation(out=e3[:, :n], in_=b[:, :n], func=AF.Exp), reads=[b], writes=[e3])
            OP("dve", lambda e: e.tensor_tensor(out=bc3, in0=b3[:, :, last:last + 1].to_broadcast([128, nchk, CH]), in1=b3, op=ALU.subtract),
               reads=[b, e1, e2], writes=[bc])
            OP("act", lambda e: e.activation(out=e4[:, :n], in_=bc[:, :n], func=AF.Exp), reads=[bc], writes=[e4])
            OP("dve", lambda e: e.tensor_tensor(out=qp[:, :n], in0=qsrc_ap, in1=e1[:, :n], op=ALU.mult), reads=[qsrc_t, e1], writes=[qp])
            OP("pool", lambda e: e.tensor_tensor(out=qin[:, :n], in0=qsrc_ap, in1=e3[:, :n], op=ALU.mult), reads=[qsrc_t, e3], writes=[qin])
            OP("dve", lambda e: e.tensor_tensor(out=kp[:, :n], in0=key[:, :n], in1=e2[:, :n], op=ALU.mult), reads=[key, e2], writes=[kp])
            OP("pool", lambda e: e.tensor_tensor(out=kout[:, :n], in0=key[:, :n], in1=e4[:, :n], op=ALU.mult), reads=[key, e4], writes=[kout])
            return last

        zps_t = [None]

        def recur(l, d, h, n, bwd, last, o_dst):
            si = d * 4 + h
            e3 = W["e3"]
            nsub = n // 128
            subs = list(range(nsub - 1, -1, -1)) if bwd else list(range(nsub))
            chunks = list(range(3, -1, -1)) if bwd else list(range(4))
            mask = maskB if bwd else maskF
            order = [(j, c) for j in subs for c in chunks]
            nck = len(order)
            hc = slice(h * 128, (h + 1) * 128)
            for j in subs:
                js = slice(j * 128, (j + 1) * 128)
                pt = npsb()
                OP("pe", lambda e: e.transpose(out=pt[:, 0:128], in_=kout[:, js], identity=ident[:]), reads=[kout, ident], writes=[pt])
                for c4 in range(4):
                    OP("act", lambda e, c4=c4: e.activation(out=kotm2[:, j, c4, :], in_=pt[:, 0:128], func=AF.Identity, scale=rowm[:, c4:c4 + 1]),
                       reads=[pt, rowm], writes=[kotm2])
                pa = nps()
                OP("pe", lambda e: e.matmul(pa[:, 0:128], lhsT=kp[:, js], rhs=qp[:, js], start=True, stop=True), reads=[kp, qp], writes=[pa])
                OP("dve", lambda e: e.tensor_tensor(out=attm2[:, j, :], in0=pa[:, 0:128], in1=mask[:], op=ALU.mult), reads=[pa, mask], writes=[attm2])
            pSb = [nps() for _ in range((nck + 3) // 4)]
            for ci, (j, c) in enumerate(order):
                pp = pSb[ci // 4]
                OP("pe", lambda e, ci=ci, j=j, c=c, pp=pp: e.matmul(pp[:, (ci % 4) * 128:(ci % 4 + 1) * 128], lhsT=kotm2[:, j, c, :], rhs=vtm[:, j, hc],
                                                                    start=True, stop=True), reads=[kotm2, vtm], writes=[pp])
            snap = snaps[rc[0] % 2]; sb16 = snapb[rc[0] % 2]; rc[0] += 1
            OP("dve", lambda e: e.tensor_copy(out=snap[:, 0, :], in_=Sst[:, si, :]), reads=[Sst], writes=[snap])
            for ci, (j, c) in enumerate(order):
                pp = pSb[ci // 4]
                dcol = j * 128 + c * CH + last
                lastc = (ci == nck - 1)
                out_ap = Sst[:, si, :] if lastc else snap[:, ci + 1, :]
                OP("dve", lambda e, ci=ci, pp=pp, dcol=dcol, out_ap=out_ap: e.scalar_tensor_tensor(
                    out=out_ap, in0=snap[:, ci, :], scalar=e3[:, dcol:dcol + 1], in1=pp[:, (ci % 4) * 128:(ci % 4 + 1) * 128], op0=ALU.mult, op1=ALU.add),
                   reads=[snap, e3, pp], writes=[Sst] if lastc else [snap])
            OP("act", lambda e: e.copy(out=sb16[:, 0:nck, :], in_=snap[:, 0:nck, :]), reads=[snap], writes=[sb16])
            for j in subs:
                js = slice(j * 128, (j + 1) * 128)
                po = nps()
                for c in chunks:
                    ci = order.index((j, c))
                    cs = slice(c * CH, (c + 1) * CH)
                    tcs = slice(j * 128 + c * CH, j * 128 + (c + 1) * CH)
                    OP("pe", lambda e, ci=ci, cs=cs, tcs=tcs: e.matmul(po[:, cs], lhsT=sb16[:, ci, :], rhs=qin[:, tcs], start=True, stop=False),
                       reads=[sb16, qin], writes=[po])
                    OP("pe", lambda e, cs=cs, j=j: e.matmul(po[:, cs], lhsT=vtm[:, j, hc], rhs=attm2[:, j, cs], start=False, stop=True),
                       reads=[vtm, attm2], writes=[po])
                OP("act", lambda e, js=js: e.copy(out=o_dst[:, h, js], in_=po[:, 0:128]), reads=[po], writes=[o_dst])

        rc = [0]

        HB = 512
        NHB = NEXP // HB

        def peer_layer(l, last_layer):
            pst = ExitStack()

            def sbp(name, shape, dt=F32):
                return Tile(pst.enter_context(nc.sbuf_tensor(f"p{l}_" + name, list(shape), dt)), name)
            alloc_psum(pst, f"p{l}", 8, 0); cnt["n"] = 6
            po = [PS["F"][6], PS["F"][7]]
            wqb = sbp("wqb", [128, KC, 2048], BF16); skb = sbp("skb", [128, 16, 128], BF16)
            for k in range(KC):
                DMA("pool", wqb[:, k, :], wq_in[l, k * 128:(k + 1) * 128, :], writes=[wqb])
            DMA("pool", skb[:], skT_in[l], writes=[skb])
            uTl = uT_in[l].rearrange("(k p) e -> p k e", p=128)
            for hb in range(NHB):
                DMA("pool", U16[hb], uTl[:, :, hb * HB:(hb + 1) * HB], writes=[U16b])
            for r in range(16):
                DMA("pool", V16[r * 1024:(r + 1) * 1024, :], v_in[l, r * 1024:(r + 1) * 1024, :], writes=[V16b])
            V16v = V16.rearrange("(b q p) d -> b p q d", q=4, p=128)
            xtp = [sbp(f"xt{i}", [128, KC, 128]) for i in range(2)]; hTp = [sbp(f"hT{i}", [128, KC, 128], BF16) for i in range(2)]
            tmp8 = sbp("tmp8", [128, KC, 128]); sq8 = sbp("sq8", [128, KC, 128], BF16)
            qT = sbp("qT", [128, 16, 128], BF16); sc = sbp("sc", [128, 16, 128]); scw = sbp("scw", [128, 128])
            top = sbp("top", [128, 16, 16]); cand = sbp("cand", [128, 8, 256]); cw2 = sbp("cw2", [128, 256]); c24 = sbp("c24", [128, 8, 24])
            tau = sbp("tau", [128, 8]); d16 = sbp("d16", [128, 8, 16]); Zs = sbp("Zs", [128, 8]); beta = sbp("beta", [128, 8])
            sA = sbp("sA", [128, 8, 128])
            NB = 3
            Sblk = [sbp(f"Sblk{i}", [128, 8, 128]) for i in range(NB)]; Exb = [sbp(f"Exb{i}", [128, 8, 128], BF16) for i in range(NB)]
            tmpg = [sbp(f"tmpg{i}", [128, 1024], BF16) for i in range(NB)]
            NR = 4
            ub = [sbp(f"ub{i}", [128, KC, HB], BF16) for i in range(NR)]; vb = [sbp(f"vb{i}", [128, 4, D], BF16) for i in range(NR)]
            gaT = [sbp(f"gaT{i}", [128, 1024]) for i in range(2)]; WT = [sbp(f"WT{i}", [128, 8, 128], BF16) for i in range(2)]
            zb16 = sbp("zb16", [128, 128], BF16)
            OP("dve", lambda e: e.memset(zb16[:], 0.0), writes=[zb16])
            tmpo_ap = cand[:].rearrange("p h c -> p (h c)")[:, 0:1024].rearrange("p (k n) -> p k n", n=128)

            class _Alias:
                def __init__(self, ap, tile_):
                    self.ap = ap; self.b = tile_.b

                def __getitem__(self, k):
                    return self.ap[k]
            tmpo = _Alias(tmpo_ap, cand)
            otm = _Alias(Sblk[0][:].rearrange("p a b -> p (a b)"), Sblk[0])
            ti = 0
            ybuf = Buf("yout")
            for sname in (["x"] if last_layer else ["c", "x"]):
                S_ = streams[sname]
                s = S_["s"]; R = S_["R"]; Rb = S_["Rb"]; T = S_["T"]; nreg = S_["n"]
                dyn = (sname == "x")
                for i in range((half if dyn else T) // 128):
                    t0 = i * 128
                    xt_ = xtp[ti % 2]; hT_ = hTp[ti % 2]; ti += 1
                    if dyn:
                        DMA("sp", xt_[:, :, :128], XRdyn[:, :, t0:t0 + 128], reads=list(Rb), writes=[xt_])
                    else:
                        rb = Rb[t0 // nreg]
                        DMA("sp", xt_[:, :, :128], rsview(R, t0, 128), reads=[rb], writes=[xt_])
                    norm_mod(128, A2, modv, 24, s, hT_, xt_, tmp8, sq8)
                    for jc in range(16):
                        ps = nps()
                        for k in range(KC):
                            OP("pe", lambda e, k=k, jc=jc, ps=ps: e.matmul(ps[:, :128], lhsT=wqb[:, k, jc * 128:(jc + 1) * 128], rhs=hT_[:, k, :128],
                                                                           start=(k == 0), stop=(k == KC - 1)), reads=[wqb, hT_], writes=[ps])
                        OP("act", lambda e, jc=jc, ps=ps: e.copy(out=qT[:, jc, :], in_=ps[:, :128]), reads=[ps], writes=[qT])
                    for g in range(4):
                        ps = nps()
                        for jj in range(4):
                            jc = g * 4 + jj
                            OP("pe", lambda e, jc=jc, jj=jj, ps=ps: e.matmul(ps[:, jj * 128:(jj + 1) * 128], lhsT=qT[:, jc, :], rhs=skb[:, jc, :], start=True, stop=True),
                               reads=[qT, skb], writes=[ps])
                        OP("act", lambda e, g=g, ps=ps: e.copy(out=sc[:, g * 4:(g + 1) * 4, :], in_=ps[:, :].rearrange("p (a b) -> p a b", b=128)), reads=[ps], writes=[sc])
                    for jc in range(16):
                        OP("dve", lambda e, jc=jc: e.max(out=top[:, jc, 0:8], in_=sc[:, jc, :]), reads=[sc], writes=[top])
                        OP("dve", lambda e, jc=jc: e.match_replace(out=scw[:], in_to_replace=top[:, jc, 0:8], in_values=sc[:, jc, :], imm_value=-1e30),
                           reads=[top, sc], writes=[scw])
                        OP("dve", lambda e, jc=jc: e.max(out=top[:, jc, 8:16], in_=scw[:]), reads=[scw], writes=[top])
                    top4 = top[:].rearrange("p (h two) r -> p h two r", two=2)
                    cand4 = cand[:].rearrange("p h (r s) -> p h r s", s=16)
                    OP("dve", lambda e: e.tensor_tensor(out=cand4, in0=top4[:, :, 0, :].unsqueeze(3).to_broadcast([128, 8, 16, 16]),
                                                         in1=top4[:, :, 1, :].unsqueeze(2).to_broadcast([128, 8, 16, 16]), op=ALU.add), reads=[top], writes=[cand])
                    for h in range(8):
                        OP("dve", lambda e, h=h: e.max(out=c24[:, h, 0:8], in_=cand[:, h, :]), reads=[cand], writes=[c24])
                        OP("dve", lambda e, h=h: e.match_replace(out=cw2[:], in_to_replace=c24[:, h, 0:8], in_values=cand[:, h, :], imm_value=-1e30),
                           reads=[c24, cand], writes=[cw2])
                        OP("dve", lambda e, h=h: e.max(out=c24[:, h, 8:16], in_=cw2[:]), reads=[cw2], writes=[c24])
                        OP("dve", lambda e, h=h: e.match_replace(out=cw2[:], in_to_replace=c24[:, h, 8:16], in_values=cw2[:], imm_value=-1e30),
                           reads=[c24, cw2], writes=[cw2])
                        OP("dve", lambda e, h=h: e.max(out=c24[:, h, 16:24], in_=cw2[:]), reads=[cw2], writes=[c24])
                    OP("dve", lambda e: e.tensor_tensor(out=tau[:], in0=c24[:, :, 15], in1=c24[:, :, 16], op=ALU.add), reads=[c24], writes=[tau])
                    OP("dve", lambda e: e.tensor_scalar(out=tau[:], in0=tau[:], scalar1=0.5, scalar2=None, op0=ALU.mult), reads=[tau], writes=[tau])
                    OP("dve", lambda e: e.tensor_tensor(out=d16[:], in0=c24[:, :, 0:16], in1=c24[:, :, 0:1].to_broadcast([128, 8, 16]), op=ALU.subtract),
                       reads=[c24], writes=[d16])
                    OP("act", lambda e: e.activation(out=d16[:], in_=d16[:], func=AF.Exp), reads=[d16], writes=[d16])
                    OP("dve", lambda e: e.tensor_reduce(out=Zs[:], in_=d16[:], axis=AX.X, op=ALU.add), reads=[d16], writes=[Zs])
                    OP("act", lambda e: e.activation(out=Zs[:], in_=Zs[:], func=AF.Ln), reads=[Zs], writes=[Zs])
                    OP("dve", lambda e: e.tensor_tensor(out=beta[:], in0=tau[:], in1=c24[:, :, 0], op=ALU.subtract), reads=[tau, c24], writes=[beta])
                    OP("dve", lambda e: e.tensor_tensor(out=beta[:], in0=beta[:], in1=Zs[:], op=ALU.subtract), reads=[beta, Zs], writes=[beta])
                    sc4 = sc[:].rearrange("p (h two) k -> p h two k", two=2)
                    OP("dve", lambda e: e.tensor_tensor(out=sA[:], in0=sc4[:, :, 0, :], in1=tau[:].unsqueeze(2).to_broadcast([128, 8, 128]), op=ALU.subtract),
                       reads=[sc, tau], writes=[sA])

                    NBK = NEXP // 1024
                    its = [(bk, h) for bk in range(NBK) for h in range(8)]
                    pg = {}; pa = {}

                    def ldu(hb):
                        DMA("sp", ub[hb % NR][:], U16[hb], reads=[U16b], writes=[ub[hb % NR]])

                    def ldv(hb):
                        DMA("sp", vb[hb % NR][:], V16v[hb], reads=[V16b], writes=[vb[hb % NR]])

                    def blk_start(bk):
                        pa[bk] = (nps(), nps()); pg[bk] = (nps(), nps())
                        for c in range(8):
                            ub_ = ub[(2 * bk + c // 4) % NR]; pp = pa[bk][c // 4]
                            for k in range(KC):
                                OP("pe", lambda e, k=k, c=c, ub_=ub_, pp=pp: e.matmul(pp[:, (c % 4) * 128:(c % 4 + 1) * 128], lhsT=ub_[:, k, (c % 4) * 128:(c % 4 + 1) * 128],
                                                                                     rhs=hT_[:, k, :128], start=(k == 0), stop=(k == KC - 1)),
                                   reads=[ub_, hT_], writes=[pp])
                        for hf in range(2):
                            OP("pe", lambda e, hf=hf: e.matmul(pg[bk][hf][:, :], lhsT=zb16[:], rhs=hT_[:, 0:4, :].rearrange("p a b -> p (a b)"), start=True, stop=False),
                               reads=[zb16, hT_], writes=[pg[bk][hf]])
                        g_ = gaT[bk % 2]
                        for hf in range(2):
                            OP("act", lambda e, hf=hf: e.activation(out=g_[:, hf * 512:(hf + 1) * 512], in_=pa[bk][hf][:, :], func=AF.Gelu),
                               reads=[pa[bk][hf]], writes=[g_])

                    def g1(n_):
                        bk, h = its[n_]
                        a0 = bk * 8
                        sbk = Sblk[n_ % NB]; exk = Exb[n_ % NB]
                        eng = "pool" if (n_ % 2 == 1) else "dve"
                        OP(eng, lambda e: e.tensor_tensor(out=sbk[:], in0=sA[:, h, a0:a0 + 8].unsqueeze(2).to_broadcast([128, 8, 128]),
                                                          in1=sc4[:, h, 1, :].unsqueeze(1).to_broadcast([128, 8, 128]), op=ALU.add),
                           reads=[sA, sc], writes=[sbk])
                        OP("act", lambda e: e.activation(out=exk[:], in_=sbk[:], func=AF.Exp, bias=beta[:, h:h + 1]), reads=[sbk, beta], writes=[exk])

                    def g2(n_):
                        bk, h = its[n_]
                        sbk = Sblk[n_ % NB]; exk = Exb[n_ % NB]; tg = tmpg[n_ % NB]
                        sb2 = sbk[:].rearrange("p a b -> p (a b)"); ex2 = exk[:].rearrange("p a b -> p (a b)")
                        OP("dve", lambda e: e.scalar_tensor_tensor(out=tg[:], in0=sb2, scalar=0.0, in1=ex2, op0=ALU.is_ge, op1=ALU.mult),
                           reads=[sbk, exk], writes=[tg])
                        for c in range(8):
                            pp = pg[bk][c // 4]
                            OP("pe", lambda e, c=c, pp=pp: e.matmul(pp[:, (c % 4) * 128:(c % 4 + 1) * 128], lhsT=tg[:, c * 128:(c + 1) * 128], rhs=ident[:],
                                                                   start=False, stop=(h == 7 and c % 4 == 3)), reads=[tg, ident], writes=[pp])

                    def blk_end(bk):
                        g_ = gaT[bk % 2]; w_ = WT[bk % 2]
                        for hf in range(2):
                            OP("dve", lambda e, hf=hf: e.tensor_tensor(out=w_[:, hf * 4:(hf + 1) * 4, :].rearrange("p a b -> p (a b)"), in0=g_[:, hf * 512:(hf + 1) * 512],
                                                                        in1=pg[bk][hf][:, :], op=ALU.mult), reads=[g_, pg[bk][hf]], writes=[w_])
                        for c in range(8):
                            vb_ = vb[(2 * bk + c // 4) % NR]
                            for hf in range(2):
                                OP("pe", lambda e, c=c, hf=hf, vb_=vb_: e.matmul(po[hf][:, :], lhsT=w_[:, c, :], rhs=vb_[:, c % 4, hf * 512:(hf + 1) * 512],
                                                                                start=(bk == 0 and c == 0), stop=(bk == NBK - 1 and c == 7)),
                                   reads=[w_, vb_], writes=[po[hf]])

                    ldu(0); ldu(1); ldv(0); ldv(1); ldv(2); ldv(3)
                    LAG = 2
                    pending_end = {}
                    for n_ in range(len(its) + LAG + 4):
                        if n_ < len(its):
                            bk, h = its[n_]
                            if h == 0:
                                blk_start(bk)
                                if 2 * bk + 2 < NHB:
                                    ldu(2 * bk + 2); ldu(2 * bk + 3)
                            g1(n_)
                        m_ = n_ - LAG
                        if 0 <= m_ < len(its):
                            g2(m_)
                            if its[m_][1] == 7:
                                pending_end[n_ + 3] = its[m_][0]
                        if n_ in pending_end:
                            bke = pending_end.pop(n_)
                            blk_end(bke)
                            if 2 * (bke + 2) < NHB:
                                ldv(2 * (bke + 2)); ldv(2 * (bke + 2) + 1)
                    for hf in range(2):
                        OP("act", lambda e, hf=hf: e.copy(out=otm[:, hf * 512:(hf + 1) * 512], in_=po[hf][:, :]), reads=[po[hf]], writes=[otm])
                    for m in range(KC):
                        pt = nps()
                        OP("pe", lambda e, m=m, pt=pt: e.transpose(out=pt[:, :128], in_=otm[:, m * 128:(m + 1) * 128], identity=identF[:]), reads=[otm, identF], writes=[pt])
                        OP("dve", lambda e, m=m, pt=pt: e.scalar_tensor_tensor(out=xt_[:, m, :128], in0=pt[:, :128], scalar=modv[:, 40 + m, s:s + 1], in1=xt_[:, m, :128],
                                                                               op0=ALU.mult, op1=ALU.add), reads=[pt, modv, xt_], writes=[xt_])
                    if last_layer and stage == 99:
                        norm_mod(128, fgt, zerob, 0, 0, tmpo, xt_, tmp8, sq8)
                        DMA("sp", rsview(yT_out, t0, 128), tmpo[:], reads=[tmpo], writes=[ybuf])
                    elif dyn:
                        DMA("sp", rsview(HXs[t0 // CW], t0 % CW, 128), xt_[:, :, :128], reads=[xt_], writes=[HXb[t0 // CW]])
                    else:
                        DMA("sp", rsview(R, t0, 128), xt_[:, :, :128], reads=[xt_], writes=[rb])
            if not (last_layer and stage == 99):
                if pair:
                    for j in range(NCW):
                        fw.collective("AllGather", [HXs[j].opt()], [HGs[j].opt()], [[0, 1], [2, 3], [4, 5], [6, 7]][:ncores // 2], reads=[HXb[j]], writes=[HGb[j]])
                    for j in range(NCW):
                        for r in range(2):
                            c0 = r * half + j * CW
                            DMA("sp", XR[:, c0:c0 + CW], HGs[j][r * D:(r + 1) * D, :], reads=[HGb[j]], writes=list(XRb))
                else:
                    for j in range(NCW):
                        DMA("sp", XR[:, j * CW:(j + 1) * CW], HXs[j][:, :], reads=[HXb[j]], writes=list(XRb))
            fw.barrier()
            pst.close()

        for l in range(depth):
            last_layer = (l == depth - 1)
            mst = ExitStack()

            def sbm(name, shape, dt=F32):
                return Tile(mst.enter_context(nc.sbuf_tensor(f"m{l}_" + name, list(shape), dt)), name)
            winb = sbm("winb", [128, KC, 4096], BF16)
            woutb = sbm("woutb", [128, KC, D], BF16)
            wmb = [sbm(f"wmb{i}", [128, KC, 256]) for i in range(2)]
            snaps = [sbm(f"snap{i}", [128, 2 * (NT // 128) * 2, 128]) for i in range(2)]
            snapb = [sbm(f"snapb{i}", [128, 2 * (NT // 128) * 2, 128], BF16) for i in range(2)]
            W = {n_: sbm("w_" + n_, [128, NT]) for n_ in ["sg", "f", "key", "lf", "b", "bc", "e1", "e2", "e3", "e4"]}
            qp = sbm("qp", [128, NT], BF16); qin = sbm("qin", [128, NT], BF16); kp = sbm("kp", [128, NT], BF16); kout = sbm("kout", [128, NT], BF16)
            vtm = sbm("vtm", [128, NT // 128, 512], BF16); kotm2 = sbm("kotm2", [128, NT // 128, 4, 128], BF16)
            attm2 = sbm("attm2", [128, NT // 128, 128], BF16)
            osb = sbm("osb", [128, 4, NT]); ofl = sbm("ofl", [128, 4, NT]); sgl = sbm("sgl", [128, 4, NT])
            stg = sbm("stg", [128, 4, NT])
            mix = sbm("mix", [128, KC, NT], BF16)
            uh = sbm("uh", [128, 4, NT + 128]); bgl = sbm("bgl", [128, 4, NT]); cva = sbm("cva", [128, NT])
            zbl = sbm("zbl", [128, 4, NT]); ql = sbm("ql", [128, 4, NT])
            xt = sbm("xt", [128, KC, NT]); tmp8 = sbm("tmp8", [128, KC, NT]); sq8 = sbm("sq8", [128, KC, NT], BF16)
            hT = sbm("hT", [128, KC, NT], BF16)
            alloc_psum(mst, f"m{l}", 6, 2); cnt["n"] = 6
            DMA("sp", n1t[:], n1_in[l], writes=[n1t]); DMA("sp", n2t[:], n2_in[l], writes=[n2t])
            DMA("sp", bmt[:], bmod_in[l], writes=[bmt]); DMA("sp", cwt[:], cw_in[l], writes=[cwt])
            for k in range(KC):
                DMA("pool", winb[:, k, :], win_in[l, k * 128:(k + 1) * 128, :], writes=[winb])
            for k in range(KC):
                DMA("pool", woutb[:, k, :], wout_in[l, k * 128:(k + 1) * 128, :], writes=[woutb])
            pm = nps()
            for cb in range(24):
                wm = wmb[cb % 2]
                DMA("sp", wm[:], wmod_in[l].rearrange("(k p) c -> p k c", p=128)[:, :, cb * 256:(cb + 1) * 256], writes=[wm])
                for cc in range(2):
                    c = cb * 2 + cc
                    for k in range(KC):
                        OP("pe", lambda e, wm=wm, cc=cc, c=c, k=k: e.matmul(pm[:, 2 * c:2 * c + 2], lhsT=wm[:, k, cc * 128:(cc + 1) * 128],
                                                                            rhs=cond[:, k, :], start=(k == 0), stop=(k == KC - 1)),
                           reads=[wm, cond], writes=[pm])
            OP("dve", lambda e: e.tensor_tensor(out=modv[:], in0=pm[:, 0:96].rearrange("p (c s) -> p c s", s=2),
                                                 in1=bmt[:].unsqueeze(2).to_broadcast([128, 48, 2]), op=ALU.add), reads=[pm, bmt], writes=[modv])
            for (A, nt_, g) in [(A1, n1t, 1), (A2, n2t, 4)]:
                OP("dve", lambda e, A=A, g=g: e.tensor_scalar(out=A[:], in0=modv[:, g * 8:(g + 1) * 8, :], scalar1=1.0, scalar2=None, op0=ALU.add),
                   reads=[modv], writes=[A])
                OP("dve", lambda e, A=A, nt_=nt_: e.tensor_tensor(out=A[:], in0=A[:], in1=nt_[:].unsqueeze(2).to_broadcast([128, KC, 2]), op=ALU.mult),
                   reads=[A, nt_], writes=[A])

            for sname in ["c", "x"]:
                S_ = streams[sname]
                s = S_["s"]; n = S_["n"]; ntile = S_["nt"]; R = S_["R"]; Rb = S_["Rb"]; T = S_["T"]
                nsub = n // 128
                if sname == "c":
                    OP("dve", lambda e: e.memset(Sst[:], 0.0), writes=[Sst])
                only_states = (sname == "c" and last_layer)
                for i in range(ntile):
                    t0 = i * n
                    DMA("sp", xt[:, :, :n], rsview(R, t0, n), reads=[Rb[i]], writes=[xt])
                    norm_mod(n, A1, modv, 0, s, hT, xt, tmp8, sq8)
                    for j in range(nsub):
                        pv = nps()
                        for k in range(KC):
                            OP("pe", lambda e, k=k, j=j, pv=pv: e.matmul(pv[:, :], lhsT=hT[:, k, j * 128:(j + 1) * 128], rhs=winb[:, k, 0:512],
                                                                         start=(k == 0), stop=(k == KC - 1)), reads=[hT, winb], writes=[pv])
                        OP("act", lambda e, j=j, pv=pv: e.copy(out=vtm[:, j, :], in_=pv[:, :]), reads=[pv], writes=[vtm])
                    DMA("sp", VS[t0:t0 + n, :].rearrange("(j p) c -> p j c", p=128), vtm[:, :nsub, :], reads=[vtm], writes=[scr_b["VS"][i]])

                    def proj(cc):
                        ps = nps()
                        for k in range(KC):
                            OP("pe", lambda e, k=k, ps=ps: e.matmul(ps[:, :n], lhsT=winb[:, k, cc * 128:(cc + 1) * 128], rhs=hT[:, k, :n],
                                                                    start=(k == 0), stop=(k == KC - 1)), reads=[hT, winb], writes=[ps])
                        return ps
                    for h in range(4):
                        ps = proj(8 + h)
                        OP("act", lambda e, h=h, ps=ps: e.copy(out=stg[:, h, :n], in_=ps[:, :n]), reads=[ps], writes=[stg])
                    DMA("sp", s4view(ZB, t0, n), stg[:, :, :n], reads=[stg], writes=[scr_b["ZB"][i]])
                    for h in range(4):
                        psq = proj(12 + h)
                        OP("act", lambda e, h=h, psq=psq: e.copy(out=ql[:, h, :n], in_=psq[:, :n]), reads=[psq], writes=[ql])
                        psz = proj(4 + h)
                        zps_t[0] = psz
                        last = gate_math(psz[:, :n], ql[:, h, :n], ql, l, 0, h, n, False)
                        recur(l, 0, h, n, False, last, osb)
                    DMA("sp", s4view(QS, t0, n), ql[:, :, :n], reads=[ql], writes=[scr_b["QS"][i]])
                    DMA("sp", s4view(OF, t0, n), osb[:, :, :n], reads=[osb], writes=[scr_b["OF"][i]])
                    if only_states:
                        continue
                    for h in range(4):
                        ps = proj(16 + h)
                        OP("act", lambda e, h=h, ps=ps: e.activation(out=stg[:, h, :n], in_=ps[:, :n], func=AF.Silu), reads=[ps], writes=[stg])
                    DMA("sp", s4view(SG, t0, n), stg[:, :, :n], reads=[stg], writes=[scr_b["SG"][i]])
                    for c in range(4):
                        pc = proj(20 + c)
                        OP("act", lambda e, pc=pc: e.copy(out=cva[:, :n], in_=pc[:, :n]), reads=[pc], writes=[cva])
                        ph = proj(28 + c)
                        OP("dve", lambda e, c=c, ph=ph: e.tensor_tensor(out=stg[:, c, :n], in0=ph[:, :n], in1=cva[:, :n], op=ALU.mult),
                           reads=[ph, cva], writes=[stg])
                    DMA("sp", s4view(US, t0, n), stg[:, :, :n], reads=[stg], writes=[scr_b["US"][i]])
                    for c in range(4):
                        pb = proj(24 + c)
                        OP("act", lambda e, c=c, pb=pb: e.copy(out=stg[:, c, :n], in_=pb[:, :n]), reads=[pb], writes=[stg])
                    DMA("sp", s4view(BG, t0, n), stg[:, :, :n], reads=[stg], writes=[scr_b["BG"][i]])
                for i in range(ntile - 1, -1, -1):
                    t0 = i * n
                    DMA("sp", zbl[:, :, :n], s4view(ZB, t0, n), reads=[scr_b["ZB"][i]], writes=[zbl])
                    DMA("sp", ql[:, :, :n], s4view(QS, t0, n), reads=[scr_b["QS"][i]], writes=[ql])
                    DMA("sp", vtm[:, :nsub, :], VS[t0:t0 + n, :].rearrange("(j p) c -> p j c", p=128), reads=[scr_b["VS"][i]], writes=[vtm])
                    for h in range(4):
                        zps_t[0] = zbl
                        last = gate_math(zbl[:, h, :n], ql[:, h, :n], ql, l, 1, h, n, True)
                        recur(l, 1, h, n, True, last, osb)
                    if only_states:
                        continue
                    DMA("sp", ofl[:, :, :n], s4view(OF, t0, n), reads=[scr_b["OF"][i]], writes=[ofl])
                    DMA("sp", sgl[:, :, :n], s4view(SG, t0, n), reads=[scr_b["SG"][i]], writes=[sgl])
                    DMA("sp", bgl[:, :, :n], s4view(BG, t0, n), reads=[scr_b["BG"][i]], writes=[bgl])
                    OP("pool", lambda e: e.memset(uh[:], 0.0), writes=[uh])
                    lo = 64 if t0 > 0 else 0
                    hi = 64 if t0 + n < T else 0
                    rd = [scr_b["US"][i]] + ([scr_b["US"][i - 1]] if lo else []) + ([scr_b["US"][i + 1]] if hi else [])
                    DMA("sp", uh[:, :, 64 - lo:64 + n + hi], s4view(US, t0 - lo, n + lo + hi), reads=rd, writes=[uh])
                    DMA("sp", xt[:, :, :n], rsview(R, t0, n), reads=[Rb[i]], writes=[xt])
                    OP("dve", lambda e: e.tensor_tensor(out=osb[:, :, :n], in0=osb[:, :, :n], in1=ofl[:, :, :n], op=ALU.add), reads=[osb, ofl], writes=[osb])
                    OP("act", lambda e: e.activation(out=sq8[:, 0:4, :n], in_=osb[:, :, :n], func=AF.Square), reads=[osb], writes=[sq8])
                    for h in range(4):
                        ps = nps()
                        OP("pe", lambda e, h=h, ps=ps: e.matmul(ps[:, :n], lhsT=ones[:], rhs=sq8[:, h, :n], start=True, stop=True), reads=[ones, sq8], writes=[ps])
                        OP("act", lambda e, ps=ps: e.activation(out=rstd[:, :n], in_=ps[:, :n], func=AF.Sqrt, scale=1.0 / 128, bias=epsc[:]),
                           reads=[ps, epsc], writes=[rstd])
                        OP("dve", lambda e: e.reciprocal(out=rstd[:, :n], in_=rstd[:, :n]), reads=[rstd], writes=[rstd])
                        OP("dve", lambda e, h=h: e.tensor_tensor(out=osb[:, h, :n], in0=osb[:, h, :n], in1=rstd[:, :n], op=ALU.mult), reads=[osb, rstd], writes=[osb])
                        OP("dve", lambda e, h=h: e.tensor_tensor(out=mix[:, h, :n], in0=osb[:, h, :n], in1=sgl[:, h, :n], op=ALU.mult), reads=[osb, sgl], writes=[mix])
                    for c in range(4):
                        ctr = uh[:, c, 64:64 + n]
                        OP("dve", lambda e, c=c, ctr=ctr: e.tensor_scalar(out=cva[:, :n], in0=ctr, scalar1=cwt[:, c, 1:2], scalar2=None, op0=ALU.mult),
                           reads=[uh, cwt], writes=[cva])
                        if sname == "x" and c >= 2:
                            OP("dve", lambda e, c=c: e.scalar_tensor_tensor(out=cva[:, :n], in0=uh[:, c, 0:n], scalar=cwt[:, c, 0:1], in1=cva[:, :n],
                                                                            op0=ALU.mult, op1=ALU.add), reads=[uh, cwt, cva], writes=[cva])
                            OP("dve", lambda e, c=c: e.scalar_tensor_tensor(out=cva[:, :n], in0=uh[:, c, 128:128 + n], scalar=cwt[:, c, 2:3], in1=cva[:, :n],
                                                                            op0=ALU.mult, op1=ALU.add), reads=[uh, cwt, cva], writes=[cva])
                        elif sname == "x":
                            u3 = uh[:, c, 64:64 + n].rearrange("p (r w) -> p r w", w=64)
                            c3 = cva[:, :n].rearrange("p (r w) -> p r w", w=64)
                            OP("dve", lambda e, c=c, u3=u3, c3=c3: e.scalar_tensor_tensor(out=c3[:, :, 1:64], in0=u3[:, :, 0:63], scalar=cwt[:, c, 0:1], in1=c3[:, :, 1:64],
                                                                                        op0=ALU.mult, op1=ALU.add), reads=[uh, cwt, cva], writes=[cva])
                            OP("dve", lambda e, c=c, u3=u3, c3=c3: e.scalar_tensor_tensor(out=c3[:, :, 0:63], in0=u3[:, :, 1:64], scalar=cwt[:, c, 2:3], in1=c3[:, :, 0:63],
                                                                                        op0=ALU.mult, op1=ALU.add), reads=[uh, cwt, cva], writes=[cva])
                        else:
                            OP("dve", lambda e, c=c: e.scalar_tensor_tensor(out=cva[:, :n], in0=uh[:, c, 63:63 + n], scalar=cwt[:, c, 0:1], in1=cva[:, :n],
                                                                            op0=ALU.mult, op1=ALU.add), reads=[uh, cwt, cva], writes=[cva])
                            OP("dve", lambda e, c=c: e.scalar_tensor_tensor(out=cva[:, :n], in0=uh[:, c, 65:65 + n], scalar=cwt[:, c, 2:3], in1=cva[:, :n],
                                                                            op0=ALU.mult, op1=ALU.add), reads=[uh, cwt, cva], writes=[cva])
                        OP("dve", lambda e, c=c: e.tensor_tensor(out=mix[:, 4 + c, :n], in0=cva[:, :n], in1=bgl[:, c, :n], op=ALU.mult), reads=[cva, bgl], writes=[mix])
                    for m in range(KC):
                        ps = nps()
                        for fch in range(KC):
                            OP("pe", lambda e, m=m, fch=fch, ps=ps: e.matmul(ps[:, :n], lhsT=woutb[:, fch, m * 128:(m + 1) * 128], rhs=mix[:, fch, :n],
                                                                             start=(fch == 0), stop=(fch == KC - 1)), reads=[woutb, mix], writes=[ps])
                        OP("dve", lambda e, m=m, ps=ps: e.scalar_tensor_tensor(out=xt[:, m, :n], in0=ps[:, :n], scalar=modv[:, 16 + m, s:s + 1], in1=xt[:, m, :n],
                                                                               op0=ALU.mult, op1=ALU.add), reads=[ps, modv, xt], writes=[xt])
                    DMA("sp", rsview(R, t0, n), xt[:, :, :n], reads=[xt], writes=[Rb[i]])
            fw.barrier()
            mst.close()
            if stage == 1:
                break
            peer_layer(l, last_layer)
            if stage == 2:
                break

        if stage in (1, 2):
            xt = sb("xo", [128, KC, NT])
            for i in range(seq // NT):
                DMA("sp", xt[:, :, :NT], rsview(XR, i * NT, NT), reads=[XRb[i]], writes=[xt])
                DMA("sp", rsview(yT_out, i * NT, NT), xt[:, :, :NT], reads=[xt], writes=[dbuf("y")])
            if dbg:
                DMA("sp", xt[:, :, :ctxlen], rsview(CR, 0, ctxlen), reads=[CRb[0]], writes=[xt])
                DMA("sp", rsview(dbgc_out, 0, ctxlen), xt[:, :, :ctxlen], reads=[xt], writes=[dbuf("y2")])
        fw.finish()
        print("instructions:", fw.ninst, {k: v.count for k, v in fw.E.items()})
    return nc


def make_consts():
    bf = ml_dtypes.bfloat16
    ident = np.eye(128, dtype=np.float32).astype(bf)
    ones = np.ones((128, 128), np.float32).astype(bf)
    s = np.arange(128)[:, None]; t = np.arange(128)[None, :]
    same = (s // CH) == (t // CH)
    maskF = (same & (s <= t)).astype(np.float32).astype(bf)
    maskB = (same & (s >= t)).astype(np.float32).astype(bf)
    resetm = np.ones((128, NT), np.float32); resetm[:, ::CH] = 0.0
    rowm = (np.arange(128)[:, None] // CH == np.arange(4)[None, :]).astype(np.float32)
    return dict(identF=np.eye(128, dtype=np.float32), ident=ident, ones=ones, maskF=maskF, maskB=maskB, resetm=resetm, rowm=rowm)


def prep_shared(inp, depth):
    f = lambda a: np.ascontiguousarray(np.asarray(a, dtype=np.float32))
    sh = {}
    sh["w_mod"] = f(inp["w_mod"])
    sh["b_mod"] = f(np.asarray(inp["b_mod"]).reshape(depth, 48, 128).transpose(0, 2, 1))
    sh["n1"] = f(np.asarray(inp["norm1_g"]).reshape(depth, KC, 128).transpose(0, 2, 1))
    sh["n2"] = f(np.asarray(inp["norm2_g"]).reshape(depth, KC, 128).transpose(0, 2, 1))
    sh["fg"] = f(np.repeat(np.asarray(inp["final_g"]).reshape(KC, 128).T[:, :, None], 2, axis=2))
    sh["w_in"] = f(inp["w_in"]); sh["w_out"] = f(inp["w_out"])
    sh["cw"] = f(np.asarray(inp["conv_w"]).reshape(depth, 3, 4, 128).transpose(0, 3, 2, 1))
    sh["lbl"] = f(np.asarray(inp["lb_logits"]).reshape(depth, 2, 4, 128).transpose(3, 0, 1, 2))
    sh["wq"] = f(inp["peer_wq"])
    sh["skT"] = f(np.asarray(inp["peer_subkeys"]).reshape(depth, 16, 128, 128).transpose(0, 3, 1, 2))
    sh["uT"] = f(np.asarray(inp["peer_u"]).transpose(0, 2, 1))
    sh["v"] = f(inp["peer_v"])
    sh.update(make_consts())
    return sh


def prep_core(inp, b, parity=0, half=0):
    f = lambda a: np.ascontiguousarray(np.asarray(a, dtype=np.float32))
    m = {"par": np.array([[parity * half]], np.int32)}
    m["xT"] = f(np.asarray(inp["x"])[b].T)
    m["cxT"] = f(np.asarray(inp["ctx"])[b].T)
    cv = np.stack([np.asarray(inp["c"])[b], np.asarray(inp["c_ctx"])], axis=-1)
    m["cv"] = f(cv.reshape(KC, 128, 2).transpose(1, 0, 2))
    return m


def kernel(**inp):
    x = np.asarray(inp["x"])
    B, seq, _ = x.shape
    ctxlen = np.asarray(inp["ctx"]).shape[1]
    depth = np.asarray(inp["w_in"]).shape[0]
    nc = build(depth, seq, ctxlen, ncores=2 * B)
    sh = prep_shared(inp, depth)
    half = seq // 2
    in_maps = []
    for c in range(2 * B):
        m = dict(sh); m.update(prep_core(inp, c // 2, c % 2, half)); in_maps.append(m)
    res = run_bass_kernel_spmd(nc, in_maps, core_ids=list(range(2 * B)))
    out = np.empty((B, seq, D), np.float32)
    for c in range(2 * B):
        out[c // 2, (c % 2) * half:(c % 2 + 1) * half, :] = res.results[c]["yT"].T
    return out
```

```python
import numpy as np
import ml_dtypes
from contextlib import ExitStack
import concourse.bass as bass
import concourse.mybir as mybir
from concourse.bass_utils import run_bass_kernel_spmd

F32 = mybir.dt.float32
BF16 = mybir.dt.bfloat16
AF = mybir.ActivationFunctionType
ALU = mybir.AluOpType
AX = mybir.AxisListType

D = 1024
KC = 8
NT = 256
NP = 256
CH = 32
EPS = 1e-6
EPOCH = 30000
NDMA = 12
NEXP = 16384


class Tok:
    __slots__ = ("sem", "val", "eng")

    def __init__(self, sem, val, eng):
        self.sem = sem; self.val = val; self.eng = eng


class Buf:
    def __init__(self, name=""):
        self.name = name; self.w = None; self.r = {}


class Eng:
    def __init__(self, name, obj):
        self.name = name; self.obj = obj
        self.sems = []; self.count = 0; self.seen = {}
        self.dma_sems = []; self.dma_n = 0


class FW:
    def __init__(self, nc, stack):
        self.nc = nc; self.stack = stack
        self.E = {n: Eng(n, getattr(nc, o)) for n, o in
                  [("pe", "tensor"), ("dve", "vector"), ("act", "scalar"), ("pool", "gpsimd"), ("sp", "sync")]}
        self.ninst = 0

    def newsem(self, name):
        return self.stack.enter_context(self.nc.semaphore(name))

    def _wait(self, e, tok):
        if tok is None:
            return
        k = id(tok.sem)
        if e.seen.get(k, 0) >= tok.val:
            return
        e.obj.wait_ge(tok.sem, tok.val)
        e.seen[k] = tok.val

    def _deps(self, e, reads, writes):
        for b in reads:
            if b.w is not None:
                self._wait(e, b.w)
        for b in writes:
            if b.w is not None and b.w.eng != e.name:
                self._wait(e, b.w)
            for en, t in b.r.items():
                if en != e.name:
                    self._wait(e, t)

    def _commit(self, tok, reads, writes, rkey):
        for b in reads:
            b.r[rkey] = tok
        for b in writes:
            b.w = tok; b.r = {}

    def op(self, eng, fn, reads=(), writes=()):
        e = self.E[eng]
        self._deps(e, reads, writes)
        ep = e.count // EPOCH
        while len(e.sems) <= ep:
            e.sems.append(self.newsem(f"s_{eng}_{len(e.sems)}"))
        ins = fn(e.obj)
        val = e.count - ep * EPOCH + 1
        ins.then_inc(e.sems[ep], 1)
        e.count += 1
        self.ninst += 1
        tok = Tok(e.sems[ep], val, eng)
        self._commit(tok, reads, writes, eng)
        return tok

    def dma(self, eng, out, in_, reads=(), writes=(), **kw):
        e = self.E[eng]
        self._deps(e, reads, writes)
        j = e.dma_n % NDMA
        if len(e.dma_sems) <= j:
            e.dma_sems.append([self.newsem(f"d_{eng}_{j}"), 0])
        slot = e.dma_sems[j]
        if slot[1] > 0:
            self._wait(e, Tok(slot[0], slot[1], "dma"))
        slot[1] += 16
        e.obj.dma_start(out=out, in_=in_, **kw).then_inc(slot[0], 16)
        e.dma_n += 1
        self.ninst += 1
        tok = Tok(slot[0], slot[1], "dma")
        self._commit(tok, reads, writes, f"dma_{eng}_{j}")
        return tok

    def raw(self, eng, fn, reads=(), writes=()):
        e = self.E[eng]
        self._deps(e, reads, writes)
        return fn(e.obj)

    def collective(self, kind, ins, outs, groups, reads=(), writes=()):
        e = self.E["pool"]
        self._deps(e, reads, writes)
        if not hasattr(self, "cc_sem"):
            self.cc_sem = self.newsem("cc_sem"); self.cc_n = 0
        self.cc_n += 1
        e.obj.collective_compute(kind, mybir.AluOpType.bypass, replica_groups=groups, ins=ins, outs=outs).then_inc(self.cc_sem, 1)
        tok = Tok(self.cc_sem, self.cc_n, "cc")
        self._commit(tok, reads, writes, "cc")
        return tok

    def barrier(self):
        toks = []
        for en in self.E.values():
            if en.count > 0:
                ep = (en.count - 1) // EPOCH
                toks.append(Tok(en.sems[ep], en.count - ep * EPOCH, en.name))
            for slot in en.dma_sems:
                if slot[1] > 0:
                    toks.append(Tok(slot[0], slot[1], "dma"))
        if hasattr(self, "cc_sem") and self.cc_n > 0:
            toks.append(Tok(self.cc_sem, self.cc_n, "cc"))
        for e in self.E.values():
            for t in toks:
                if t.eng != e.name:
                    self._wait(e, t)

    def finish(self):
        e = self.E["sp"]
        for en in self.E.values():
            for slot in en.dma_sems:
                if slot[1] > 0:
                    self._wait(e, Tok(slot[0], slot[1], "dma"))


class Tile:
    def __init__(self, t, name):
        self.t = t; self.b = Buf(name)

    def __getitem__(self, k):
        return self.t[k]


def build(depth, seq, ctxlen, stage=99, dbg=False, pair=True, ncores=8):
    nc = bass.Bass("TRN2", target_bir_lowering=False)
    assert seq % NT == 0 and ctxlen <= NT and ctxlen % 128 == 0 and seq % NP == 0
    rows_per_tile = NT // 64

    def din(name, shape, dt=F32):
        return nc.dram_tensor(name, list(shape), dt, kind="ExternalInput").ap()

    def dscr(name, shape, dt=F32):
        return nc.dram_tensor(name, list(shape), dt).ap()

    xT_in = din("xT", [D, seq]); cxT_in = din("cxT", [D, ctxlen])
    cv_in = din("cv", [128, KC, 2])
    wmod_in = din("w_mod", [depth, D, 6 * D]); bmod_in = din("b_mod", [depth, 128, 48])
    n1_in = din("n1", [depth, 128, KC]); n2_in = din("n2", [depth, 128, KC]); fg_in = din("fg", [128, KC, 2])
    win_in = din("w_in", [depth, D, 4096]); wout_in = din("w_out", [depth, D, D])
    cw_in = din("cw", [depth, 128, 4, 3]); lbl_in = din("lbl", [128, depth, 2, 4])
    wq_in = din("wq", [depth, D, 2048]); skT_in = din("skT", [depth, 128, 16, 128])
    uT_in = din("uT", [depth, D, NEXP]); v_in = din("v", [depth, NEXP, D])
    ident_in = din("ident", [128, 128], BF16); ones_in = din("ones", [128, 128], BF16)
    maskF_in = din("maskF", [128, 128], BF16); maskB_in = din("maskB", [128, 128], BF16)
    identF_in = din("identF", [128, 128]); reset_in = din("resetm", [128, NT]); rowm_in = din("rowm", [128, 4])
    half = seq // 2 if pair else seq
    I32 = mybir.dt.int32
    par_in = din("par", [1, 1], I32)
    yT_out = nc.dram_tensor("yT", [D, half if stage == 99 else seq], F32, kind="ExternalOutput").ap()
    dbg_out = nc.dram_tensor("dbg", [D, seq], F32, kind="ExternalOutput").ap() if dbg else None
    dbgc_out = nc.dram_tensor("dbgc", [D, ctxlen], F32, kind="ExternalOutput").ap() if dbg else None

    TM = max(seq, ctxlen)
    XRh = nc.dram_tensor("XR", [D, seq], F32); XR = XRh.ap(); CR = dscr("CR", [D, ctxlen])
    CW = min(512, half); NCW = half // CW
    HXs = [dscr(f"HX{j}", [D, CW]) for j in range(NCW)]; HGs = [dscr(f"HG{j}", [2 * D, CW]) for j in range(NCW)]
    HXb = [Buf(f"HX{j}") for j in range(NCW)]; HGb = [Buf(f"HG{j}") for j in range(NCW)]
    ZB = dscr("ZB", [512, TM]); QS = dscr("QS", [512, TM]); SG = dscr("SG", [512, TM])
    OF = dscr("OF", [512, TM]); US = dscr("US", [512, TM]); BG = dscr("BG", [512, TM])
    VS = dscr("VS", [TM, 512], BF16)
    U16 = dscr("U16", [NEXP // 512, 128, KC, 512], BF16); V16 = dscr("V16", [NEXP, D], BF16)
    U16b = Buf("U16"); V16b = Buf("V16")

    with ExitStack() as st:
        fw = FW(nc, st)

        def sb(name, shape, dt=F32):
            return Tile(st.enter_context(nc.sbuf_tensor("sb_" + name, list(shape), dt)), name)

        def dbuf(name):
            return Buf(name)

        PS = {"F": [], "B": []}
        cnt = {"f": 0, "b": 0}

        def alloc_psum(stack, tag, nf, nb):
            PS["F"] = [Tile(stack.enter_context(nc.psum_tensor(f"psF{tag}_{i}", [128, 512], F32)), f"psF{i}") for i in range(nf)]
            PS["B"] = [Tile(stack.enter_context(nc.psum_tensor(f"psB{tag}_{i}", [128, 1024], BF16)), f"psB{i}") for i in range(nb)]

        def nps():
            cnt["f"] += 1
            return PS["F"][cnt["f"] % cnt["n"]]

        def npsb():
            cnt["b"] += 1
            return PS["B"][cnt["b"] % 2]

        def OP(eng, fn, reads=(), writes=()):
            return fw.op(eng, fn, reads=[x.b if hasattr(x, "b") else x for x in reads],
                         writes=[x.b if hasattr(x, "b") else x for x in writes])

        def DMA(eng, out, in_, reads=(), writes=(), **kw):
            return fw.dma(eng, out, in_, reads=[x.b if hasattr(x, "b") else x for x in reads],
                          writes=[x.b if hasattr(x, "b") else x for x in writes], **kw)

        ident = sb("ident", [128, 128], BF16); ones = sb("ones", [128, 128], BF16)
        maskF = sb("maskF", [128, 128], BF16); maskB = sb("maskB", [128, 128], BF16)
        resetm = sb("resetm", [128, NT]); rowm = sb("rowm", [128, 4])
        DMA("sp", rowm[:], rowm_in[:, :], writes=[rowm])
        for tl, src in [(ident, ident_in), (ones, ones_in), (maskF, maskF_in), (maskB, maskB_in), (resetm, reset_in)]:
            DMA("sp", tl[:], src[:, :], writes=[tl])
        part = Tile(st.enter_context(nc.sbuf_tensor("sb_part", [1, 1], I32)), "part")
        preg = st.enter_context(nc.sync.register("preg"))
        DMA("sp", part[:], par_in[:, :], writes=[part])
        fw.raw("sp", lambda e: e.reg_load(preg, part[:1, :1]), reads=[part.b])
        pval = nc.sync.snap(preg)
        XRdyn = bass.AP(XRh, pval, [[seq, 128], [128 * seq, KC], [1, half]])
        cond = sb("cond", [128, KC, 2])
        DMA("sp", cond[:], cv_in[:, :, :], writes=[cond])
        OP("act", lambda e: e.activation(out=cond[:], in_=cond[:], func=AF.Silu), reads=[cond], writes=[cond])
        epsc = sb("epsc", [128, 1])
        OP("dve", lambda e: e.memset(epsc[:], EPS), writes=[epsc])
        lbe = sb("lbe", [128, depth, 8]); lbs = sb("lbs", [128, 8]); lb = sb("lb", [128, depth, 8]); oml = sb("oml", [128, depth, 8])
        DMA("sp", lbe[:], lbl_in.rearrange("p l a b -> p l (a b)"), writes=[lbe])
        OP("act", lambda e: e.activation(out=lbe[:], in_=lbe[:], func=AF.Exp), reads=[lbe], writes=[lbe])
        OP("dve", lambda e: e.tensor_copy(out=lbs[:], in_=lbe[:, 0, :]), reads=[lbe], writes=[lbs])
        for l in range(1, depth):
            OP("dve", lambda e, l=l: e.tensor_tensor(out=lbs[:], in0=lbs[:], in1=lbe[:, l, :], op=ALU.add), reads=[lbs, lbe], writes=[lbs])
        OP("dve", lambda e: e.reciprocal(out=lbs[:], in_=lbs[:]), reads=[lbs], writes=[lbs])
        OP("dve", lambda e: e.memset(lb[:, 0, :], 0.0), writes=[lb])
        for l in range(1, depth):
            OP("dve", lambda e, l=l: e.tensor_tensor(out=lbe[:, l, :], in0=lbe[:, l, :], in1=lbs[:], op=ALU.mult), reads=[lbe, lbs], writes=[lbe])
            OP("dve", lambda e, l=l: e.tensor_tensor(out=lb[:, l, :], in0=lb[:, l - 1, :], in1=lbe[:, l, :], op=ALU.add), reads=[lb, lbe], writes=[lb])
        OP("dve", lambda e: e.tensor_scalar(out=oml[:], in0=lb[:], scalar1=-1.0, scalar2=1.0, op0=ALU.mult, op1=ALU.add), reads=[lb], writes=[oml])

        XRb = [dbuf(f"XR{i}") for i in range(seq // NT)]
        CRb = [dbuf("CR")]
        for i in range(seq // NT):
            DMA("sp", XR[:, i * NT:(i + 1) * NT], xT_in[:, i * NT:(i + 1) * NT], writes=[XRb[i]])
        DMA("sp", CR[:, :], cxT_in[:, :], writes=[CRb[0]])

        modv = sb("modv", [128, 48, 2])
        A1 = sb("A1", [128, KC, 2]); A2 = sb("A2", [128, KC, 2])
        n1t = sb("n1t", [128, KC]); n2t = sb("n2t", [128, KC]); bmt = sb("bmt", [128, 48]); cwt = sb("cwt", [128, 4, 3])
        fgt = sb("fgt", [128, KC, 2]); zerob = sb("zerob", [128, KC, 2])
        DMA("sp", fgt[:], fg_in[:, :, :], writes=[fgt])
        OP("dve", lambda e: e.memset(zerob[:], 0.0), writes=[zerob])
        identF = sb("identF", [128, 128])
        DMA("sp", identF[:], identF_in[:, :], writes=[identF])
        Sst = sb("Sst", [128, 8, 128])
        rstd = sb("rstd", [128, NT])

        streams = {
            "c": dict(T=ctxlen, R=CR, Rb=CRb, s=1, nt=1, n=ctxlen),
            "x": dict(T=seq, R=XR, Rb=XRb, s=0, nt=seq // NT, n=NT),
        }
        scr_b = {nm: [dbuf(f"{nm}{i}") for i in range(TM // min(NT, ctxlen) + 1)] for nm in ["ZB", "QS", "SG", "OF", "US", "BG", "VS"]}

        def rsview(R, t0, n):
            return R.rearrange("(k p) t -> p k t", p=128)[:, :, t0:t0 + n]

        def s4view(S, t0, n):
            return S.rearrange("(k p) t -> p k t", p=128)[:, :, t0:t0 + n]

        def norm_mod(n, A, Bt, Bm_col0, s, out_bf, xin, tmp8, sq8):
            OP("act", lambda e: e.activation(out=sq8[:, :, :n], in_=xin[:, :, :n], func=AF.Square), reads=[xin], writes=[sq8])
            ps = nps()
            for k in range(KC):
                OP("pe", lambda e, k=k: e.matmul(ps[:, :n], lhsT=ones[:], rhs=sq8[:, k, :n], start=(k == 0), stop=(k == KC - 1)),
                   reads=[ones, sq8], writes=[ps])
            OP("act", lambda e: e.activation(out=rstd[:, :n], in_=ps[:, :n], func=AF.Sqrt, scale=1.0 / D, bias=epsc[:]), reads=[ps, epsc], writes=[rstd])
            OP("dve", lambda e: e.reciprocal(out=rstd[:, :n], in_=rstd[:, :n]), reads=[rstd], writes=[rstd])
            OP("dve", lambda e: e.tensor_tensor(out=tmp8[:, :, :n], in0=xin[:, :, :n],
                                                 in1=rstd[:, :n].unsqueeze(1).to_broadcast([128, KC, n]), op=ALU.mult),
               reads=[xin, rstd], writes=[tmp8])
            for k in range(KC):
                OP("act", lambda e, k=k: e.activation(out=out_bf[:, k, :n], in_=tmp8[:, k, :n], func=AF.Identity,
                                                       scale=A[:, k, s:s + 1], bias=Bt[:, Bm_col0 + k, s:s + 1]),
                   reads=[tmp8, A, Bt], writes=[out_bf])

        def gate_math(zps, qsrc_ap, qsrc_t, l, d, h, n, bwd):
            nchk = n // CH
            sg, f, key, lf, b, bc, e1, e2, e3, e4 = (W[x] for x in ["sg", "f", "key", "lf", "b", "bc", "e1", "e2", "e3", "e4"])
            col = d * 4 + h
            OP("act", lambda e: e.activation(out=sg[:, :n], in_=zps, func=AF.Sigmoid), reads=[zps_t[0]], writes=[sg])
            OP("dve", lambda e: e.tensor_scalar(out=f[:, :n], in0=sg[:, :n], scalar1=oml[:, l, col:col + 1], scalar2=lb[:, l, col:col + 1],
                                                 op0=ALU.mult, op1=ALU.add), reads=[sg, oml, lb], writes=[f])
            OP("pool", lambda e: e.tensor_scalar(out=f[:, :n], in0=f[:, :n], scalar1=1e-20, scalar2=None, op0=ALU.max), reads=[f], writes=[f])
            OP("pool", lambda e: e.tensor_scalar(out=key[:, :n], in0=f[:, :n], scalar1=-1.0, scalar2=1.0, op0=ALU.mult, op1=ALU.add),
               reads=[f], writes=[key])
            OP("act", lambda e: e.activation(out=lf[:, :n], in_=f[:, :n], func=AF.Ln), reads=[f], writes=[lf])
            OP("dve", lambda e: e.tensor_tensor_scan(out=b[:, :n], data0=resetm[:, :n], data1=lf[:, :n], initial=0.0, op0=ALU.mult, op1=ALU.add),
               reads=[resetm, lf], writes=[b])
            b3 = b[:, :n].rearrange("p (c t) -> p c t", t=CH)
            if bwd:
                lf3 = lf[:, :n].rearrange("p (c t) -> p c t", t=CH)
                bc3 = bc[:, :n].rearrange("p (c t) -> p c t", t=CH)
                OP("dve", lambda e: e.tensor_tensor(out=bc3, in0=b3[:, :, CH - 1:CH].to_broadcast([128, nchk, CH]), in1=b3, op=ALU.subtract),
                   reads=[b], writes=[bc])
                OP("dve", lambda e: e.tensor_tensor(out=b[:, :n], in0=bc[:, :n], in1=lf[:, :n], op=ALU.add), reads=[bc, lf], writes=[b])
                mid = CH // 2; last = 0
            else:
                mid = CH // 2 - 1; last = CH - 1
            bc3 = bc[:, :n].rearrange("p (c t) -> p c t", t=CH)
            OP("dve", lambda e: e.tensor_tensor(out=bc3, in0=b3, in1=b3[:, :, mid:mid + 1].to_broadcast([128, nchk, CH]), op=ALU.subtract),
               reads=[b], writes=[bc])
            OP("act", lambda e: e.activation(out=e1[:, :n], in_=bc[:, :n], func=AF.Exp), reads=[bc], writes=[e1])
            OP("act", lambda e: e.activation(out=e2[:, :n], in_=bc[:, :n], func=AF.Exp, scale=-1.0), reads=[bc], writes=[e2])
            OP("act", lambda e: e.activation(out=e3[:, :n], in_=b[:, :n], func=AF.Exp), reads=[b], writes=[e3])
            OP("dve", lambda e: e.tensor_tensor(out=bc3, in0=b3[:, :, last:last + 1].to_broadcast([128, nchk, CH]), in1=b3, op=ALU.subtract),
               reads=[b, e1, e2], writes=[bc])
            OP("act", lambda e: e.activation(out=e4[:, :n], in_=bc[:, :n], func=AF.Exp), reads=[bc], writes=[e4])
            OP("dve", lambda e: e.tensor_tensor(out=qp[:, :n], in0=qsrc_ap, in1=e1[:, :n], op=ALU.mult), reads=[qsrc_t, e1], writes=[qp])
            OP("pool", lambda e: e.tensor_tensor(out=qin[:, :n], in0=qsrc_ap, in1=e3[:, :n], op=ALU.mult), reads=[qsrc_t, e3], writes=[qin])
            OP("dve", lambda e: e.tensor_tensor(out=kp[:, :n], in0=key[:, :n], in1=e2[:, :n], op=ALU.mult), reads=[key, e2], writes=[kp])
            OP("pool", lambda e: e.tensor_tensor(out=kout[:, :n], in0=key[:, :n], in1=e4[:, :n], op=ALU.mult), reads=[key, e4], writes=[kout])
            return last

        zps_t = [None]

        def recur(l, d, h, n, bwd, last, o_dst):
            si = d * 4 + h
            e3 = W["e3"]
            nsub = n // 128
            subs = list(range(nsub - 1, -1, -1)) if bwd else list(range(nsub))
            chunks = list(range(3, -1, -1)) if bwd else list(range(4))
            mask = maskB if bwd else maskF
            order = [(j, c) for j in subs for c in chunks]
            nck = len(order)
            hc = slice(h * 128, (h + 1) * 128)
            for j in subs:
                js = slice(j * 128, (j + 1) * 128)
                pt = npsb()
                OP("pe", lambda e: e.transpose(out=pt[:, 0:128], in_=kout[:, js], identity=ident[:]), reads=[kout, ident], writes=[pt])
                for c4 in range(4):
                    OP("act", lambda e, c4=c4: e.activation(out=kotm2[:, j, c4, :], in_=pt[:, 0:128], func=AF.Identity, scale=rowm[:, c4:c4 + 1]),
                       reads=[pt, rowm], writes=[kotm2])
                pa = nps()
                OP("pe", lambda e: e.matmul(pa[:, 0:128], lhsT=kp[:, js], rhs=qp[:, js], start=True, stop=True), reads=[kp, qp], writes=[pa])
                OP("dve", lambda e: e.tensor_tensor(out=attm2[:, j, :], in0=pa[:, 0:128], in1=mask[:], op=ALU.mult), reads=[pa, mask], writes=[attm2])
            pSb = [nps() for _ in range((nck + 3) // 4)]
            for ci, (j, c) in enumerate(order):
                pp = pSb[ci // 4]
                OP("pe", lambda e, ci=ci, j=j, c=c, pp=pp: e.matmul(pp[:, (ci % 4) * 128:(ci % 4 + 1) * 128], lhsT=kotm2[:, j, c, :], rhs=vtm[:, j, hc],
                                                                    start=True, stop=True), reads=[kotm2, vtm], writes=[pp])
            snap = snaps[rc[0] % 2]; sb16 = snapb[rc[0] % 2]; rc[0] += 1
            OP("dve", lambda e: e.tensor_copy(out=snap[:, 0, :], in_=Sst[:, si, :]), reads=[Sst], writes=[snap])
            for ci, (j, c) in enumerate(order):
                pp = pSb[ci // 4]
                dcol = j * 128 + c * CH + last
                lastc = (ci == nck - 1)
                out_ap = Sst[:, si, :] if lastc else snap[:, ci + 1, :]
                OP("dve", lambda e, ci=ci, pp=pp, dcol=dcol, out_ap=out_ap: e.scalar_tensor_tensor(
                    out=out_ap, in0=snap[:, ci, :], scalar=e3[:, dcol:dcol + 1], in1=pp[:, (ci % 4) * 128:(ci % 4 + 1) * 128], op0=ALU.mult, op1=ALU.add),
                   reads=[snap, e3, pp], writes=[Sst] if lastc else [snap])
            OP("act", lambda e: e.copy(out=sb16[:, 0:nck, :], in_=snap[:, 0:nck, :]), reads=[snap], writes=[sb16])
            for j in subs:
                js = slice(j * 128, (j + 1) * 128)
                po = nps()
                for c in chunks:
                    ci = order.index((j, c))
                    cs = slice(c * CH, (c + 1) * CH)
                    tcs = slice(j * 128 + c * CH, j * 128 + (c + 1) * CH)
                    OP("pe", lambda e, ci=ci, cs=cs, tcs=tcs: e.matmul(po[:, cs], lhsT=sb16[:, ci, :], rhs=qin[:, tcs], start=True, stop=False),
                       reads=[sb16, qin], writes=[po])
                    OP("pe", lambda e, cs=cs, j=j: e.matmul(po[:, cs], lhsT=vtm[:, j, hc], rhs=attm2[:, j, cs], start=False, stop=True),
                       reads=[vtm, attm2], writes=[po])
                OP("act", lambda e, js=js: e.copy(out=o_dst[:, h, js], in_=po[:, 0:128]), reads=[po], writes=[o_dst])

        rc = [0]
        pending_casts = []

        HB = 512
        NHB = NEXP // HB

        def peer_layer(l, last_layer):
            pst = ExitStack()

            def sbp(name, shape, dt=F32):
                return Tile(pst.enter_context(nc.sbuf_tensor(f"p{l}_" + name, list(shape), dt)), name)
            alloc_psum(pst, f"p{l}", 8, 0); cnt["n"] = 6
            po = [PS["F"][6], PS["F"][7]]
            wqb = sbp("wqb", [128, KC, 2048], BF16); skb = sbp("skb", [128, 16, 128], BF16)
            for k in range(KC):
                DMA("pool", wqb[:, k, :], wq_in[l, k * 128:(k + 1) * 128, :], writes=[wqb])
            DMA("pool", skb[:], skT_in[l], writes=[skb])
            while pending_casts:
                pending_casts.pop(0)()
            V16v = V16.rearrange("(b q p) d -> b p q d", q=4, p=128)
            xtp = [sbp(f"xt{i}", [128, KC, 128]) for i in range(2)]; hTp = [sbp(f"hT{i}", [128, KC, 128], BF16) for i in range(2)]
            tmp8 = sbp("tmp8", [128, KC, 128]); sq8 = sbp("sq8", [128, KC, 128], BF16)
            qT = sbp("qT", [128, 16, 128], BF16); sc = sbp("sc", [128, 16, 128]); scw = sbp("scw", [128, 128])
            top = sbp("top", [128, 16, 16]); cand = sbp("cand", [128, 8, 256]); cw2 = sbp("cw2", [128, 256]); c24 = sbp("c24", [128, 8, 24])
            tau = sbp("tau", [128, 8]); d16 = sbp("d16", [128, 8, 16]); Zs = sbp("Zs", [128, 8]); beta = sbp("beta", [128, 8])
            sA = sbp("sA", [128, 8, 128])
            NB = 3
            Sblk = [sbp(f"Sblk{i}", [128, 8, 128]) for i in range(NB)]; Exb = [sbp(f"Exb{i}", [128, 8, 128], BF16) for i in range(NB)]
            tmpg = [sbp(f"tmpg{i}", [128, 1024], BF16) for i in range(NB)]
            NR = 4
            ub = [sbp(f"ub{i}", [128, KC, HB], BF16) for i in range(NR)]; vb = [sbp(f"vb{i}", [128, 4, D], BF16) for i in range(NR)]
            gaT = [sbp(f"gaT{i}", [128, 1024]) for i in range(2)]; WT = [sbp(f"WT{i}", [128, 8, 128], BF16) for i in range(2)]
            zb16 = sbp("zb16", [128, 128], BF16)
            OP("dve", lambda e: e.memset(zb16[:], 0.0), writes=[zb16])
            tmpo_ap = cand[:].rearrange("p h c -> p (h c)")[:, 0:1024].rearrange("p (k n) -> p k n", n=128)

            class _Alias:
                def __init__(self, ap, tile_):
                    self.ap = ap; self.b = tile_.b

                def __getitem__(self, k):
                    return self.ap[k]
            tmpo = _Alias(tmpo_ap, cand)
            otm = _Alias(Sblk[0][:].rearrange("p a b -> p (a b)"), Sblk[0])
            ti = 0
            ybuf = Buf("yout")
            for sname in (["x"] if last_layer else ["c", "x"]):
                S_ = streams[sname]
                s = S_["s"]; R = S_["R"]; Rb = S_["Rb"]; T = S_["T"]; nreg = S_["n"]
                dyn = (sname == "x")
                for i in range((half if dyn else T) // 128):
                    t0 = i * 128
                    xt_ = xtp[ti % 2]; hT_ = hTp[ti % 2]; ti += 1
                    if dyn:
                        DMA("sp", xt_[:, :, :128], XRdyn[:, :, t0:t0 + 128], reads=list(Rb), writes=[xt_])
                    else:
                        rb = Rb[t0 // nreg]
                        DMA("sp", xt_[:, :, :128], rsview(R, t0, 128), reads=[rb], writes=[xt_])
                    norm_mod(128, A2, modv, 24, s, hT_, xt_, tmp8, sq8)
                    for jc in range(16):
                        ps = nps()
                        for k in range(KC):
                            OP("pe", lambda e, k=k, jc=jc, ps=ps: e.matmul(ps[:, :128], lhsT=wqb[:, k, jc * 128:(jc + 1) * 128], rhs=hT_[:, k, :128],
                                                                           start=(k == 0), stop=(k == KC - 1)), reads=[wqb, hT_], writes=[ps])
                        OP("act", lambda e, jc=jc, ps=ps: e.copy(out=qT[:, jc, :], in_=ps[:, :128]), reads=[ps], writes=[qT])
                    for g in range(4):
                        ps = nps()
                        for jj in range(4):
                            jc = g * 4 + jj
                            OP("pe", lambda e, jc=jc, jj=jj, ps=ps: e.matmul(ps[:, jj * 128:(jj + 1) * 128], lhsT=qT[:, jc, :], rhs=skb[:, jc, :], start=True, stop=True),
                               reads=[qT, skb], writes=[ps])
                        OP("act", lambda e, g=g, ps=ps: e.copy(out=sc[:, g * 4:(g + 1) * 4, :], in_=ps[:, :].rearrange("p (a b) -> p a b", b=128)), reads=[ps], writes=[sc])
                    for jc in range(16):
                        OP("dve", lambda e, jc=jc: e.max(out=top[:, jc, 0:8], in_=sc[:, jc, :]), reads=[sc], writes=[top])
                        OP("dve", lambda e, jc=jc: e.match_replace(out=scw[:], in_to_replace=top[:, jc, 0:8], in_values=sc[:, jc, :], imm_value=-1e30),
                           reads=[top, sc], writes=[scw])
                        OP("dve", lambda e, jc=jc: e.max(out=top[:, jc, 8:16], in_=scw[:]), reads=[scw], writes=[top])
                    top4 = top[:].rearrange("p (h two) r -> p h two r", two=2)
                    cand4 = cand[:].rearrange("p h (r s) -> p h r s", s=16)
                    OP("dve", lambda e: e.tensor_tensor(out=cand4, in0=top4[:, :, 0, :].unsqueeze(3).to_broadcast([128, 8, 16, 16]),
                                                         in1=top4[:, :, 1, :].unsqueeze(2).to_broadcast([128, 8, 16, 16]), op=ALU.add), reads=[top], writes=[cand])
                    for h in range(8):
                        OP("dve", lambda e, h=h: e.max(out=c24[:, h, 0:8], in_=cand[:, h, :]), reads=[cand], writes=[c24])
                        OP("dve", lambda e, h=h: e.match_replace(out=cw2[:], in_to_replace=c24[:, h, 0:8], in_values=cand[:, h, :], imm_value=-1e30),
                           reads=[c24, cand], writes=[cw2])
                        OP("dve", lambda e, h=h: e.max(out=c24[:, h, 8:16], in_=cw2[:]), reads=[cw2], writes=[c24])
                        OP("dve", lambda e, h=h: e.match_replace(out=cw2[:], in_to_replace=c24[:, h, 8:16], in_values=cw2[:], imm_value=-1e30),
                           reads=[c24, cw2], writes=[cw2])
                        OP("dve", lambda e, h=h: e.max(out=c24[:, h, 16:24], in_=cw2[:]), reads=[cw2], writes=[c24])
                    OP("dve", lambda e: e.tensor_tensor(out=tau[:], in0=c24[:, :, 15], in1=c24[:, :, 16], op=ALU.add), reads=[c24], writes=[tau])
                    OP("dve", lambda e: e.tensor_scalar(out=tau[:], in0=tau[:], scalar1=0.5, scalar2=None, op0=ALU.mult), reads=[tau], writes=[tau])
                    OP("dve", lambda e: e.tensor_tensor(out=d16[:], in0=c24[:, :, 0:16], in1=c24[:, :, 0:1].to_broadcast([128, 8, 16]), op=ALU.subtract),
                       reads=[c24], writes=[d16])
                    OP("act", lambda e: e.activation(out=d16[:], in_=d16[:], func=AF.Exp), reads=[d16], writes=[d16])
                    OP("dve", lambda e: e.tensor_reduce(out=Zs[:], in_=d16[:], axis=AX.X, op=ALU.add), reads=[d16], writes=[Zs])
                    OP("act", lambda e: e.activation(out=Zs[:], in_=Zs[:], func=AF.Ln), reads=[Zs], writes=[Zs])
                    OP("dve", lambda e: e.tensor_tensor(out=beta[:], in0=tau[:], in1=c24[:, :, 0], op=ALU.subtract), reads=[tau, c24], writes=[beta])
                    OP("dve", lambda e: e.tensor_tensor(out=beta[:], in0=beta[:], in1=Zs[:], op=ALU.subtract), reads=[beta, Zs], writes=[beta])
                    sc4 = sc[:].rearrange("p (h two) k -> p h two k", two=2)
                    OP("dve", lambda e: e.tensor_tensor(out=sA[:], in0=sc4[:, :, 0, :], in1=tau[:].unsqueeze(2).to_broadcast([128, 8, 128]), op=ALU.subtract),
                       reads=[sc, tau], writes=[sA])

                    NBK = NEXP // 1024
                    its = [(bk, h) for bk in range(NBK) for h in range(8)]
                    pg = {}; pa = {}

                    def ldu(hb):
                        DMA("sp", ub[hb % NR][:], U16[hb], reads=[U16b], writes=[ub[hb % NR]])

                    def ldv(hb):
                        DMA("sp", vb[hb % NR][:], V16v[hb], reads=[V16b], writes=[vb[hb % NR]])

                    def blk_start(bk):
                        pa[bk] = (nps(), nps()); pg[bk] = (nps(), nps())
                        for c in range(8):
                            ub_ = ub[(2 * bk + c // 4) % NR]; pp = pa[bk][c // 4]
                            for k in range(KC):
                                OP("pe", lambda e, k=k, c=c, ub_=ub_, pp=pp: e.matmul(pp[:, (c % 4) * 128:(c % 4 + 1) * 128], lhsT=ub_[:, k, (c % 4) * 128:(c % 4 + 1) * 128],
                                                                                     rhs=hT_[:, k, :128], start=(k == 0), stop=(k == KC - 1)),
                                   reads=[ub_, hT_], writes=[pp])
                        for hf in range(2):
                            OP("pe", lambda e, hf=hf: e.matmul(pg[bk][hf][:, :], lhsT=zb16[:], rhs=hT_[:, 0:4, :].rearrange("p a b -> p (a b)"), start=True, stop=False),
                               reads=[zb16, hT_], writes=[pg[bk][hf]])
                        g_ = gaT[bk % 2]
                        for hf in range(2):
                            OP("act", lambda e, hf=hf: e.activation(out=g_[:, hf * 512:(hf + 1) * 512], in_=pa[bk][hf][:, :], func=AF.Gelu),
                               reads=[pa[bk][hf]], writes=[g_])

                    def g1(n_):
                        bk, h = its[n_]
                        a0 = bk * 8
                        sbk = Sblk[n_ % NB]; exk = Exb[n_ % NB]
                        eng = "pool" if (n_ % 2 == 1) else "dve"
                        OP(eng, lambda e: e.tensor_tensor(out=sbk[:], in0=sA[:, h, a0:a0 + 8].unsqueeze(2).to_broadcast([128, 8, 128]),
                                                          in1=sc4[:, h, 1, :].unsqueeze(1).to_broadcast([128, 8, 128]), op=ALU.add),
                           reads=[sA, sc], writes=[sbk])
                        OP("act", lambda e: e.activation(out=exk[:], in_=sbk[:], func=AF.Exp, bias=beta[:, h:h + 1]), reads=[sbk, beta], writes=[exk])

                    def g2(n_):
                        bk, h = its[n_]
                        sbk = Sblk[n_ % NB]; exk = Exb[n_ % NB]; tg = tmpg[n_ % NB]
                        sb2 = sbk[:].rearrange("p a b -> p (a b)"); ex2 = exk[:].rearrange("p a b -> p (a b)")
                        OP("dve", lambda e: e.scalar_tensor_tensor(out=tg[:], in0=sb2, scalar=0.0, in1=ex2, op0=ALU.is_ge, op1=ALU.mult),
                           reads=[sbk, exk], writes=[tg])
                        for c in range(8):
                            pp = pg[bk][c // 4]
                            OP("pe", lambda e, c=c, pp=pp: e.matmul(pp[:, (c % 4) * 128:(c % 4 + 1) * 128], lhsT=tg[:, c * 128:(c + 1) * 128], rhs=ident[:],
                                                                   start=False, stop=(h == 7 and c % 4 == 3)), reads=[tg, ident], writes=[pp])

                    def blk_end(bk):
                        g_ = gaT[bk % 2]; w_ = WT[bk % 2]
                        for hf in range(2):
                            OP("dve", lambda e, hf=hf: e.tensor_tensor(out=w_[:, hf * 4:(hf + 1) * 4, :].rearrange("p a b -> p (a b)"), in0=g_[:, hf * 512:(hf + 1) * 512],
                                                                        in1=pg[bk][hf][:, :], op=ALU.mult), reads=[g_, pg[bk][hf]], writes=[w_])
                        for c in range(8):
                            vb_ = vb[(2 * bk + c // 4) % NR]
                            for hf in range(2):
                                OP("pe", lambda e, c=c, hf=hf, vb_=vb_: e.matmul(po[hf][:, :], lhsT=w_[:, c, :], rhs=vb_[:, c % 4, hf * 512:(hf + 1) * 512],
                                                                                start=(bk == 0 and c == 0), stop=(bk == NBK - 1 and c == 7)),
                                   reads=[w_, vb_], writes=[po[hf]])

                    ldu(0); ldu(1); ldv(0); ldv(1); ldv(2); ldv(3)
                    LAG = 2
                    pending_end = {}
                    for n_ in range(len(its) + LAG + 4):
                        if n_ < len(its):
                            bk, h = its[n_]
                            if h == 0:
                                blk_start(bk)
                                if 2 * bk + 2 < NHB:
                                    ldu(2 * bk + 2); ldu(2 * bk + 3)
                            g1(n_)
                        m_ = n_ - LAG
                        if 0 <= m_ < len(its):
                            g2(m_)
                            if its[m_][1] == 7:
                                pending_end[n_ + 3] = its[m_][0]
                        if n_ in pending_end:
                            bke = pending_end.pop(n_)
                            blk_end(bke)
                            if 2 * (bke + 2) < NHB:
                                ldv(2 * (bke + 2)); ldv(2 * (bke + 2) + 1)
                    for hf in range(2):
                        OP("act", lambda e, hf=hf: e.copy(out=otm[:, hf * 512:(hf + 1) * 512], in_=po[hf][:, :]), reads=[po[hf]], writes=[otm])
                    for m in range(KC):
                        pt = nps()
                        OP("pe", lambda e, m=m, pt=pt: e.transpose(out=pt[:, :128], in_=otm[:, m * 128:(m + 1) * 128], identity=identF[:]), reads=[otm, identF], writes=[pt])
                        OP("dve", lambda e, m=m, pt=pt: e.scalar_tensor_tensor(out=xt_[:, m, :128], in0=pt[:, :128], scalar=modv[:, 40 + m, s:s + 1], in1=xt_[:, m, :128],
                                                                               op0=ALU.mult, op1=ALU.add), reads=[pt, modv, xt_], writes=[xt_])
                    if last_layer and stage == 99:
                        norm_mod(128, fgt, zerob, 0, 0, tmpo, xt_, tmp8, sq8)
                        DMA("sp", rsview(yT_out, t0, 128), tmpo[:], reads=[tmpo], writes=[ybuf])
                    elif dyn:
                        DMA("sp", rsview(HXs[t0 // CW], t0 % CW, 128), xt_[:, :, :128], reads=[xt_], writes=[HXb[t0 // CW]])
                    else:
                        DMA("sp", rsview(R, t0, 128), xt_[:, :, :128], reads=[xt_], writes=[rb])
            if not (last_layer and stage == 99):
                if pair:
                    for j in range(NCW):
                        fw.collective("AllGather", [HXs[j].opt()], [HGs[j].opt()], [[0, 1], [2, 3], [4, 5], [6, 7]][:ncores // 2], reads=[HXb[j]], writes=[HGb[j]])
                    for j in range(NCW):
                        for r in range(2):
                            c0 = r * half + j * CW
                            DMA("sp", XR[:, c0:c0 + CW], HGs[j][r * D:(r + 1) * D, :], reads=[HGb[j]], writes=list(XRb))
                else:
                    for j in range(NCW):
                        DMA("sp", XR[:, j * CW:(j + 1) * CW], HXs[j][:, :], reads=[HXb[j]], writes=list(XRb))
            fw.barrier()
            pst.close()

        for l in range(depth):
            last_layer = (l == depth - 1)
            mst = ExitStack()

            def sbm(name, shape, dt=F32):
                return Tile(mst.enter_context(nc.sbuf_tensor(f"m{l}_" + name, list(shape), dt)), name)
            winb = sbm("winb", [128, KC, 4096], BF16)
            woutb = sbm("woutb", [128, KC, D], BF16)
            wmb = [sbm(f"wmb{i}", [128, KC, 256]) for i in range(2)]
            snaps = [sbm(f"snap{i}", [128, 2 * (NT // 128) * 2, 128]) for i in range(2)]
            snapb = [sbm(f"snapb{i}", [128, 2 * (NT // 128) * 2, 128], BF16) for i in range(2)]
            W = {n_: sbm("w_" + n_, [128, NT]) for n_ in ["sg", "f", "key", "lf", "b", "bc", "e1", "e2", "e3", "e4"]}
            qp = sbm("qp", [128, NT], BF16); qin = sbm("qin", [128, NT], BF16); kp = sbm("kp", [128, NT], BF16); kout = sbm("kout", [128, NT], BF16)
            vtm = sbm("vtm", [128, NT // 128, 512], BF16); kotm2 = sbm("kotm2", [128, NT // 128, 4, 128], BF16)
            attm2 = sbm("attm2", [128, NT // 128, 128], BF16)
            osb = sbm("osb", [128, 4, NT]); ofl = sbm("ofl", [128, 4, NT]); sgl = sbm("sgl", [128, 4, NT])
            stg = sbm("stg", [128, 4, NT])
            mix = sbm("mix", [128, KC, NT], BF16)
            uh = sbm("uh", [128, 4, NT + 128]); bgl = sbm("bgl", [128, 4, NT]); cva = sbm("cva", [128, NT])
            zbl = sbm("zbl", [128, 4, NT]); ql = sbm("ql", [128, 4, NT])
            xt = sbm("xt", [128, KC, NT]); tmp8 = sbm("tmp8", [128, KC, NT]); sq8 = sbm("sq8", [128, KC, NT], BF16)
            hT = sbm("hT", [128, KC, NT], BF16)
            alloc_psum(mst, f"m{l}", 6, 2); cnt["n"] = 6
            DMA("sp", n1t[:], n1_in[l], writes=[n1t]); DMA("sp", n2t[:], n2_in[l], writes=[n2t])
            DMA("sp", bmt[:], bmod_in[l], writes=[bmt]); DMA("sp", cwt[:], cw_in[l], writes=[cwt])
            for k in range(KC):
                DMA("pool", winb[:, k, :], win_in[l, k * 128:(k + 1) * 128, :], writes=[winb])
            for k in range(KC):
                DMA("pool", woutb[:, k, :], wout_in[l, k * 128:(k + 1) * 128, :], writes=[woutb])
            uTl_ = uT_in[l].rearrange("(k p) e -> p k e", p=128)
            for hb_ in range(NEXP // 512):
                pending_casts.append(lambda hb_=hb_, uTl_=uTl_: DMA("pool", U16[hb_], uTl_[:, :, hb_ * 512:(hb_ + 1) * 512], writes=[U16b]))
            for r_ in range(16):
                pending_casts.append(lambda r_=r_, l=l: DMA("pool", V16[r_ * 1024:(r_ + 1) * 1024, :], v_in[l, r_ * 1024:(r_ + 1) * 1024, :], writes=[V16b]))
            pm = nps()
            for cb in range(24):
                wm = wmb[cb % 2]
                DMA("sp", wm[:], wmod_in[l].rearrange("(k p) c -> p k c", p=128)[:, :, cb * 256:(cb + 1) * 256], writes=[wm])
                for cc in range(2):
                    c = cb * 2 + cc
                    for k in range(KC):
                        OP("pe", lambda e, wm=wm, cc=cc, c=c, k=k: e.matmul(pm[:, 2 * c:2 * c + 2], lhsT=wm[:, k, cc * 128:(cc + 1) * 128],
                                                                            rhs=cond[:, k, :], start=(k == 0), stop=(k == KC - 1)),
                           reads=[wm, cond], writes=[pm])
            OP("dve", lambda e: e.tensor_tensor(out=modv[:], in0=pm[:, 0:96].rearrange("p (c s) -> p c s", s=2),
                                                 in1=bmt[:].unsqueeze(2).to_broadcast([128, 48, 2]), op=ALU.add), reads=[pm, bmt], writes=[modv])
            for (A, nt_, g) in [(A1, n1t, 1), (A2, n2t, 4)]:
                OP("dve", lambda e, A=A, g=g: e.tensor_scalar(out=A[:], in0=modv[:, g * 8:(g + 1) * 8, :], scalar1=1.0, scalar2=None, op0=ALU.add),
                   reads=[modv], writes=[A])
                OP("dve", lambda e, A=A, nt_=nt_: e.tensor_tensor(out=A[:], in0=A[:], in1=nt_[:].unsqueeze(2).to_broadcast([128, KC, 2]), op=ALU.mult),
                   reads=[A, nt_], writes=[A])

            for sname in ["c", "x"]:
                S_ = streams[sname]
                s = S_["s"]; n = S_["n"]; ntile = S_["nt"]; R = S_["R"]; Rb = S_["Rb"]; T = S_["T"]
                nsub = n // 128
                if sname == "c":
                    OP("dve", lambda e: e.memset(Sst[:], 0.0), writes=[Sst])
                only_states = (sname == "c" and last_layer)
                for i in range(ntile):
                    t0 = i * n
                    DMA("sp", xt[:, :, :n], rsview(R, t0, n), reads=[Rb[i]], writes=[xt])
                    norm_mod(n, A1, modv, 0, s, hT, xt, tmp8, sq8)
                    for j in range(nsub):
                        pv = nps()
                        for k in range(KC):
                            OP("pe", lambda e, k=k, j=j, pv=pv: e.matmul(pv[:, :], lhsT=hT[:, k, j * 128:(j + 1) * 128], rhs=winb[:, k, 0:512],
                                                                         start=(k == 0), stop=(k == KC - 1)), reads=[hT, winb], writes=[pv])
                        OP("act", lambda e, j=j, pv=pv: e.copy(out=vtm[:, j, :], in_=pv[:, :]), reads=[pv], writes=[vtm])
                    DMA("sp", VS[t0:t0 + n, :].rearrange("(j p) c -> p j c", p=128), vtm[:, :nsub, :], reads=[vtm], writes=[scr_b["VS"][i]])

                    def proj(cc):
                        ps = nps()
                        for k in range(KC):
                            OP("pe", lambda e, k=k, ps=ps: e.matmul(ps[:, :n], lhsT=winb[:, k, cc * 128:(cc + 1) * 128], rhs=hT[:, k, :n],
                                                                    start=(k == 0), stop=(k == KC - 1)), reads=[hT, winb], writes=[ps])
                        return ps
                    for h in range(4):
                        ps = proj(8 + h)
                        OP("act", lambda e, h=h, ps=ps: e.copy(out=stg[:, h, :n], in_=ps[:, :n]), reads=[ps], writes=[stg])
                    DMA("sp", s4view(ZB, t0, n), stg[:, :, :n], reads=[stg], writes=[scr_b["ZB"][i]])
                    for h in range(4):
                        psq = proj(12 + h)
                        OP("act", lambda e, h=h, psq=psq: e.copy(out=ql[:, h, :n], in_=psq[:, :n]), reads=[psq], writes=[ql])
                        psz = proj(4 + h)
                        zps_t[0] = psz
                        last = gate_math(psz[:, :n], ql[:, h, :n], ql, l, 0, h, n, False)
                        recur(l, 0, h, n, False, last, osb)
                    DMA("sp", s4view(QS, t0, n), ql[:, :, :n], reads=[ql], writes=[scr_b["QS"][i]])
                    DMA("sp", s4view(OF, t0, n), osb[:, :, :n], reads=[osb], writes=[scr_b["OF"][i]])
                    if only_states:
                        continue
                    for h in range(4):
                        ps = proj(16 + h)
                        OP("act", lambda e, h=h, ps=ps: e.activation(out=stg[:, h, :n], in_=ps[:, :n], func=AF.Silu), reads=[ps], writes=[stg])
                    DMA("sp", s4view(SG, t0, n), stg[:, :, :n], reads=[stg], writes=[scr_b["SG"][i]])
                    for c in range(4):
                        pc = proj(20 + c)
                        OP("act", lambda e, pc=pc: e.copy(out=cva[:, :n], in_=pc[:, :n]), reads=[pc], writes=[cva])
                        ph = proj(28 + c)
                        OP("dve", lambda e, c=c, ph=ph: e.tensor_tensor(out=stg[:, c, :n], in0=ph[:, :n], in1=cva[:, :n], op=ALU.mult),
                           reads=[ph, cva], writes=[stg])
                    DMA("sp", s4view(US, t0, n), stg[:, :, :n], reads=[stg], writes=[scr_b["US"][i]])
                    for c in range(4):
                        pb = proj(24 + c)
                        OP("act", lambda e, c=c, pb=pb: e.copy(out=stg[:, c, :n], in_=pb[:, :n]), reads=[pb], writes=[stg])
                    DMA("sp", s4view(BG, t0, n), stg[:, :, :n], reads=[stg], writes=[scr_b["BG"][i]])
                for i in range(ntile - 1, -1, -1):
                    t0 = i * n
                    if sname == "x":
                        for _ in range(6):
                            if pending_casts:
                                pending_casts.pop(0)()
                    DMA("sp", zbl[:, :, :n], s4view(ZB, t0, n), reads=[scr_b["ZB"][i]], writes=[zbl])
                    DMA("sp", ql[:, :, :n], s4view(QS, t0, n), reads=[scr_b["QS"][i]], writes=[ql])
                    DMA("sp", vtm[:, :nsub, :], VS[t0:t0 + n, :].rearrange("(j p) c -> p j c", p=128), reads=[scr_b["VS"][i]], writes=[vtm])
                    for h in range(4):
                        zps_t[0] = zbl
                        last = gate_math(zbl[:, h, :n], ql[:, h, :n], ql, l, 1, h, n, True)
                        recur(l, 1, h, n, True, last, osb)
                    if only_states:
                        continue
                    DMA("sp", ofl[:, :, :n], s4view(OF, t0, n), reads=[scr_b["OF"][i]], writes=[ofl])
                    DMA("sp", sgl[:, :, :n], s4view(SG, t0, n), reads=[scr_b["SG"][i]], writes=[sgl])
                    DMA("sp", bgl[:, :, :n], s4view(BG, t0, n), reads=[scr_b["BG"][i]], writes=[bgl])
                    OP("pool", lambda e: e.memset(uh[:], 0.0), writes=[uh])
                    lo = 64 if t0 > 0 else 0
                    hi = 64 if t0 + n < T else 0
                    rd = [scr_b["US"][i]] + ([scr_b["US"][i - 1]] if lo else []) + ([scr_b["US"][i + 1]] if hi else [])
                    DMA("sp", uh[:, :, 64 - lo:64 + n + hi], s4view(US, t0 - lo, n + lo + hi), reads=rd, writes=[uh])
                    DMA("sp", xt[:, :, :n], rsview(R, t0, n), reads=[Rb[i]], writes=[xt])
                    OP("dve", lambda e: e.tensor_tensor(out=osb[:, :, :n], in0=osb[:, :, :n], in1=ofl[:, :, :n], op=ALU.add), reads=[osb, ofl], writes=[osb])
                    OP("act", lambda e: e.activation(out=sq8[:, 0:4, :n], in_=osb[:, :, :n], func=AF.Square), reads=[osb], writes=[sq8])
                    for h in range(4):
                        ps = nps()
                        OP("pe", lambda e, h=h, ps=ps: e.matmul(ps[:, :n], lhsT=ones[:], rhs=sq8[:, h, :n], start=True, stop=True), reads=[ones, sq8], writes=[ps])
                        OP("act", lambda e, ps=ps: e.activation(out=rstd[:, :n], in_=ps[:, :n], func=AF.Sqrt, scale=1.0 / 128, bias=epsc[:]),
                           reads=[ps, epsc], writes=[rstd])
                        OP("dve", lambda e: e.reciprocal(out=rstd[:, :n], in_=rstd[:, :n]), reads=[rstd], writes=[rstd])
                        OP("dve", lambda e, h=h: e.tensor_tensor(out=osb[:, h, :n], in0=osb[:, h, :n], in1=rstd[:, :n], op=ALU.mult), reads=[osb, rstd], writes=[osb])
                        OP("dve", lambda e, h=h: e.tensor_tensor(out=mix[:, h, :n], in0=osb[:, h, :n], in1=sgl[:, h, :n], op=ALU.mult), reads=[osb, sgl], writes=[mix])
                    for c in range(4):
                        ctr = uh[:, c, 64:64 + n]
                        OP("dve", lambda e, c=c, ctr=ctr: e.tensor_scalar(out=cva[:, :n], in0=ctr, scalar1=cwt[:, c, 1:2], scalar2=None, op0=ALU.mult),
                           reads=[uh, cwt], writes=[cva])
                        if sname == "x" and c >= 2:
                            OP("dve", lambda e, c=c: e.scalar_tensor_tensor(out=cva[:, :n], in0=uh[:, c, 0:n], scalar=cwt[:, c, 0:1], in1=cva[:, :n],
                                                                            op0=ALU.mult, op1=ALU.add), reads=[uh, cwt, cva], writes=[cva])
                            OP("dve", lambda e, c=c: e.scalar_tensor_tensor(out=cva[:, :n], in0=uh[:, c, 128:128 + n], scalar=cwt[:, c, 2:3], in1=cva[:, :n],
                                                                            op0=ALU.mult, op1=ALU.add), reads=[uh, cwt, cva], writes=[cva])
                        elif sname == "x":
                            u3 = uh[:, c, 64:64 + n].rearrange("p (r w) -> p r w", w=64)
                            c3 = cva[:, :n].rearrange("p (r w) -> p r w", w=64)
                            OP("dve", lambda e, c=c, u3=u3, c3=c3: e.scalar_tensor_tensor(out=c3[:, :, 1:64], in0=u3[:, :, 0:63], scalar=cwt[:, c, 0:1], in1=c3[:, :, 1:64],
                                                                                        op0=ALU.mult, op1=ALU.add), reads=[uh, cwt, cva], writes=[cva])
                            OP("dve", lambda e, c=c, u3=u3, c3=c3: e.scalar_tensor_tensor(out=c3[:, :, 0:63], in0=u3[:, :, 1:64], scalar=cwt[:, c, 2:3], in1=c3[:, :, 0:63],
                                                                                        op0=ALU.mult, op1=ALU.add), reads=[uh, cwt, cva], writes=[cva])
                        else:
                            OP("dve", lambda e, c=c: e.scalar_tensor_tensor(out=cva[:, :n], in0=uh[:, c, 63:63 + n], scalar=cwt[:, c, 0:1], in1=cva[:, :n],
                                                                            op0=ALU.mult, op1=ALU.add), reads=[uh, cwt, cva], writes=[cva])
                            OP("dve", lambda e, c=c: e.scalar_tensor_tensor(out=cva[:, :n], in0=uh[:, c, 65:65 + n], scalar=cwt[:, c, 2:3], in1=cva[:, :n],
                                                                            op0=ALU.mult, op1=ALU.add), reads=[uh, cwt, cva], writes=[cva])
                        OP("dve", lambda e, c=c: e.tensor_tensor(out=mix[:, 4 + c, :n], in0=cva[:, :n], in1=bgl[:, c, :n], op=ALU.mult), reads=[cva, bgl], writes=[mix])
                    for m in range(KC):
                        ps = nps()
                        for fch in range(KC):
                            OP("pe", lambda e, m=m, fch=fch, ps=ps: e.matmul(ps[:, :n], lhsT=woutb[:, fch, m * 128:(m + 1) * 128], rhs=mix[:, fch, :n],
                                                                             start=(fch == 0), stop=(fch == KC - 1)), reads=[woutb, mix], writes=[ps])
                        OP("dve", lambda e, m=m, ps=ps: e.scalar_tensor_tensor(out=xt[:, m, :n], in0=ps[:, :n], scalar=modv[:, 16 + m, s:s + 1], in1=xt[:, m, :n],
                                                                               op0=ALU.mult, op1=ALU.add), reads=[ps, modv, xt], writes=[xt])
                    DMA("sp", rsview(R, t0, n), xt[:, :, :n], reads=[xt], writes=[Rb[i]])
            fw.barrier()
            mst.close()
            if stage == 1:
                break
            peer_layer(l, last_layer)
            if stage == 2:
                break

        if stage in (1, 2):
            xt = sb("xo", [128, KC, NT])
            for i in range(seq // NT):
                DMA("sp", xt[:, :, :NT], rsview(XR, i * NT, NT), reads=[XRb[i]], writes=[xt])
                DMA("sp", rsview(yT_out, i * NT, NT), xt[:, :, :NT], reads=[xt], writes=[dbuf("y")])
            if dbg:
                DMA("sp", xt[:, :, :ctxlen], rsview(CR, 0, ctxlen), reads=[CRb[0]], writes=[xt])
                DMA("sp", rsview(dbgc_out, 0, ctxlen), xt[:, :, :ctxlen], reads=[xt], writes=[dbuf("y2")])
        fw.finish()
        print("instructions:", fw.ninst, {k: v.count for k, v in fw.E.items()})
    return nc


def make_consts():
    bf = ml_dtypes.bfloat16
    ident = np.eye(128, dtype=np.float32).astype(bf)
    ones = np.ones((128, 128), np.float32).astype(bf)
    s = np.arange(128)[:, None]; t = np.arange(128)[None, :]
    same = (s // CH) == (t // CH)
    maskF = (same & (s <= t)).astype(np.float32).astype(bf)
    maskB = (same & (s >= t)).astype(np.float32).astype(bf)
    resetm = np.ones((128, NT), np.float32); resetm[:, ::CH] = 0.0
    rowm = (np.arange(128)[:, None] // CH == np.arange(4)[None, :]).astype(np.float32)
    return dict(identF=np.eye(128, dtype=np.float32), ident=ident, ones=ones, maskF=maskF, maskB=maskB, resetm=resetm, rowm=rowm)


def prep_shared(inp, depth):
    f = lambda a: np.ascontiguousarray(np.asarray(a, dtype=np.float32))
    sh = {}
    sh["w_mod"] = f(inp["w_mod"])
    sh["b_mod"] = f(np.asarray(inp["b_mod"]).reshape(depth, 48, 128).transpose(0, 2, 1))
    sh["n1"] = f(np.asarray(inp["norm1_g"]).reshape(depth, KC, 128).transpose(0, 2, 1))
    sh["n2"] = f(np.asarray(inp["norm2_g"]).reshape(depth, KC, 128).transpose(0, 2, 1))
    sh["fg"] = f(np.repeat(np.asarray(inp["final_g"]).reshape(KC, 128).T[:, :, None], 2, axis=2))
    sh["w_in"] = f(inp["w_in"]); sh["w_out"] = f(inp["w_out"])
    sh["cw"] = f(np.asarray(inp["conv_w"]).reshape(depth, 3, 4, 128).transpose(0, 3, 2, 1))
    sh["lbl"] = f(np.asarray(inp["lb_logits"]).reshape(depth, 2, 4, 128).transpose(3, 0, 1, 2))
    sh["wq"] = f(inp["peer_wq"])
    sh["skT"] = f(np.asarray(inp["peer_subkeys"]).reshape(depth, 16, 128, 128).transpose(0, 3, 1, 2))
    sh["uT"] = f(np.asarray(inp["peer_u"]).transpose(0, 2, 1))
    sh["v"] = f(inp["peer_v"])
    sh.update(make_consts())
    return sh


def prep_core(inp, b, parity=0, half=0):
    f = lambda a: np.ascontiguousarray(np.asarray(a, dtype=np.float32))
    m = {"par": np.array([[parity * half]], np.int32)}
    m["xT"] = f(np.asarray(inp["x"])[b].T)
    m["cxT"] = f(np.asarray(inp["ctx"])[b].T)
    cv = np.stack([np.asarray(inp["c"])[b], np.asarray(inp["c_ctx"])], axis=-1)
    m["cv"] = f(cv.reshape(KC, 128, 2).transpose(1, 0, 2))
    return m


def kernel(**inp):
    x = np.asarray(inp["x"])
    B, seq, _ = x.shape
    ctxlen = np.asarray(inp["ctx"]).shape[1]
    depth = np.asarray(inp["w_in"]).shape[0]
    nc = build(depth, seq, ctxlen, ncores=2 * B)
    sh = prep_shared(inp, depth)
    half = seq // 2
    in_maps = []
    for c in range(2 * B):
        m = dict(sh); m.update(prep_core(inp, c // 2, c % 2, half)); in_maps.append(m)
    res = run_bass_kernel_spmd(nc, in_maps, core_ids=list(range(2 * B)))
    out = np.empty((B, seq, D), np.float32)
    for c in range(2 * B):
        out[c // 2, (c % 2) * half:(c % 2 + 1) * half, :] = res.results[c]["yT"].T
    return out
```
